# Optimizing a Trainium2 kernel written in Bass

```python
import math
import jax, jax.numpy as jnp
from jax import lax
import numpy as np

D_MODEL = 1024
BATCH = 8
SEQ = 4096
DEPTH = 2

N_META = 16
GRID_W = 64
HEAD_DIM = 64
EPS = 1e-6

SSD_HEADS = 4
SSD_HEAD_DIM = 64
SSD_INNER = SSD_HEADS * SSD_HEAD_DIM
SSD_GROUPS = 2
SSD_STATE = 128
SSD_CONV = 5
SSD_CHUNK = 128
SSD_CONV_DIM = SSD_INNER + 2 * SSD_GROUPS * SSD_STATE

GLA_HEADS = 4
GLA_DK = 32
GLA_DV = 64
GLA_KEY = GLA_HEADS * GLA_DK
GLA_VAL = GLA_HEADS * GLA_DV
GLA_GATE_RANK = 16
GLA_GATE_NORM = 16.0
GLA_CHUNK = 64

SWA_Q_HEADS = 4
SWA_KV_HEADS = 2
WINDOW = 128
ATT_BLOCK = 128

G2_Q_HEADS = 4
G2_KV_HEADS = 2
ROPE_THETA = 10000.0

REL_BUCKETS = 32
REL_MAX_DIST = 128

MIX_WIDTH = SSD_INNER + GLA_VAL + SWA_Q_HEADS * HEAD_DIM + G2_Q_HEADS * HEAD_DIM

FFN_DENSE = 2816
N_EXPERTS = 8
TOP_K = 2
FFN_EXPERT = 3584

IN_SIZES = (
    SSD_INNER, SSD_CONV_DIM, 2 * SSD_HEADS,
    GLA_KEY, GLA_KEY, GLA_VAL, GLA_VAL, 2 * GLA_GATE_RANK,
    SWA_Q_HEADS * HEAD_DIM, SWA_KV_HEADS * HEAD_DIM, SWA_KV_HEADS * HEAD_DIM,
    G2_Q_HEADS * HEAD_DIM, G2_KV_HEADS * HEAD_DIM, G2_KV_HEADS * HEAD_DIM,
)
IN_WIDTH = sum(IN_SIZES)

kernel_name = 'hybrid_parallel_heads_encoder'


def _rmsnorm(x, w):
    xf = x.astype(jnp.float32)
    xf = xf * lax.rsqrt(jnp.mean(jnp.square(xf), axis=-1, keepdims=True) + EPS)
    return (xf * w.astype(jnp.float32)).astype(x.dtype)


def _pad_front(t, n):
    return jnp.pad(t, [(0, 0), (n, 0)] + [(0, 0)] * (t.ndim - 2))


def _flip(t):
    return jnp.flip(t, axis=1)


def _scan_states(decay, states):
    def step(s, inp):
        d, st = inp
        return d * s + st, s
    s0 = jnp.zeros_like(states[:, 0])
    _, s_in = lax.scan(step, s0, (jnp.moveaxis(decay, 1, 0), jnp.moveaxis(states, 1, 0)))
    return jnp.moveaxis(s_in, 0, 1)


def _depthwise_conv_centred(u, w, b):
    k = w.shape[0]
    out = lax.conv_general_dilated(u, w[:, None, :].astype(u.dtype), window_strides=(1,),
                                   padding=[((k - 1) // 2, k // 2)],
                                   dimension_numbers=('NWC', 'WIO', 'NWC'),
                                   feature_group_count=u.shape[-1])
    return out + b.astype(u.dtype)


def _ssd_chunked(x, dt, a, b, c):
    bsz, t, h, p = x.shape
    q = SSD_CHUNK
    nc = t // q
    xd = (x * dt[..., None]).reshape(bsz, nc, q, h, p)
    a_cs = jnp.cumsum((dt * a).reshape(bsz, nc, q, h), axis=2)
    b = b.reshape(bsz, nc, q, h, -1)
    c = c.reshape(bsz, nc, q, h, -1)
    causal = jnp.tril(jnp.ones((q, q), bool))[..., None]
    seg = a_cs[:, :, :, None, :] - a_cs[:, :, None, :, :]
    decay = jnp.exp(jnp.where(causal, seg, -jnp.inf))
    scores = jnp.einsum('bclhn,bcshn->bclsh', c, b) * decay
    y_diag = jnp.einsum('bclsh,bcshp->bclhp', scores, xd)
    decay_to_end = jnp.exp(a_cs[:, :, -1:, :] - a_cs)
    chunk_states = jnp.einsum('bclhn,bclhp->bchpn', b * decay_to_end[..., None], xd)
    s_in = _scan_states(jnp.exp(a_cs[:, :, -1, :])[..., None, None], chunk_states)
    y_off = jnp.einsum('bclhn,bchpn->bclhp', c, s_in) * jnp.exp(a_cs)[..., None]
    return (y_diag + y_off).reshape(bsz, t, h, p)


def _ssd_mixer(z, xbc, dt_raw, conv_w, conv_b, dt_bias, a_log, d_skip, norm_w):
    f32 = jnp.float32
    bsz, L, _ = xbc.shape
    xbc = jax.nn.silu(_depthwise_conv_centred(xbc, conv_w, conv_b)).astype(f32)
    xs, bs, cs = jnp.split(xbc, [SSD_INNER, SSD_INNER + SSD_GROUPS * SSD_STATE], axis=-1)
    xs = xs.reshape(bsz, L, SSD_HEADS, SSD_HEAD_DIM)
    rep = SSD_HEADS // SSD_GROUPS
    bs = jnp.repeat(bs.reshape(bsz, L, SSD_GROUPS, SSD_STATE), rep, axis=2)
    cs = jnp.repeat(cs.reshape(bsz, L, SSD_GROUPS, SSD_STATE), rep, axis=2)
    dt = jax.nn.softplus(dt_raw.astype(f32).reshape(bsz, L, 2, SSD_HEADS) + dt_bias.astype(f32))
    a = -jnp.exp(a_log.astype(f32))
    pad = (-L) % SSD_CHUNK
    xp, bp, cp, dtp = [_pad_front(t, pad) for t in (xs, bs, cs, dt)]
    y_f = _ssd_chunked(xp, dtp[:, :, 0], a[0], bp, cp)
    y_b = _flip(_ssd_chunked(_flip(xp), _flip(dtp[:, :, 1]), a[1], _flip(bp), _flip(cp)))
    y = (y_f + y_b)[:, pad:] + d_skip.astype(f32)[:, None] * xs
    y = y.reshape(bsz, L, SSD_INNER) * jax.nn.silu(z.astype(f32))
    return _rmsnorm(y, norm_w).astype(z.dtype)


def _gla_chunked(q, k, v, g):
    bsz, t, h, dk = q.shape
    dv = v.shape[-1]
    cl = GLA_CHUNK
    n = t // cl
    q, k, g = [u.reshape(bsz, n, cl, h, dk) for u in (q, k, g)]
    v = v.reshape(bsz, n, cl, h, dv)
    bcum = jnp.cumsum(g, axis=2)
    q_t = q * jnp.exp(bcum)
    k_t = k * jnp.exp(-bcum)
    att = jnp.einsum('bclhk,bcshk->bchls', q_t, k_t)
    att = jnp.where(jnp.tril(jnp.ones((cl, cl), bool)), att, 0.0)
    o_intra = jnp.einsum('bchls,bcshv->bclhv', att, v)
    b_last = bcum[:, :, -1]
    chunk_states = jnp.einsum('bclhk,bclhv->bchkv', k * jnp.exp(b_last[:, :, None] - bcum), v)
    s_in = _scan_states(jnp.exp(b_last)[..., None], chunk_states)
    o_inter = jnp.einsum('bclhk,bchkv->bclhv', q_t, s_in)
    return (o_intra + o_inter).reshape(bsz, t, h, dv)


def _gla_mixer(q, k, v, r, a, gate_w2, gate_b, norm_w):
    f32 = jnp.float32
    bsz, L, _ = q.shape
    q = q.astype(f32).reshape(bsz, L, GLA_HEADS, GLA_DK) * GLA_DK ** -0.5
    k = k.astype(f32).reshape(bsz, L, GLA_HEADS, GLA_DK)
    v = v.astype(f32).reshape(bsz, L, GLA_HEADS, GLA_DV)
    a = a.astype(f32).reshape(bsz, L, 2, GLA_GATE_RANK)
    g = jax.nn.log_sigmoid(jnp.einsum('bldr,drk->bldk', a, gate_w2.astype(f32)) + gate_b.astype(f32)) / GLA_GATE_NORM
    g = g.reshape(bsz, L, 2, GLA_HEADS, GLA_DK)
    pad = (-L) % GLA_CHUNK
    qp, kp, vp, gp = [_pad_front(t, pad) for t in (q, k, v, g)]
    o_f = _gla_chunked(qp, kp, vp, gp[:, :, 0])
    o_b = _flip(_gla_chunked(_flip(qp), _flip(kp), _flip(vp), _flip(gp[:, :, 1])))
    o = _rmsnorm((o_f + o_b)[:, pad:], norm_w)
    o = o.reshape(bsz, L, GLA_VAL) * jax.nn.silu(r.astype(f32))
    return o.astype(r.dtype)


def _t5_bucket(rel):
    nb = REL_BUCKETS // 2
    max_exact = nb // 2
    ret = (rel > 0).astype(jnp.int32) * nb
    n = jnp.abs(rel)
    nf = jnp.maximum(n, 1).astype(jnp.float32)
    large = max_exact + (jnp.log(nf / max_exact) / math.log(REL_MAX_DIST / max_exact)
                         * (nb - max_exact)).astype(jnp.int32)
    large = jnp.minimum(large, nb - 1)
    return ret + jnp.where(n < max_exact, n, large)


def _swa_mixer(q, k, v, sink, rel_bias):
    f32 = jnp.float32
    bsz, L, _ = q.shape
    S = L - N_META
    blk = ATT_BLOCK
    nb = S // blk
    R = SWA_Q_HEADS // SWA_KV_HEADS
    G = SWA_KV_HEADS
    q = q.reshape(bsz, L, G, R, HEAD_DIM) * HEAD_DIM ** -0.5
    k = k.reshape(bsz, L, G, HEAD_DIM)
    v = v.reshape(bsz, L, G, HEAD_DIM)
    qm, qr = q[:, :N_META], q[:, N_META:]
    km, kr = k[:, :N_META], k[:, N_META:]
    vm, vr = v[:, :N_META], v[:, N_META:]

    def band(t):
        tp = jnp.pad(t, [(0, 0), (blk, blk), (0, 0), (0, 0)]).reshape(bsz, nb + 2, blk, G, HEAD_DIM)
        return jnp.concatenate([tp[:, :-2], tp[:, 1:-1], tp[:, 2:]], axis=2)

    def head_bias(bucket):
        bb = jnp.moveaxis(rel_bias[bucket], -1, -3).astype(f32)
        return bb.reshape(bb.shape[:-3] + (G, R) + bb.shape[-2:])

    sink_l = sink.astype(f32).reshape(G, R, 1, 1)
    kb, vb = band(kr), band(vr)
    qb = qr.reshape(bsz, nb, blk, G, R, HEAD_DIM)
    qi = jnp.arange(blk)
    ki = jnp.arange(3 * blk)
    rel = ki[None, :] - blk - qi[:, None]
    kidx = jnp.arange(nb)[:, None] * blk + ki[None, :] - blk
    mask = (jnp.abs(rel) <= WINDOW)[None] & ((kidx >= 0) & (kidx < S))[:, None, :]
    qpos = N_META + jnp.arange(S).reshape(nb, blk)
    rel_m = jnp.arange(N_META)[None, None, :] - qpos[:, :, None]
    s_band = jnp.einsum('bnqgrd,bnkgd->bngrqk', qb, kb).astype(f32) + head_bias(_t5_bucket(rel))
    s_band = jnp.where(mask[None, :, None, None], s_band, -jnp.inf)
    s_meta = jnp.einsum('bnqgrd,bmgd->bngrqm', qb, km).astype(f32) + head_bias(_t5_bucket(rel_m))
    s_sink = jnp.broadcast_to(sink_l, s_band.shape[:-1] + (1,))
    p = jax.nn.softmax(jnp.concatenate([s_band, s_meta, s_sink], axis=-1), axis=-1).astype(v.dtype)
    o_r = (jnp.einsum('bngrqk,bnkgd->bnqgrd', p[..., :3 * blk], vb)
           + jnp.einsum('bngrqm,bmgd->bnqgrd', p[..., 3 * blk:3 * blk + N_META], vm))
    o_r = o_r.reshape(bsz, S, SWA_Q_HEADS * HEAD_DIM)

    kc = jnp.concatenate([km, kr[:, :blk]], axis=1)
    vc = jnp.concatenate([vm, vr[:, :blk]], axis=1)
    rel_q = jnp.arange(N_META + blk)[None, :] - jnp.arange(N_META)[:, None]
    s_q = jnp.einsum('bqgrd,bkgd->bgrqk', qm, kc).astype(f32) + head_bias(_t5_bucket(rel_q))
    s_q = jnp.where(jnp.abs(rel_q) <= WINDOW, s_q, -jnp.inf)
    s_q_sink = jnp.broadcast_to(sink_l, s_q.shape[:-1] + (1,))
    p_q = jax.nn.softmax(jnp.concatenate([s_q, s_q_sink], axis=-1), axis=-1).astype(v.dtype)
    o_m = jnp.einsum('bgrqk,bkgd->bqgrd', p_q[..., :-1], vc).reshape(bsz, N_META, SWA_Q_HEADS * HEAD_DIM)
    return jnp.concatenate([o_m, o_r], axis=1)


def _axial_rope(L):
    f32 = jnp.float32
    n_tok = L - N_META
    rows = n_tok // GRID_W
    t = jnp.arange(rows * GRID_W)
    meta_pos = jnp.arange(N_META) - N_META
    row = jnp.concatenate([meta_pos, t // GRID_W]).astype(f32)
    col = jnp.concatenate([meta_pos, t % GRID_W]).astype(f32)
    half = HEAD_DIM // 2
    inv = ROPE_THETA ** (-jnp.arange(0, half, 2, dtype=f32) / half)
    ang = jnp.concatenate([row[:, None] * inv, col[:, None] * inv], axis=-1)
    return jnp.cos(ang), jnp.sin(ang)


def _apply_rope(x, cos, sin):
    shp = x.shape
    xf = x.astype(jnp.float32).reshape(shp[:-1] + (HEAD_DIM // 2, 2))
    bshape = (1, shp[1]) + (1,) * (x.ndim - 3) + (HEAD_DIM // 2,)
    c = cos.reshape(bshape)
    s = sin.reshape(bshape)
    x0, x1 = xf[..., 0], xf[..., 1]
    return jnp.stack([x0 * c - x1 * s, x0 * s + x1 * c], axis=-1).reshape(shp).astype(x.dtype)


def _gqa2d_mixer(q, k, v, qn_w, kn_w):
    bsz, L, _ = q.shape
    G = G2_KV_HEADS
    R = G2_Q_HEADS // G2_KV_HEADS
    blk = ATT_BLOCK
    q = _rmsnorm(q.reshape(bsz, L, G, R, HEAD_DIM), qn_w)
    k = _rmsnorm(k.reshape(bsz, L, G, HEAD_DIM), kn_w)
    v = v.reshape(bsz, L, G, HEAD_DIM)
    cos, sin = _axial_rope(L)
    q = _apply_rope(q, cos, sin) * HEAD_DIM ** -0.5
    k = _apply_rope(k, cos, sin)
    pad = (-L) % blk
    nq = (L + pad) // blk
    qp = jnp.moveaxis(_pad_front(q, pad).reshape(bsz, nq, blk, G, R, HEAD_DIM), 1, 0)

    def block(qb):
        s = jnp.einsum('bqgrd,bkgd->bgrqk', qb, k).astype(jnp.float32)
        p = jax.nn.softmax(s, axis=-1).astype(v.dtype)
        return jnp.einsum('bgrqk,bkgd->bqgrd', p, v)

    o = lax.map(block, qp)
    o = jnp.moveaxis(o, 0, 1).reshape(bsz, nq * blk, G2_Q_HEADS * HEAD_DIM)
    return o[:, pad:]


def _swiglu(u, wg, wu, wd):
    return (jax.nn.silu(u @ wg) * (u @ wu)) @ wd


def _moe(u, router, wg, wu, wd):
    bsz, L, d = u.shape
    t = u.reshape(-1, d)
    logits = (t @ router).astype(jnp.float32)
    top_v, top_i = lax.top_k(logits, TOP_K)
    gates = jax.nn.softmax(top_v, axis=-1)
    combine = jnp.sum(jax.nn.one_hot(top_i, N_EXPERTS, dtype=jnp.float32) * gates[..., None], axis=1)
    out = jnp.zeros_like(t)
    for e in range(N_EXPERTS):
        out = out + combine[:, e:e + 1].astype(t.dtype) * _swiglu(t, wg[e], wu[e], wd[e])
    return out.reshape(bsz, L, d)


def setup_inputs(seed: int = 0) -> dict:
    key = jax.random.key(seed)
    ks = iter(jax.random.split(key, 32))
    f32 = jnp.float32
    n_dense = (DEPTH + 1) // 2
    n_moe = DEPTH // 2

    def nrm(shape, scale):
        return jax.random.normal(next(ks), shape, f32) * scale

    def gain(shape):
        return 1.0 + 0.05 * jax.random.normal(next(ks), shape, f32)

    x = nrm((BATCH, SEQ, D_MODEL), 1.0)
    meta_tokens = nrm((N_META, D_MODEL), 1.0)
    rel_bias = nrm((REL_BUCKETS, SWA_Q_HEADS), 0.5)
    norm_mix_w = gain((DEPTH, D_MODEL))
    norm_ffn_w = gain((DEPTH, D_MODEL))
    w_in = nrm((DEPTH, D_MODEL, IN_WIDTH), D_MODEL ** -0.5)
    ssd_conv_w = nrm((DEPTH, SSD_CONV, SSD_CONV_DIM), SSD_CONV ** -0.5)
    ssd_conv_b = nrm((DEPTH, SSD_CONV_DIM), 0.02)
    dt0 = jnp.exp(jax.random.uniform(next(ks), (DEPTH, 2, SSD_HEADS), f32, math.log(1e-3), math.log(1e-1)))
    ssd_dt_bias = dt0 + jnp.log(-jnp.expm1(-dt0))
    ssd_a_log = jnp.log(jax.random.uniform(next(ks), (DEPTH, 2, SSD_HEADS), f32, 1.0, 16.0))
    ssd_d = gain((DEPTH, SSD_HEADS))
    ssd_norm_w = gain((DEPTH, SSD_INNER))
    gla_gate_w2 = nrm((DEPTH, 2, GLA_GATE_RANK, GLA_KEY), GLA_GATE_RANK ** -0.5)
    gla_gate_b = nrm((DEPTH, 2, GLA_KEY), 0.1)
    gla_norm_w = gain((DEPTH, GLA_DV))
    swa_sink = nrm((DEPTH, SWA_Q_HEADS), 0.5)
    gqa_q_norm_w = gain((DEPTH, HEAD_DIM))
    gqa_k_norm_w = gain((DEPTH, HEAD_DIM))
    w_out = nrm((DEPTH, MIX_WIDTH, D_MODEL), MIX_WIDTH ** -0.5)
    ffn_w_gate = nrm((n_dense, D_MODEL, FFN_DENSE), D_MODEL ** -0.5)
    ffn_w_up = nrm((n_dense, D_MODEL, FFN_DENSE), D_MODEL ** -0.5)
    ffn_w_down = nrm((n_dense, FFN_DENSE, D_MODEL), FFN_DENSE ** -0.5)
    moe_router = nrm((n_moe, D_MODEL, N_EXPERTS), D_MODEL ** -0.5)
    moe_w_gate = nrm((n_moe, N_EXPERTS, D_MODEL, FFN_EXPERT), D_MODEL ** -0.5)
    moe_w_up = nrm((n_moe, N_EXPERTS, D_MODEL, FFN_EXPERT), D_MODEL ** -0.5)
    moe_w_down = nrm((n_moe, N_EXPERTS, FFN_EXPERT, D_MODEL), FFN_EXPERT ** -0.5)
    final_norm_w = gain((D_MODEL,))
    return {'x': x, 'meta_tokens': meta_tokens, 'rel_bias': rel_bias,
            'norm_mix_w': norm_mix_w, 'norm_ffn_w': norm_ffn_w, 'w_in': w_in,
            'ssd_conv_w': ssd_conv_w, 'ssd_conv_b': ssd_conv_b, 'ssd_dt_bias': ssd_dt_bias,
            'ssd_a_log': ssd_a_log, 'ssd_d': ssd_d, 'ssd_norm_w': ssd_norm_w,
            'gla_gate_w2': gla_gate_w2, 'gla_gate_b': gla_gate_b, 'gla_norm_w': gla_norm_w,
            'swa_sink': swa_sink, 'gqa_q_norm_w': gqa_q_norm_w, 'gqa_k_norm_w': gqa_k_norm_w,
            'w_out': w_out, 'ffn_w_gate': ffn_w_gate, 'ffn_w_up': ffn_w_up, 'ffn_w_down': ffn_w_down,
            'moe_router': moe_router, 'moe_w_gate': moe_w_gate, 'moe_w_up': moe_w_up,
            'moe_w_down': moe_w_down, 'final_norm_w': final_norm_w}


def reference(x, meta_tokens, rel_bias, norm_mix_w, norm_ffn_w, w_in, ssd_conv_w, ssd_conv_b,
              ssd_dt_bias, ssd_a_log, ssd_d, ssd_norm_w, gla_gate_w2, gla_gate_b, gla_norm_w,
              swa_sink, gqa_q_norm_w, gqa_k_norm_w, w_out, ffn_w_gate, ffn_w_up, ffn_w_down,
              moe_router, moe_w_gate, moe_w_up, moe_w_down, final_norm_w):
    bsz = x.shape[0]
    offs = [int(o) for o in np.cumsum(IN_SIZES)[:-1]]
    meta = jnp.broadcast_to(meta_tokens[None].astype(x.dtype), (bsz, N_META, D_MODEL))
    h = jnp.concatenate([meta, x], axis=1)
    for i in range(DEPTH):
        u = _rmsnorm(h, norm_mix_w[i])
        proj = u @ w_in[i]
        (z, xbc, dt_raw, gq, gk, gv, gr, ga, sq, sk, sv, aq, ak, av) = jnp.split(proj, offs, axis=-1)
        y_ssd = _ssd_mixer(z, xbc, dt_raw, ssd_conv_w[i], ssd_conv_b[i], ssd_dt_bias[i],
                           ssd_a_log[i], ssd_d[i], ssd_norm_w[i])
        y_gla = _gla_mixer(gq, gk, gv, gr, ga, gla_gate_w2[i], gla_gate_b[i], gla_norm_w[i])
        y_swa = _swa_mixer(sq, sk, sv, swa_sink[i], rel_bias)
        y_g2 = _gqa2d_mixer(aq, ak, av, gqa_q_norm_w[i], gqa_k_norm_w[i])
        mixed = jnp.concatenate([y_ssd, y_gla, y_swa, y_g2], axis=-1)
        h = h + mixed @ w_out[i]
        u = _rmsnorm(h, norm_ffn_w[i])
        if i % 2 == 0:
            j = i // 2
            h = h + _swiglu(u, ffn_w_gate[j], ffn_w_up[j], ffn_w_down[j])
        else:
            j = i // 2
            h = h + _moe(u, moe_router[j], moe_w_gate[j], moe_w_up[j], moe_w_down[j])
    return _rmsnorm(h, final_norm_w)[:, N_META:]
```

```python
import contextlib
import math
import numpy as np
import concourse.bass as bass
import concourse.mybir as mybir
from concourse.bass_utils import run_bass_kernel_spmd

F32 = mybir.dt.float32
BF16 = mybir.dt.bfloat16
AF = mybir.ActivationFunctionType
ALU = mybir.AluOpType
AX = mybir.AxisListType

D = 1024
SEQ = 4096
NM = 16
L = SEQ + NM
DEPTH = 2
EPS = 1e-6
INW = 2856
FFN_F = 2816
MOE_F = 3584
NEXP = 8
NEG = -30000.0

O_Z, O_XS, O_B, O_C, O_DT = 0, 256, 512, 768, 1024
O_GQ, O_GK, O_GV, O_GR, O_GA = 1032, 1160, 1288, 1544, 1800
O_SQ, O_SK, O_SV = 1832, 2088, 2216
O_AQ, O_AK, O_AV = 2344, 2600, 2728

FM_TILES = [
    ("z0", [(O_Z, 128)]), ("z1", [(O_Z + 128, 128)]),
    ("xs0", [(O_XS, 128)]), ("xs1", [(O_XS + 128, 128)]),
    ("B0", [(O_B, 128)]), ("B1", [(O_B + 128, 128)]),
    ("C0", [(O_C, 128)]), ("C1", [(O_C + 128, 128)]),
    ("gq", [(O_GQ, 128)]), ("gk", [(O_GK, 128)]),
    ("gr0", [(O_GR, 128)]), ("gr1", [(O_GR + 128, 128)]),
    ("ga", [(O_GA, 32)]),
    ("sq0", [(O_SQ, 128)]), ("sq1", [(O_SQ + 128, 128)]),
    ("sk0", [(O_SK, 64), (O_SK, 64)]), ("sk1", [(O_SK + 64, 64), (O_SK + 64, 64)]),
    ("aq0", [(O_AQ, 128)]), ("aq1", [(O_AQ + 128, 128)]),
    ("ak0", [(O_AK, 64), (O_AK, 64)]), ("ak1", [(O_AK + 64, 64), (O_AK + 64, 64)]),
]
FM_IDX = {n: i for i, (n, _) in enumerate(FM_TILES)}
NFM = len(FM_TILES)
TM_SEGS = [(O_GV, 256), (O_SV, 128), (O_AV, 128), (O_DT, 8)]
TMW = 520
V_GV, V_SV, V_AV, V_DT = 0, 256, 384, 512

CHUNKS = [(0, NM)] + [(NM + 128 * i, 128) for i in range(32)]
GROUPS = [(0, NM)] + [(NM + 512 * i, 512) for i in range(8)]
HALVES = [(0, NM + 2048), (NM + 2048, 2048)]


def chunks_in(t0, n):
    return [(c0, cn) for (c0, cn) in CHUNKS if c0 >= t0 and c0 + cn <= t0 + n]


ENGS = ("pe", "act", "dve", "pool", "sp")
NDSEM = 8


class Buf:
    __slots__ = ("name", "w", "rc", "rd", "const")

    def __init__(self, name="b"):
        self.name = name
        self.w = None
        self.rc = {}
        self.rd = []
        self.const = False


class Prog:
    def __init__(self, nc):
        self.nc = nc
        self.q = {e: [] for e in ENGS}
        self.ndma = {e: 0 for e in ENGS}
        self.seen_c = {e: {} for e in ENGS}
        self.seen_d = {e: set() for e in ENGS}
        self.marked = {e: set() for e in ENGS}
        self.pending = {e: [] for e in ENGS}
        self.last_tok = None

    def _need(self, eng, tok):
        k, e2, i2 = tok
        if k == "c":
            if e2 == "pe" and eng == "pe":
                return False
            if self.seen_c[eng].get(e2, -1) >= i2:
                return False
            self.seen_c[eng][e2] = i2
            self.marked[e2].add(i2)
            return True
        if tok in self.seen_d[eng]:
            return False
        self.seen_d[eng].add(tok)
        return True

    def barrier(self):
        toks = []
        for e in ENGS:
            n = len(self.q[e])
            for i in range(n - 1, -1, -1):
                if self.q[e][i][2] < 0:
                    toks.append(("c", e, i))
                    break
            nd = self.ndma[e]
            for d in range(max(0, nd - NDSEM), nd):
                toks.append(("d", e, d))
        for e in ENGS:
            self.pending[e] = list(toks)

    def op(self, eng, fn, reads=(), writes=(), dma=False):
        idx = len(self.q[eng])
        deps = list(self.pending[eng])
        self.pending[eng] = []
        for b in reads:
            if b.w is not None:
                deps.append(b.w)
        for b in writes:
            if b.w is not None:
                deps.append(b.w)
            for e2, v in b.rc.items():
                deps.append(("c", e2, v))
            deps.extend(b.rd)
        best = {}
        dl = []
        for d in deps:
            if d[0] == "c":
                if d[1] == eng and d[2] >= idx:
                    continue
                if best.get(d[1], -1) < d[2]:
                    best[d[1]] = d[2]
            else:
                dl.append(d)
        waits = []
        for e2, i2 in best.items():
            if self._need(eng, ("c", e2, i2)):
                waits.append(("c", e2, i2))
        for d in dl:
            if self._need(eng, d):
                waits.append(d)
        if dma:
            didx = self.ndma[eng]
            self.ndma[eng] += 1
            if didx >= NDSEM:
                d = ("d", eng, didx - NDSEM)
                if self._need(eng, d):
                    waits.append(d)
            tok = ("d", eng, didx)
        else:
            didx = -1
            tok = ("c", eng, idx)
        self.q[eng].append((fn, waits, didx))
        for b in reads:
            if b.const:
                continue
            if dma:
                b.rd.append(tok)
            else:
                b.rc[eng] = idx
        for b in writes:
            b.w = tok
            b.rc = {}
            b.rd = []
        self.last_tok = tok
        return tok

    def emit(self):
        nc = self.nc
        with contextlib.ExitStack() as st:
            csem = {e: st.enter_context(nc.semaphore("cs_" + e)) for e in ENGS}
            dsem = {e: [st.enter_context(nc.semaphore("ds_%s%d" % (e, i))) for i in range(NDSEM)]
                    for e in ENGS if self.ndma[e]}
            block = st.enter_context(nc.Block())
            cnt = {}
            for e in ENGS:
                c = 0
                arr = []
                m = self.marked[e]
                for i in range(len(self.q[e])):
                    if i in m:
                        c += 1
                    arr.append(c)
                cnt[e] = arr

            def run(en, eobj):
                mk = self.marked[en]
                for i, (fn, waits, didx) in enumerate(self.q[en]):
                    for (k, e2, i2) in waits:
                        if k == "c":
                            eobj.wait_ge(csem[e2], cnt[e2][i2])
                        else:
                            eobj.wait_ge(dsem[e2][i2 % NDSEM], 16 * (i2 // NDSEM + 1))
                    ins = fn(eobj)
                    if didx >= 0:
                        ins.then_inc(dsem[en][didx % NDSEM], 16)
                    elif i in mk:
                        ins.then_inc(csem[en], 1)
                n = self.ndma[en]
                for s in range(NDSEM):
                    k = len(range(s, n, NDSEM))
                    if k:
                        eobj.wait_ge(dsem[en][s], 16 * k)

            block.tensor(lambda e: run("pe", e))
            block.scalar(lambda e: run("act", e))
            block.vector(lambda e: run("dve", e))
            block.gpsimd(lambda e: run("pool", e))
            block.sync(lambda e: run("sp", e))


def t5_bucket_np(rel):
    nb = 16
    max_exact = 8
    rel = rel.astype(np.int32)
    ret = (rel > 0).astype(np.int32) * nb
    n = np.abs(rel)
    nf = np.maximum(n, 1).astype(np.float32)
    large = max_exact + (np.log(nf / np.float32(max_exact)) / np.float32(math.log(128 / max_exact))
                         * np.float32(nb - max_exact)).astype(np.int32)
    large = np.minimum(large, nb - 1)
    return ret + np.where(n < max_exact, n, large)


def make_consts():
    c = {}
    c["ident_f"] = np.eye(128, dtype=np.float32)
    c["antiid_f"] = np.ascontiguousarray(np.eye(128, dtype=np.float32)[::-1])
    r = np.arange(128)
    c["tri_incl"] = (r[:, None] <= r[None, :]).astype(np.float32)
    c["tri_strict"] = (r[:, None] < r[None, :]).astype(np.float32)
    blk = np.zeros((128, 128), np.float32)
    blk[:64, :64] = 1
    blk[64:, 64:] = 1
    c["blk64"] = blk
    rot = np.zeros((128, 128), np.float32)
    for i in range(64):
        rot[2 * i, 2 * i + 1] = -1.0
        rot[2 * i + 1, 2 * i] = 1.0
    c["rotT"] = np.ascontiguousarray(rot.T)
    n_tok = SEQ
    t = np.arange(n_tok)
    meta_pos = np.arange(NM) - NM
    row = np.concatenate([meta_pos, t // 64]).astype(np.float32)
    col = np.concatenate([meta_pos, t % 64]).astype(np.float32)
    half = 32
    inv = (np.float32(10000.0) ** (-np.arange(0, half, 2, dtype=np.float32) / np.float32(half))).astype(np.float32)
    ang = np.concatenate([row[:, None] * inv, col[:, None] * inv], axis=-1).astype(np.float32)
    cs = np.cos(ang).astype(np.float32)
    sn = np.sin(ang).astype(np.float32)
    c["ropeC"] = np.ascontiguousarray(np.tile(np.repeat(cs.T, 2, axis=0), (2, 1)))
    c["ropeS"] = np.ascontiguousarray(np.tile(np.repeat(sn.T, 2, axis=0), (2, 1)))
    rel = np.arange(512) - 256
    bk = t5_bucket_np(rel)
    oh = np.zeros((33, 512), np.float32)
    for i in range(512):
        if abs(rel[i]) > 128:
            oh[32, i] = 1.0
        else:
            oh[bk[i], i] = 1.0
    c["t5oh"] = oh
    rm = np.ones((128, 512), np.float32)
    rm[:, ::128] = 0.0
    c["resetm"] = rm
    hm = np.zeros((128, 4), np.float32)
    for h in range(4):
        hm[h * 32:(h + 1) * 32, h] = 1.0
    c["headmask"] = hm
    c["tri_ge"] = (r[:, None] >= r[None, :]).astype(np.float32)
    return c


CONST_SHAPES = {k: v.shape for k, v in make_consts().items()}

PARAM_SHAPES = {
    "meta_tokens": (16, 1024), "rel_bias": (32, 4), "norm_mix_w": (2, 1024), "norm_ffn_w": (2, 1024),
    "w_in": (2, 1024, 2856), "ssd_conv_w": (2, 5, 768), "ssd_conv_b": (2, 768), "ssd_dt_bias": (2, 2, 4),
    "ssd_a_log": (2, 2, 4), "ssd_d": (2, 4), "ssd_norm_w": (2, 256), "gla_gate_w2": (2, 2, 16, 128),
    "gla_gate_b": (2, 2, 128), "gla_norm_w": (2, 64), "swa_sink": (2, 4), "gqa_q_norm_w": (2, 64),
    "gqa_k_norm_w": (2, 64), "w_out": (2, 1024, 1024), "ffn_w_gate": (1, 1024, 2816),
    "ffn_w_up": (1, 1024, 2816), "ffn_w_down": (1, 2816, 1024), "moe_router": (1, 1024, 8),
    "moe_w_gate": (1, 8, 1024, 3584), "moe_w_up": (1, 8, 1024, 3584), "moe_w_down": (1, 8, 3584, 1024),
    "final_norm_w": (1024,),
}


class KB:
    def __init__(self, dbg_out=(), dbg_in=()):
        self.nc = bass.Bass("TRN2", target_bir_lowering=False)
        self.P = Prog(self.nc)
        self.dbg_out = set(dbg_out)
        self.dbg_in = set(dbg_in)
        nc = self.nc
        self.din = {}
        self.din["x"] = nc.dram_tensor("x", [SEQ, D], F32, kind="ExternalInput").ap()
        for k, s in PARAM_SHAPES.items():
            self.din[k] = nc.dram_tensor(k, list(s), F32, kind="ExternalInput").ap()
        for k, s in CONST_SHAPES.items():
            self.din[k] = nc.dram_tensor("c_" + k, list(s), F32, kind="ExternalInput").ap()
        self.out = nc.dram_tensor("out", [SEQ, D], F32, kind="ExternalOutput").ap()
        self.bout = Buf("out")
        self.scr = {}
        self.sb = {}
        self._scratch("projF", [NFM, 128, L], F32, [NFM])
        self._scratch("vtok", [L, TMW], F32, [1])
        self._scratch("mixT", [8, 128, L], BF16, [8])
        self._scratch("hbuf", [L, D], F32, [1])
        self._scratch("t5tab", [4, 512], F32, [1])
        self.const_bufs = {}

    def _scratch(self, name, shape, dt, nb):
        kind = None
        if name in self.dbg_out:
            kind = "ExternalOutput"
        elif name in self.dbg_in:
            kind = "ExternalInput"
        if kind:
            t = self.nc.dram_tensor(name, shape, dt, kind=kind)
        else:
            t = self.nc.dram_tensor(name, shape, dt)
        self.scr[name] = t.ap()
        self.sb[name] = [Buf(name + str(i)) for i in range(nb[0])]

    def nm(self, n):
        self._uid = getattr(self, "_uid", 0) + 1
        return "%s_%d" % (n, self._uid)

    def dma(self, q, out, in_, reads=(), writes=()):
        self.P.op(q, lambda e: e.dma_start(out=out, in_=in_), reads, writes, dma=True)

    def mm(self, out, lhsT, rhs, start, stop, reads=(), writes=()):
        self.P.op("pe", lambda e: e.matmul(out, lhsT=lhsT, rhs=rhs, start=start, stop=stop), reads, writes)

    def tr(self, out, in_, ident, reads=(), writes=()):
        self.P.op("pe", lambda e: e.transpose(out, in_, ident), reads, writes)

    def act(self, out, in_, func, reads=(), writes=(), bias=0.0, scale=1.0, accum_out=None):
        if accum_out is None:
            self.P.op("act", lambda e: e.activation(out=out, in_=in_, func=func, bias=bias, scale=scale), reads, writes)
        else:
            self.P.op("act", lambda e: e.activation(out=out, in_=in_, func=func, bias=bias, scale=scale,
                                                    accum_out=accum_out), reads, writes)

    def tt(self, eng, out, in0, in1, op, reads=(), writes=()):
        self.P.op(eng, lambda e: e.tensor_tensor(out=out, in0=in0, in1=in1, op=op), reads, writes)

    def ts(self, eng, out, in0, s1, op0, s2=None, op1=None, reads=(), writes=()):
        if op1 is None:
            self.P.op(eng, lambda e: e.tensor_scalar(out=out, in0=in0, scalar1=s1, scalar2=None, op0=op0), reads, writes)
        else:
            self.P.op(eng, lambda e: e.tensor_scalar(out=out, in0=in0, scalar1=s1, scalar2=s2, op0=op0, op1=op1),
                      reads, writes)

    def stt(self, eng, out, in0, scalar, in1, op0, op1, reads=(), writes=()):
        self.P.op(eng, lambda e: e.scalar_tensor_tensor(out=out, in0=in0, scalar=scalar, in1=in1, op0=op0, op1=op1),
                  reads, writes)

    def cp(self, eng, out, in_, reads=(), writes=()):
        if eng == "act":
            self.P.op("act", lambda e: e.copy(out=out, in_=in_), reads, writes)
        else:
            self.P.op(eng, lambda e: e.tensor_copy(out=out, in_=in_), reads, writes)

    def memset(self, eng, ap, val, writes=()):
        self.P.op(eng, lambda e: e.memset(ap, val), (), writes)

    def recip(self, out, in_, reads=(), writes=()):
        self.P.op("dve", lambda e: e.reciprocal(out=out, in_=in_), reads, writes)

    def rstd_from_ss(self, dst, src, n_feat, reads, writes):
        self.act(dst, src, AF.Ln, list(reads) + [self.b_eps], writes, bias=self.eps_ap(dst), scale=1.0 / n_feat)
        self.act(dst, dst, AF.Exp, writes, writes, scale=-0.5)

    def eps_ap(self, like):
        np_ = like.shape[0]
        return self.epsT[0:np_, 0:1] if like.base_partition() == 0 else self.epsT[like.base_partition():like.base_partition() + np_, 0:1]

    def load_consts(self, st):
        nc = self.nc
        S = lambda n, s, d=F32: st.enter_context(nc.sbuf_tensor(self.nm(n), s, d))
        self.c = {}
        for k in ("ident_f", "antiid_f", "tri_incl", "tri_strict", "tri_ge", "blk64", "rotT"):
            t = S("sc_" + k, [128, 128])
            b = Buf(k)
            self.dma("sp", t[:], self.din[k][:, :], (), [b])
            b.const = True
            self.c[k] = t
            self.const_bufs[k] = b
        self.identb = S("identb", [128, 128], BF16)
        self.b_identb = Buf("identb")
        self.cp("dve", self.identb[:], self.c["ident_f"][:], [self.const_bufs["ident_f"]], [self.b_identb])
        self.b_identb.const = True
        self.epsT = S("epsT", [128, 1])
        self.b_eps = Buf("eps")
        self.memset("dve", self.epsT[:], EPS, [self.b_eps])
        self.b_eps.const = True
        self.ones_f = S("ones_f", [128, 128])
        self.b_ones = Buf("ones")
        self.memset("dve", self.ones_f[:], 1.0, [self.b_ones])
        self.b_ones.const = True

    def hsrc(self, layer, t0, nt):
        if layer == 0:
            if t0 < NM:
                return self.din["meta_tokens"][t0:t0 + nt, :], None
            return self.din["x"][t0 - NM:t0 - NM + nt, :], None
        return self.scr["hbuf"][t0:t0 + nt, :], self.sb["hbuf"][0]

    def norm_T(self, xt, nt, wrow, ub, uT, col0, pts, bx, bub, bpts, buT, bw, junk, ss, bss, keep_f32=None):
        self.norm_p1(xt, nt, wrow, ub, bx, bub, bw, junk, ss, bss, keep_f32)
        self.norm_p2(nt, ub, uT, col0, pts, bub, bpts, buT)

    def norm_p1(self, xt, nt, wrow, ub, bx, bub, bw, junk, ss, bss, keep_f32=None):
        self.act(junk[:nt, :], xt[:nt, :], AF.Square, [bx], [bss], accum_out=ss[:nt, 0:1])
        self.rstd_from_ss(ss[:nt, 0:1], ss[:nt, 0:1], D, [bss], [bss])
        self.stt("dve", ub[:nt, :], xt[:nt, :], ss[:nt, 0:1], wrow[:nt, :], ALU.mult, ALU.mult, [bx, bss, bw], [bub])
        if keep_f32 is not None:
            kf, bkf = keep_f32
            self.stt("dve", kf[:nt, :], xt[:nt, :], ss[:nt, 0:1], wrow[:nt, :], ALU.mult, ALU.mult, [bx, bss, bw], [bkf])

    def norm_p2(self, nt, ub, uT, col0, pts, bub, bpts, buT):
        for k4 in range(2):
            pt = pts[k4]
            for k in range(4):
                kk = k4 * 4 + k
                self.tr(pt[:, k * 128:k * 128 + nt], ub[:nt, kk * 128:(kk + 1) * 128], self.identb[:nt, :nt],
                        [bub, self.b_identb], [bpts[k4]])
            src = pt[:].rearrange("p (k t) -> p k t", k=4)[:, :, :nt]
            dst = uT[:, k4 * 4:(k4 + 1) * 4, col0:col0 + nt]
            self.cp("act" if k4 == 0 else "dve", dst, src, [bpts[k4]], [buT])

    def phase_A(self, layer):
        nc, P = self.nc, self.P
        P.barrier()
        with contextlib.ExitStack() as st:
            S = lambda n, s, d=F32: st.enter_context(nc.sbuf_tensor(self.nm(n), s, d))
            PS = lambda n, s, d=F32: st.enter_context(nc.psum_tensor(self.nm(n), s, d))
            Wf = S("A_Wf", [128, 8, NFM * 128], BF16)
            Wv = S("A_Wv", [128, 8, TMW], BF16)
            bWf = [Buf("Wf%d" % i) for i in range(NFM)]
            bWv = Buf("Wv")
            self.memset("pool", Wf[:, :, FM_IDX["ga"] * 128:(FM_IDX["ga"] + 1) * 128], 0.0, [bWf[FM_IDX["ga"]]])
            win = self.din["w_in"][layer].rearrange("(k p) c -> p k c", p=128)
            for i, (name, segs) in enumerate(FM_TILES):
                off = i * 128
                for (c0, n) in segs:
                    self.dma("pool", Wf[:, :, off:off + n], win[:, :, c0:c0 + n], (), [bWf[i]])
                    off += n
            off = 0
            for (c0, n) in TM_SEGS:
                self.dma("pool", Wv[:, :, off:off + n], win[:, :, c0:c0 + n], (), [bWv])
                off += n
            wrow = S("A_wrow", [128, D])
            bw = Buf("wrow")
            self.dma("sp", wrow[:], self.din["norm_mix_w"][layer:layer + 1, :].partition_broadcast(128), (), [bw])
            NX = 8
            xts = [S("A_xt%d" % i, [128, D]) for i in range(NX)]
            bxs = [Buf("xt%d" % i) for i in range(NX)]
            junk = S("A_junk", [128, D], BF16)
            ub = [S("A_ub%d" % i, [128, D], BF16) for i in range(NX)]
            bub = [Buf("ub%d" % i) for i in range(NX)]
            ss = [S("A_ss%d" % i, [128, 1]) for i in range(NX)]
            bss = [Buf("ss%d" % i) for i in range(NX)]
            uT = [S("A_uT%d" % i, [128, 8, 512], BF16) for i in range(2)]
            buT = [Buf("uT%d" % i) for i in range(2)]
            pts = [PS("A_pt%d" % i, [128, 512], BF16) for i in range(2)]
            bpts = [Buf("pt%d" % i) for i in range(2)]
            pmm = [PS("A_pm%d" % i, [128, 512]) for i in range(4)]
            bpmm = [Buf("pm%d" % i) for i in range(4)]
            stg = [S("A_stg%d" % i, [128, 512]) for i in range(4)]
            bstg = [Buf("stg%d" % i) for i in range(4)]
            pv8 = PS("A_pv8", [128, 8])
            bpv8 = Buf("pv8")
            stv = [S("A_stv%d" % i, [128, TMW]) for i in range(2)]
            bstv = [Buf("stv%d" % i) for i in range(2)]
            im = 0
            slot = {}
            cnt = [0]

            def pre(gi):
                g0, gn = GROUPS[gi]
                for (t0, nt) in chunks_in(g0, gn):
                    k = cnt[0] % NX
                    cnt[0] += 1
                    slot[(gi, t0)] = k
                    src, sbuf_ = self.hsrc(layer, t0, nt)
                    self.dma("sp", xts[k][:nt, :], src, [sbuf_] if sbuf_ else (), [bxs[k]])
                    self.norm_p1(xts[k], nt, wrow, ub[k], bxs[k], bub[k], bw, junk, ss[k], bss[k])

            def trs(gi):
                g0, gn = GROUPS[gi]
                for (t0, nt) in chunks_in(g0, gn):
                    k = slot[(gi, t0)]
                    self.norm_p2(nt, ub[k], uT[gi % 2], t0 - g0, pts, bub[k], bpts, buT[gi % 2])

            def mms(gi):
                nonlocal im
                g0, gn = GROUPS[gi]
                u = uT[gi % 2]
                bu = buT[gi % 2]
                subs = chunks_in(g0, gn)
                for f in range(NFM):
                    pm, bpm = pmm[im % 4], bpmm[im % 4]
                    sg, bsg = stg[im % 4], bstg[im % 4]
                    for k in range(8):
                        self.mm(pm[:, :gn], Wf[:, k, f * 128:(f + 1) * 128], u[:, k, :gn], k == 0, k == 7, [bWf[f], bu], [bpm])
                    self.cp("act" if im % 2 == 0 else "dve", sg[:, :gn], pm[:, :gn], [bpm], [bsg])
                    self.dma("sp", self.scr["projF"][f, :, g0:g0 + gn], sg[:, :gn], [bsg], [self.sb["projF"][f]])
                    im += 1
                for (t0, nt) in subs:
                    pm, bpm = pmm[im % 4], bpmm[im % 4]
                    sv_, bsv = stv[im % 2], bstv[im % 2]
                    c0 = t0 - g0
                    for k in range(8):
                        self.mm(pm[:nt, :], u[:, k, c0:c0 + nt], Wv[:, k, 0:512], k == 0, k == 7, [bWv, bu], [bpm])
                    for k in range(8):
                        self.mm(pv8[:nt, :], u[:, k, c0:c0 + nt], Wv[:, k, 512:520], k == 0, k == 7, [bWv, bu], [bpv8])
                    self.cp("act", sv_[:nt, 0:512], pm[:nt, :], [bpm], [bsv])
                    self.cp("dve", sv_[:nt, 512:520], pv8[:nt, :], [bpv8], [bsv])
                    self.dma("sp", self.scr["vtok"][t0:t0 + nt, :], sv_[:nt, :], [bsv], [self.sb["vtok"][0]])
                    im += 1

            NGR = len(GROUPS)
            pre(0)
            trs(0)
            for gi in range(NGR):
                if gi + 1 < NGR:
                    pre(gi + 1)
                mms(gi)
                if gi + 1 < NGR:
                    trs(gi + 1)

    def load_vaug(self, S, name, vcol, sink_row=None):
        va = S(name, [128, 33, 2, 65], BF16)
        b = Buf(name)
        self.memset("pool", va[:], 0.0, [b])
        self.memset("pool", va[:, :, :, 64:65], 1.0, [b])
        if sink_row is not None:
            self.memset("pool", va[:, 0, :, 64:65], 0.0, [b])
            self.memset("pool", va[0:NM, 0, :, 64:65], 1.0, [b])
            self.memset("pool", va[sink_row:sink_row + 1, 0, :, 64:65], 1.0, [b])
        vt = self.scr["vtok"]
        for g in range(2):
            src = vt[NM:, vcol + g * 64:vcol + (g + 1) * 64].rearrange("(c p) d -> p c d", p=128)
            self.dma("pool", va[:, 1:33, g, 0:64], src, [self.sb["vtok"][0]], [b])
            self.dma("pool", va[0:NM, 0, g, 0:64], vt[0:NM, vcol + g * 64:vcol + (g + 1) * 64], [self.sb["vtok"][0]], [b])
        return va, b

    def attn_finish(self, ps_o, bps_o, n, rden, brden, ps_b, bps_b, osb, bosb, stg, bstg, dst, bdst):
        self.attn_fin_a(ps_o, bps_o, n, rden, brden, osb, bosb)
        self.attn_fin_b(n, rden, brden, ps_b, bps_b, osb, bosb, stg, bstg, dst, bdst)

    def attn_fin_a(self, ps_o, bps_o, n, rden, brden, osb, bosb):
        self.recip(rden[64:65, :n], ps_o[64:65, :n], [bps_o], [brden])
        self.cp("dve", osb[0:64, :n], ps_o[0:64, :n], [bps_o], [bosb])

    def attn_fin_b(self, n, rden, brden, ps_b, bps_b, osb, bosb, stg, bstg, dst, bdst):
        self.mm(ps_b[0:64, :n], self.ones_f[64:65, 0:64], rden[64:65, :n], True, True, [brden, self.b_ones], [bps_b])
        self.tt("dve", stg[0:64, :n], osb[0:64, :n], ps_b[0:64, :n], ALU.mult, [bosb, bps_b], [bstg])
        self.dma("sp", dst, stg[0:64, :n], [bstg], [bdst])

    def phase_G2(self, layer):
        nc, P = self.nc, self.P
        P.barrier()
        with contextlib.ExitStack() as st:
            S = lambda n, s, d=F32: st.enter_context(nc.sbuf_tensor(self.nm(n), s, d))
            PS = lambda n, s, d=F32: st.enter_context(nc.psum_tensor(self.nm(n), s, d))
            qr = [S("G_qr%d" % g, [128, L], BF16) for g in range(2)]
            kr = [S("G_kr%d" % g, [128, L], BF16) for g in range(2)]
            bqk = {}
            va, bva = self.load_vaug(S, "G_va", V_AV)
            wn = S("G_wn", [128, 2])
            bwn = Buf("wn")
            for c, nm in enumerate(("gqa_q_norm_w", "gqa_k_norm_w")):
                src = self.din[nm][layer:layer + 1, :].rearrange("o d -> d o")
                self.dma("sp", wn[0:64, c:c + 1], src, (), [bwn])
                self.dma("sp", wn[64:128, c:c + 1], src, (), [bwn])
            Ct = [S("G_C%d" % i, [128, 512]) for i in range(2)]
            St = [S("G_S%d" % i, [128, 512]) for i in range(2)]
            bCS = [Buf("CS%d" % i) for i in range(2)]
            xs = [S("G_x%d" % i, [128, 512]) for i in range(2)]
            bxs = [Buf("x%d" % i) for i in range(2)]
            mk2 = lambda nme: ([S("G_%s%d" % (nme, i), [128, 512]) for i in range(3)], [Buf("%s%d" % (nme, i)) for i in range(3)])
            sq2, bsq2 = mk2("sq"); rs2, brs2 = mk2("rs"); xn2, bxn2 = mk2("xn"); t12, bt12 = mk2("t1"); t22, bt22 = mk2("t2")
            NB = 6
            SK = 2
            pss2 = [PS("G_ps%d" % i, [128, 1024]) for i in range(3)]
            bpss2 = [Buf("ps%d" % i) for i in range(3)]
            pss = [pss2[i // 2][:, (i % 2) * 512:(i % 2 + 1) * 512] for i in range(NB)]
            bpss = [bpss2[i // 2] for i in range(NB)]
            pso = [PS("G_po%d" % i, [128, 512]) for i in range(2)]
            bpso = [Buf("po%d" % i) for i in range(2)]
            pn = [pss[3], pss[4]]
            bpn = [bpss[3], bpss[4]]
            xs3, bxs3 = mk2("xx")
            tiles = [("aq0", qr[0], 0), ("aq1", qr[1], 0), ("ak0", kr[0], 1), ("ak1", kr[1], 1)]
            work = []
            for gi, (g0, gn) in enumerate(GROUPS):
                for (nm, dst, wc) in tiles:
                    work.append((gi, g0, gn, nm, dst, wc))

            def st_a(it):
                gi, g0, gn, nm, dst, wc = work[it]
                i3 = it % 3
                if nm == "aq0":
                    C_, S_, bcs = Ct[gi % 2], St[gi % 2], bCS[gi % 2]
                    self.dma("sp", C_[:, :gn], self.din["ropeC"][:, g0:g0 + gn], (), [bcs])
                    self.dma("sp", S_[:, :gn], self.din["ropeS"][:, g0:g0 + gn], (), [bcs])
                f = FM_IDX[nm]
                x, bx = xs3[i3], bxs3[i3]
                self.dma("sp", x[:, :gn], self.scr["projF"][f, :, g0:g0 + gn], [self.sb["projF"][f]], [bx])
                self.act(sq2[i3][:, :gn], x[:, :gn], AF.Square, [bx], [bsq2[i3]])
                p1, bp1 = pn[it % 2], bpn[it % 2]
                self.mm(p1[:, :gn], self.c["blk64"][:], sq2[i3][:, :gn], True, True, [bsq2[i3], self.const_bufs["blk64"]], [bp1])
                self.rstd_from_ss(rs2[i3][:, :gn], p1[:, :gn], 64, [bp1], [brs2[i3]])
                self.stt("dve", xn2[i3][:, :gn], x[:, :gn], wn[:, wc:wc + 1], rs2[i3][:, :gn], ALU.mult, ALU.mult,
                         [bx, bwn, brs2[i3]], [bxn2[i3]])

            def st_b(it):
                gi, g0, gn, nm, dst, wc = work[it]
                i3 = it % 3
                self.mm(pss[i3][:, :gn], self.c["rotT"][:], xn2[i3][:, :gn], True, True, [bxn2[i3], self.const_bufs["rotT"]], [bpss[i3]])

            def st_c(it):
                gi, g0, gn, nm, dst, wc = work[it]
                i3 = it % 3
                C_, S_, bcs = Ct[gi % 2], St[gi % 2], bCS[gi % 2]
                bd = bqk.setdefault((nm, gi), Buf(nm + str(gi)))
                self.tt("dve", t12[i3][:, :gn], xn2[i3][:, :gn], C_[:, :gn], ALU.mult, [bxn2[i3], bcs], [bt12[i3]])
                self.tt("dve", t22[i3][:, :gn], pss[i3][:, :gn], S_[:, :gn], ALU.mult, [bpss[i3], bcs], [bt22[i3]])
                self.tt("pool", dst[:, g0:g0 + gn], t12[i3][:, :gn], t22[i3][:, :gn], ALU.add, [bt12[i3], bt22[i3]], [bd])

            NW = len(work)
            for it in range(NW + 2):
                if it < NW:
                    st_a(it)
                if 1 <= it <= NW:
                    st_b(it - 1)
                if it >= 2:
                    st_c(it - 2)
            pT2 = [S("G_pT%d" % i, [128, 1024], BF16) for i in range(3)]
            bpT2 = [Buf("pT%d" % i) for i in range(3)]
            rden = [S("G_rden%d" % i, [128, 512]) for i in range(2)]; brden = [Buf("rden%d" % i) for i in range(2)]
            osb = [S("G_osb%d" % i, [128, 512]) for i in range(2)]; bosb = [Buf("osb%d" % i) for i in range(2)]
            stg = [S("G_stg%d" % i, [128, 512], BF16) for i in range(2)]
            bstg = [Buf("stg%d" % i) for i in range(2)]
            kbufs = {g: [bqk[("ak%d" % g, gi)] for gi in range(len(GROUPS))] for g in range(2)}
            items = [(gi, g, kt) for gi in range(len(GROUPS)) for g in range(2) for kt in range(len(CHUNKS))]
            pend_b = []
            NI = 3

            def stage_qk(i3, it):
                gi, g, kt = it
                g0, gn = GROUPS[gi]
                k0, kn = CHUNKS[kt]
                kb = kbufs[g][0] if kt == 0 else kbufs[g][1 + (kt - 1) // 4]
                j = i3 % NI
                for r in range(2):
                    hp = r * 64
                    self.mm(pss2[j][:kn, r * 512:r * 512 + gn], kr[g][hp:hp + 64, k0:k0 + kn], qr[g][hp:hp + 64, g0:g0 + gn],
                            True, True, [kb, bqk[("aq%d" % g, gi)]], [bpss2[j]])
                v3 = lambda t: t[:kn, :].rearrange("p (r q) -> p r q", r=2)[:, :, :gn]
                self.act(v3(pT2[j]), v3(pss2[j]), AF.Exp, [bpss2[j]], [bpT2[j]], scale=0.125)

            def stage_pv(i3, it):
                gi, g, kt = it
                g0, gn = GROUPS[gi]
                k0, kn = CHUNKS[kt]
                j = i3 % NI
                for r in range(2):
                    self.mm(pso[r][0:65, :gn], va[:kn, kt, g, :], pT2[j][:kn, r * 512:r * 512 + gn], kt == 0, kt == 32,
                            [bva, bpT2[j]], [bpso[r]])
                if kt == 32:
                    for r in range(2):
                        hp = r * 64
                        self.attn_fin_a(pso[r], bpso[r], gn, rden[r], brden[r], osb[r], bosb[r])
                        dst = self.scr["mixT"][6 + g, hp:hp + 64, g0:g0 + gn]
                        pend_b.append((i3 + 1, (gn, rden[r], brden[r], pso[r], bpso[r], osb[r], bosb[r],
                                                stg[r], bstg[r], dst, self.sb["mixT"][6 + g])))

            N = len(items)
            for i3 in range(N + SK):
                if i3 < N:
                    stage_qk(i3, items[i3])
                if i3 >= SK:
                    stage_pv(i3 - SK, items[i3 - SK])
                while pend_b and pend_b[0][0] <= i3:
                    self.attn_fin_b(*pend_b.pop(0)[1])
            while pend_b:
                self.attn_fin_b(*pend_b.pop(0)[1])

    def setup_swa_bias(self, st):
        nc, P = self.nc, self.P
        S = lambda n, s, d=F32: st.enter_context(nc.sbuf_tensor(self.nm(n), s, d))
        self.BT1 = [S("BT1_%d" % h, [128, 512]) for h in range(4)]
        self.BT0 = [S("BT0_%d" % h, [128, 512]) for h in range(4)]
        self.BTq = [S("BTq_%d" % h, [128, 32]) for h in range(4)]
        self.bBT = [Buf("BT%d" % h) for h in range(4)]
        self.zrow = S("zrow", [128, 128])
        self.b_zrow = Buf("zrow")
        self.memset("pool", self.zrow[:], 0.0, [self.b_zrow])
        self.b_zrow.const = True
        with contextlib.ExitStack() as st2:
            S2 = lambda n, s, d=F32: st2.enter_context(nc.sbuf_tensor(self.nm(n), s, d))
            PS2 = lambda n, s, d=F32: st2.enter_context(nc.psum_tensor(self.nm(n), s, d))
            relb = S2("sw_relb", [33, 4]); brelb = Buf("relb")
            oh = S2("sw_oh", [33, 512]); boh = Buf("oh")
            tab = S2("sw_tab", [4, 512]); btab = Buf("tab")
            t15 = S2("sw_t15", [16, 4]); bt15 = Buf("t15")
            self.memset("dve", relb[32:33, :], NEG, [brelb])
            self.dma("sp", relb[0:32, :], self.din["rel_bias"][:, :], (), [brelb])
            self.dma("sp", oh[:], self.din["t5oh"][:, :], (), [boh])
            self.dma("sp", t15[:], self.din["rel_bias"][15:16, :].partition_broadcast(16), (), [bt15])
            pt = PS2("sw_pt", [128, 512]); bpt = Buf("pt")
            self.mm(pt[0:4, :], relb[:, :], oh[:, :], True, True, [brelb, boh], [bpt])
            self.cp("act", tab[:], pt[0:4, :], [bpt], [btab])
            btd = self.sb["t5tab"][0]
            self.dma("sp", self.scr["t5tab"][:, :], tab[:], [btab], [btd])
            hk = [S2("sw_hk%d" % i, [128, 128]) for i in range(2)]
            bhk = [Buf("hk%d" % i) for i in range(2)]
            pm = [PS2("sw_pm%d" % i, [128, 128]) for i in range(2)]
            bpm = [Buf("pm%d" % i) for i in range(2)]
            n = 0
            t5t = self.scr["t5tab"].tensor
            for h in range(4):
                b = self.bBT[h]
                self.memset("pool", self.BT1[h][:, 384:512], NEG, [b])
                self.memset("pool", self.BT0[h][:, 384:512], NEG, [b])
                self.memset("pool", self.BTq[h][:, 0:16], NEG, [b])
                self.ts("dve", self.BT1[h][0:16, 384:512], self.zrow[0:16, :], t15[0:16, h:h + 1], ALU.add,
                        reads=[bt15, self.b_zrow], writes=[b])
                for c in (-128, 0, 128, -16, 16):
                    i = n % 2
                    src = bass.AP(t5t, h * 512 + c + 129, [[1, 128], [1, 128]])
                    self.dma("sp", hk[i][:], src, [btd], [bhk[i]])
                    self.mm(pm[i][:], hk[i][:], self.c["antiid_f"][:], True, True, [bhk[i], self.const_bufs["antiid_f"]], [bpm[i]])
                    if c in (-128, 0, 128):
                        e = c // 128 + 1
                        self.cp("act", self.BT1[h][:, e * 128:(e + 1) * 128], pm[i][:], [bpm[i]], [b])
                        self.cp("dve", self.BT0[h][:, e * 128:(e + 1) * 128], pm[i][:], [bpm[i]], [b])
                        if c == 0:
                            self.cp("dve", self.BTq[h][0:16, 0:16], pm[i][0:16, 0:16], [bpm[i]], [b])
                    elif c == -16:
                        self.cp("dve", self.BT0[h][0:16, 384:512], pm[i][0:16, :], [bpm[i]], [b])
                    else:
                        self.cp("dve", self.BTq[h][:, 16:32], pm[i][:, 0:16], [bpm[i]], [b])
                    n += 1
            P.barrier()

    def phase_SWA(self, layer):
        nc, P = self.nc, self.P
        P.barrier()
        SINK = 32
        with contextlib.ExitStack() as st:
            S = lambda n, s, d=F32: st.enter_context(nc.sbuf_tensor(self.nm(n), s, d))
            PS = lambda n, s, d=F32: st.enter_context(nc.psum_tensor(self.nm(n), s, d))
            self.setup_swa_bias(st)
            qT = [S("W_q%d" % g, [128, L], BF16) for g in range(2)]
            kT = [S("W_k%d" % g, [128, L], BF16) for g in range(2)]
            bq = [Buf("q%d" % g) for g in range(2)]
            bk = [Buf("k%d" % g) for g in range(2)]
            km = [S("W_km%d" % g, [128, 128], BF16) for g in range(2)]
            bkm = [Buf("km%d" % g) for g in range(2)]
            zk = S("W_zk", [128, 128], BF16); bzk = Buf("zk")
            self.memset("pool", zk[:], 0.0, [bzk])
            for g in range(2):
                fq, fk = FM_IDX["sq%d" % g], FM_IDX["sk%d" % g]
                self.dma("pool", qT[g][:], self.scr["projF"][fq, :, :], [self.sb["projF"][fq]], [bq[g]])
                self.dma("pool", kT[g][:], self.scr["projF"][fk, :, :], [self.sb["projF"][fk]], [bk[g]])
                self.memset("pool", km[g][:], 0.0, [bkm[g]])
                self.cp("pool", km[g][:, 0:NM], kT[g][:, 0:NM], [bk[g]], [bkm[g]])
            va, bva = self.load_vaug(S, "W_va", V_SV, sink_row=SINK)
            sinkt = S("W_sink", [128, 4]); bsink = Buf("sink")
            self.dma("sp", sinkt[SINK:SINK + 1, :], self.din["swa_sink"][layer:layer + 1, :], (), [bsink])
            for h in range(4):
                b = self.bBT[h]
                for (t, c0, n) in ((self.BT1[h], 384, 128), (self.BT0[h], 384, 128), (self.BTq[h], 0, 16)):
                    self.ts("dve", t[SINK:SINK + 1, c0:c0 + n], self.zrow[SINK:SINK + 1, 0:n],
                            sinkt[SINK:SINK + 1, h:h + 1], ALU.add, reads=[bsink, self.b_zrow], writes=[b])
            ND = 3
            pss = [PS("W_ps%d" % i, [128, 512]) for i in range(ND)]
            bpss = [Buf("ps%d" % i) for i in range(ND)]
            pso = [PS("W_po%d" % i, [128, 512]) for i in range(ND)]
            bpso = [Buf("po%d" % i) for i in range(ND)]
            psb = PS("W_pb", [128, 512]); bpsb = Buf("pb")
            sbs = [S("W_sb%d" % i, [128, 512]) for i in range(ND)]
            bsbs = [Buf("sb%d" % i) for i in range(ND)]
            pT = [S("W_pT%d" % i, [128, 512], BF16) for i in range(ND)]
            bpT = [Buf("pT%d" % i) for i in range(ND)]
            rden = [S("W_rden%d" % i, [128, 128]) for i in range(ND)]; brden = [Buf("rden%d" % i) for i in range(ND)]
            osb = [S("W_osb%d" % i, [128, 128]) for i in range(ND)]; bosb = [Buf("osb%d" % i) for i in range(ND)]
            stg = [S("W_stg%d" % i, [128, 128], BF16) for i in range(ND)]
            bstg = [Buf("stg%d" % i) for i in range(ND)]
            items = [(qb, g, r) for qb in range(-1, 32) for g in range(2) for r in range(2)]

            def stage_qk(it, item):
                qb, g, r = item
                h = 2 * g + r
                hp = r * 64
                i2 = it % ND
                ps_, bps_ = pss[i2], bpss[i2]
                sb_, bsb_ = sbs[i2], bsbs[i2]
                p_, bp_ = pT[i2], bpT[i2]
                if qb < 0:
                    qa = qT[g][hp:hp + 64, 0:NM]
                    self.mm(ps_[:, 0:16], km[g][hp:hp + 64, :], qa, True, True, [bkm[g], bq[g]], [bps_])
                    self.mm(ps_[:, 16:32], kT[g][hp:hp + 64, NM:NM + 128], qa, True, True, [bk[g], bq[g]], [bps_])
                    self.stt("dve", sb_[:, 0:32], ps_[:, 0:32], 0.125, self.BTq[h][:, :], ALU.mult, ALU.add,
                             [bps_, self.bBT[h]], [bsb_])
                    self.act(p_[:, 0:32], sb_[:, 0:32], AF.Exp, [bsb_], [bp_])
                else:
                    q0 = NM + 128 * qb
                    qa = qT[g][hp:hp + 64, q0:q0 + 128]
                    for e in (-1, 0, 1):
                        kb = qb + e
                        if 0 <= kb < 32:
                            self.mm(ps_[:, (e + 1) * 128:(e + 2) * 128], kT[g][hp:hp + 64, NM + 128 * kb:NM + 128 * kb + 128],
                                    qa, True, True, [bk[g], bq[g]], [bps_])
                        else:
                            self.mm(ps_[:, (e + 1) * 128:(e + 2) * 128], zk[hp:hp + 64, :], qa, True, True,
                                    [bzk, bq[g]], [bps_])
                    self.mm(ps_[:, 384:512], km[g][hp:hp + 64, :], qa, True, True, [bkm[g], bq[g]], [bps_])
                    BT = self.BT0[h] if qb == 0 else self.BT1[h]
                    self.stt("dve", sb_[:, :], ps_[:, :], 0.125, BT[:, :], ALU.mult, ALU.add,
                             [bps_, self.bBT[h]], [bsb_])
                    self.act(p_[:, :], sb_[:, :], AF.Exp, [bsb_], [bp_])

            def stage_pv(it, item):
                qb, g, r = item
                hp = r * 64
                i2 = it % ND
                p_, bp_ = pT[i2], bpT[i2]
                po, bpo = pso[i2], bpso[i2]
                if qb < 0:
                    self.mm(po[0:65, 0:NM], va[:, 0, g, :], p_[:, 0:16], True, False, [bva, bp_], [bpo])
                    self.mm(po[0:65, 0:NM], va[:, 1, g, :], p_[:, 16:32], False, True, [bva, bp_], [bpo])
                    nq, q0 = NM, 0
                else:
                    q0 = NM + 128 * qb
                    nq = 128
                    self.mm(po[0:65, 0:128], va[:, 0, g, :], p_[:, 384:512], True, False, [bva, bp_], [bpo])
                    valid = [e for e in (-1, 0, 1) if 0 <= qb + e < 32]
                    for j, e in enumerate(valid):
                        self.mm(po[0:65, 0:128], va[:, qb + e + 1, g, :], p_[:, (e + 1) * 128:(e + 2) * 128],
                                False, j == len(valid) - 1, [bva, bp_], [bpo])
                self.attn_fin_a(po, bpo, nq, rden[i2], brden[i2], osb[i2], bosb[i2])
                dst = self.scr["mixT"][4 + g, hp:hp + 64, q0:q0 + nq]
                return (nq, rden[i2], brden[i2], psb, bpsb, osb[i2], bosb[i2], stg[i2], bstg[i2], dst, self.sb["mixT"][4 + g])

            N = len(items)
            pend = {}
            for it in range(N + 2):
                if it < N:
                    stage_qk(it, items[it])
                if 1 <= it <= N:
                    pend[it - 1] = stage_pv(it - 1, items[it - 1])
                if it >= 2:
                    self.attn_fin_b(*pend.pop(it - 2))

    def phase_C(self, layer):
        nc, P = self.nc, self.P
        is_moe = (layer % 2 == 1)
        last = (layer == DEPTH - 1)
        for (h0, hn) in HALVES:
            P.barrier()
            subs = chunks_in(h0, hn)
            nsub = len(subs)
            with contextlib.ExitStack() as st:
                S = lambda n, s, d=F32: st.enter_context(nc.sbuf_tensor(self.nm(n), s, d))
                acc = S("C_acc", [128, 17, D])
                bacc = [Buf("acc%d" % i) for i in range(17)]
                u2T = S("C_u2T", [128, 8, NM + 2048], BF16)
                bu2 = [Buf("u2T%d" % i) for i in range(17)]
                wrow = S("C_wrow", [128, D]); bw = Buf("wrow")
                self.dma("sp", wrow[:], self.din["norm_ffn_w"][layer:layer + 1, :].partition_broadcast(128), (), [bw])
                lg = S("C_lg", [128, 17, 8]); blg = Buf("lg")
                comb = S("C_comb", [128, 17, 8]); bcomb = Buf("comb")
                with contextlib.ExitStack() as st1:
                    S1 = lambda n, s, d=F32: st1.enter_context(nc.sbuf_tensor(self.nm(n), s, d))
                    PS1 = lambda n, s, d=F32: st1.enter_context(nc.psum_tensor(self.nm(n), s, d))
                    wout = S1("C_wout", [128, 8, D], BF16); bwout = Buf("wout")
                    self.dma("pool", wout[:], self.din["w_out"][layer].rearrange("(c p) d -> p c d", p=128), (), [bwout])
                    mx = [S1("C_mx%d" % i, [128, 8, 128], BF16) for i in range(2)]
                    bmx = [Buf("mx%d" % i) for i in range(2)]
                    junk = S1("C_junk", [128, D], BF16)
                    ub = [S1("C_ub%d" % i, [128, D], BF16) for i in range(2)]
                    bub = [Buf("ub%d" % i) for i in range(2)]
                    ss = [S1("C_ss%d" % i, [128, 1]) for i in range(2)]
                    bss = [Buf("ss%d" % i) for i in range(2)]
                    if is_moe:
                        pd = [PS1("C_pd0", [128, D])] * 2
                        bpd = [Buf("pd0")] * 2
                    else:
                        pd = [PS1("C_pd%d" % i, [128, D]) for i in range(2)]
                        bpd = [Buf("pd%d" % i) for i in range(2)]
                    pts = [PS1("C_pt%d" % i, [128, 512], BF16) for i in range(2)]
                    bpts = [Buf("pt%d" % i) for i in range(2)]
                    if is_moe:
                        ufs = [S1("C_uf%d" % i, [128, D]) for i in range(2)]; bufs_ = [Buf("uf%d" % i) for i in range(2)]
                        ufT = S1("C_ufT", [128, 8, 128]); bufT = Buf("ufT")
                        rt = S1("C_rt", [128, 8, NEXP]); brt = Buf("rt")
                        self.dma("sp", rt[:], self.din["moe_router"][0].rearrange("(k p) e -> p k e", p=128), (), [brt])
                        ptf = PS1("C_ptf", [128, D]); bptf = Buf("ptf")
                        pl = PS1("C_pl", [128, NEXP]); bpl = Buf("pl")
                        self.memset("pool", lg[:], 0.0, [blg])
                    def p1_stage(si):
                        t0, nt = subs[si]
                        a = acc[:, si, :]
                        ba = bacc[si]
                        src, sbuf_ = self.hsrc(layer, t0, nt)
                        self.dma("sp", a[:nt, :], src, [sbuf_] if sbuf_ else (), [ba])
                        m_, bm_ = mx[si % 2], bmx[si % 2]
                        self.dma("sp", m_[:, :, :nt], self.scr["mixT"][:, :, t0:t0 + nt].rearrange("c p t -> p c t"),
                                 self.sb["mixT"], [bm_])
                        p_, bp_ = pd[si % 2], bpd[si % 2]
                        for dh in range(2):
                            for c in range(8):
                                self.mm(p_[:nt, dh * 512:(dh + 1) * 512], m_[:, c, :nt], wout[:, c, dh * 512:(dh + 1) * 512],
                                        c == 0, c == 7, [bm_, bwout], [bp_])
                        self.tt("dve", a[:nt, :], a[:nt, :], p_[:nt, :], ALU.add, [ba, bp_], [ba])
                        kf = (ufs[si % 2], bufs_[si % 2]) if is_moe else None
                        self.norm_p1(a, nt, wrow, ub[si % 2], ba, bub[si % 2], bw, junk, ss[si % 2], bss[si % 2], keep_f32=kf)

                    def p2_stage(si):
                        t0, nt = subs[si]
                        self.norm_p2(nt, ub[si % 2], u2T, t0 - h0, pts, bub[si % 2], bpts, bu2[si])
                        if is_moe:
                            uf, buf_ = ufs[si % 2], bufs_[si % 2]
                            for k in range(8):
                                self.tr(ptf[:, k * 128:k * 128 + nt], uf[:nt, k * 128:(k + 1) * 128], self.c["ident_f"][:nt, :nt],
                                        [buf_, self.const_bufs["ident_f"]], [bptf])
                            self.cp("act", ufT[:, :, :nt], ptf[:].rearrange("p (k t) -> p k t", k=8)[:, :, :nt], [bptf], [bufT])
                            for k in range(8):
                                self.mm(pl[:nt, :], ufT[:, k, :nt], rt[:, k, :], k == 0, k == 7, [bufT, brt], [bpl])
                            self.cp("dve", lg[:nt, si, :], pl[:nt, :], [bpl], [blg])

                    p1_stage(0)
                    for si in range(nsub):
                        if si + 1 < nsub:
                            p1_stage(si + 1)
                        p2_stage(si)
                    if is_moe:
                        m1 = S1("C_m1", [128, 17]); bm1 = Buf("m1")
                        m2 = S1("C_m2", [128, 17]); bm2 = Buf("m2")
                        t3 = S1("C_t3", [128, 17, 8]); bt3 = Buf("t3")
                        t4 = S1("C_t4", [128, 17, 8]); bt4 = Buf("t4")
                        bc = lambda t: t[:].unsqueeze(2).to_broadcast([128, 17, 8])
                        P.op("dve", lambda e: e.tensor_reduce(out=m1[:], in_=lg[:], axis=AX.X, op=ALU.max), [blg], [bm1])
                        self.tt("dve", t3[:], lg[:], bc(m1), ALU.is_equal, [blg, bm1], [bt3])
                        self.stt("dve", t3[:], t3[:], -1e30, lg[:], ALU.mult, ALU.add, [bt3, blg], [bt3])
                        P.op("dve", lambda e: e.tensor_reduce(out=m2[:], in_=t3[:], axis=AX.X, op=ALU.max), [bt3], [bm2])
                        self.tt("dve", t3[:], lg[:], bc(m2), ALU.is_ge, [blg, bm2], [bt3])
                        self.tt("dve", t4[:], lg[:], bc(m1), ALU.subtract, [blg, bm1], [bt4])
                        self.act(t4[:], t4[:], AF.Exp, [bt4], [bt4])
                        self.tt("dve", t4[:], t4[:], t3[:], ALU.mult, [bt4, bt3], [bt4])
                        P.op("dve", lambda e: e.tensor_reduce(out=m1[:], in_=t4[:], axis=AX.X, op=ALU.add), [bt4], [bm1])
                        self.recip(m1[:], m1[:], [bm1], [bm1])
                        self.tt("dve", comb[:], t4[:], bc(m1), ALU.mult, [bt4, bm1], [bcomb])
                P.barrier()
                with contextlib.ExitStack() as st2:
                    S2 = lambda n, s, d=F32: st2.enter_context(nc.sbuf_tensor(self.nm(n), s, d))
                    PS2 = lambda n, s, d=F32: st2.enter_context(nc.psum_tensor(self.nm(n), s, d))
                    G = 4
                    Wg = [S2("F_wg%d" % i, [128, 8, G * 128], BF16) for i in range(2)]
                    Wu = [S2("F_wu%d" % i, [128, 8, G * 128], BF16) for i in range(2)]
                    Wd = [S2("F_wd%d" % i, [128, G, D], BF16) for i in range(2)]
                    bW = [Buf("W%d" % i) for i in range(2)]
                    aT = [S2("F_aT%d" % i, [128, G, 512], BF16) for i in range(2)]
                    baT = [Buf("aT%d" % i) for i in range(2)]
                    sg = [S2("F_sg%d" % i, [128, 512]) for i in range(2)]
                    bsg = [Buf("sg%d" % i) for i in range(2)]
                    pgu = [PS2("F_pgu%d" % i, [128, 512]) for i in range(4)]
                    bpgu = [Buf("pgu%d" % i) for i in range(4)]
                    py = [PS2("F_py%d" % i, [128, D]) for i in range(2)]
                    bpy = [Buf("py%d" % i) for i in range(2)]
                    if is_moe:
                        wsets = [(self.din["moe_w_gate"][0, e], self.din["moe_w_up"][0, e], self.din["moe_w_down"][0, e], MOE_F, e)
                                 for e in range(NEXP)]
                    else:
                        j = layer // 2
                        wsets = [(self.din["ffn_w_gate"][j], self.din["ffn_w_up"][j], self.din["ffn_w_down"][j], FFN_F, None)]
                    tgroups = [(g0, gn) for (g0, gn) in GROUPS if g0 >= h0 and g0 + gn <= h0 + hn]
                    nw = 0
                    ia = 0
                    ig = 0
                    iy = 0
                    for (wg_ap, wu_ap, wd_ap, F, e) in wsets:
                        wgr = wg_ap.rearrange("(k p) f -> p k f", p=128)
                        wur = wu_ap.rearrange("(k p) f -> p k f", p=128)
                        for f0 in range(0, F, G * 128):
                            fw = min(G * 128, F - f0)
                            nj = fw // 128
                            s_ = nw % 2
                            self.dma("pool", Wg[s_][:, :, :fw], wgr[:, :, f0:f0 + fw], (), [bW[s_]])
                            self.dma("pool", Wu[s_][:, :, :fw], wur[:, :, f0:f0 + fw], (), [bW[s_]])
                            self.dma("pool", Wd[s_][:, :nj, :], wd_ap[f0:f0 + fw, :].rearrange("(j p) d -> p j d", p=128), (), [bW[s_]])
                            nw += 1
                            def gate_up(g0, gn, a_, ba_):
                                nonlocal ig
                                c0 = g0 - h0
                                rb = [bu2[subs.index(s)] for s in chunks_in(g0, gn)]
                                for j in range(nj):
                                    pg, bpg = pgu[ig % 4], bpgu[ig % 4]
                                    pu, bpu = pgu[(ig + 1) % 4], bpgu[(ig + 1) % 4]
                                    s2, bs2 = sg[(ig // 2) % 2], bsg[(ig // 2) % 2]
                                    ig += 2
                                    for k in range(8):
                                        self.mm(pg[:, :gn], Wg[s_][:, k, j * 128:(j + 1) * 128], u2T[:, k, c0:c0 + gn], k == 0, k == 7,
                                                [bW[s_]] + rb, [bpg])
                                    for k in range(8):
                                        self.mm(pu[:, :gn], Wu[s_][:, k, j * 128:(j + 1) * 128], u2T[:, k, c0:c0 + gn], k == 0, k == 7,
                                                [bW[s_]] + rb, [bpu])
                                    self.act(s2[:, :gn], pg[:, :gn], AF.Silu, [bpg], [bs2])
                                    self.tt("dve", a_[:, j, :gn], s2[:, :gn], pu[:, :gn], ALU.mult, [bs2, bpu], [ba_])

                            def down(g0, gn, a_, ba_):
                                nonlocal iy
                                for (t0, nt) in chunks_in(g0, gn):
                                    si = subs.index((t0, nt))
                                    cs = t0 - g0
                                    p_, bp_ = py[iy % 2], bpy[iy % 2]
                                    iy += 1
                                    for j in range(nj):
                                        for dh in range(2):
                                            self.mm(p_[:nt, dh * 512:(dh + 1) * 512], a_[:, j, cs:cs + nt],
                                                    Wd[s_][:, j, dh * 512:(dh + 1) * 512], j == 0, j == nj - 1, [ba_, bW[s_]], [bp_])
                                    sc = 1.0 if e is None else comb[:nt, si, e:e + 1]
                                    rd = [bp_, bacc[si]] + ([] if e is None else [bcomb])
                                    self.stt("dve", acc[:nt, si, :], p_[:nt, :], sc, acc[:nt, si, :], ALU.mult, ALU.add, rd, [bacc[si]])

                            prev = None
                            for (g0, gn) in tgroups:
                                a_, ba_ = aT[ia % 2], baT[ia % 2]
                                ia += 1
                                gate_up(g0, gn, a_, ba_)
                                if prev is not None:
                                    down(*prev)
                                prev = (g0, gn, a_, ba_)
                            down(*prev)
                    if not last:
                        for si, (t0, nt) in enumerate(subs):
                            self.dma("sp", self.scr["hbuf"][t0:t0 + nt, :], acc[:nt, si, :], [bacc[si]], [self.sb["hbuf"][0]])
                    else:
                        wf = S2("F_wf", [128, D]); bwf = Buf("wf")
                        self.dma("sp", wf[:], self.din["final_norm_w"].rearrange("(o d) -> o d", o=1).partition_broadcast(128), (), [bwf])
                        junk2 = S2("F_junk", [128, D], BF16)
                        ss2 = [S2("F_ss%d" % i, [128, 1]) for i in range(2)]
                        bss2 = [Buf("ss%d" % i) for i in range(2)]
                        ob = [S2("F_ob%d" % i, [128, D]) for i in range(2)]
                        bob = [Buf("ob%d" % i) for i in range(2)]
                        for si, (t0, nt) in enumerate(subs):
                            if t0 < NM:
                                continue
                            i2 = si % 2
                            self.act(junk2[:nt, :], acc[:nt, si, :], AF.Square, [bacc[si]], [bss2[i2]], accum_out=ss2[i2][:nt, 0:1])
                            self.rstd_from_ss(ss2[i2][:nt, 0:1], ss2[i2][:nt, 0:1], D, [bss2[i2]], [bss2[i2]])
                            self.stt("dve", ob[i2][:nt, :], acc[:nt, si, :], ss2[i2][:nt, 0:1], wf[:nt, :], ALU.mult, ALU.mult,
                                     [bacc[si], bss2[i2], bwf], [bob[i2]])
                            self.dma("sp", self.out[t0 - NM:t0 - NM + nt, :], ob[i2][:nt, :], [bob[i2]], [self.bout])

    def phase_GLA(self, layer):
        nc, P = self.nc, self.P
        P.barrier()
        NC = len(CHUNKS)
        with contextlib.ExitStack() as st:
            S = lambda n, s, d=F32: st.enter_context(nc.sbuf_tensor(self.nm(n), s, d))
            PS = lambda n, s, d=F32: st.enter_context(nc.psum_tensor(self.nm(n), s, d))
            Qf = S("L_Qf", [128, L], BF16); Kf = S("L_Kf", [128, L], BF16)
            Qb = S("L_Qb", [128, L], BF16); Kb = S("L_Kb", [128, L], BF16)
            Qbar = S("L_Qbar", [128, L], BF16)
            bfm = [Buf("fm%d" % i) for i in range(len(GROUPS))]
            Ktf = S("L_Ktf", [128, NC, 128], BF16); Ktb = S("L_Ktb", [128, NC, 128], BF16)
            bKt = [Buf("Kt%d" % i) for i in range(NC)]
            decf = S("L_decf", [128, NC]); decb = S("L_decb", [128, NC])
            bdec = [Buf("dec%d" % i) for i in range(len(GROUPS))]
            vt = S("L_vt", [128, NC, 256], BF16); bvt = Buf("vt")
            src = self.scr["vtok"][NM:, V_GV:V_GV + 256].rearrange("(c p) d -> p c d", p=128)
            self.dma("pool", vt[:, 1:NC, :], src, [self.sb["vtok"][0]], [bvt])
            self.dma("pool", vt[0:NM, 0, :], self.scr["vtok"][0:NM, V_GV:V_GV + 256], [self.sb["vtok"][0]], [bvt])
            w2 = S("L_w2", [32, 2, 128]); bw2 = Buf("w2")
            self.memset("pool", w2[:], 0.0, [bw2])
            for d in range(2):
                self.dma("sp", w2[16 * d:16 * d + 16, d, :], self.din["gla_gate_w2"][layer, d], (), [bw2])
            negb = S("L_negb", [128, 2]); bnegb = Buf("negb")
            for d in range(2):
                self.dma("sp", negb[:, d:d + 1], self.din["gla_gate_b"][layer, d:d + 1, :].rearrange("o c -> c o"), (), [bnegb])
            self.ts("dve", negb[:], negb[:], -1.0, ALU.mult, reads=[bnegb], writes=[bnegb])
            gnw = S("L_gnw", [128, 1]); bgnw = Buf("gnw")
            for r_ in range(2):
                self.dma("sp", gnw[64 * r_:64 * r_ + 64, :], self.din["gla_norm_w"][layer:layer + 1, :].rearrange("o d -> d o"), (), [bgnw])
            hm = S("L_hm", [128, 4]); bhm = Buf("hm")
            self.dma("sp", hm[:], self.din["headmask"][:, :], (), [bhm])
            bdm = S("L_bdm", [128, 256]); bbdm = Buf("bdm")
            self.cp("dve", bdm[:].rearrange("p (h v) -> p h v", h=4), hm[:].unsqueeze(2).to_broadcast([128, 4, 64]), [bhm], [bbdm])
            rm = S("L_rm", [128, 512]); brm = Buf("rm")
            self.dma("sp", rm[:], self.din["resetm"][:, :], (), [brm])
            ones1 = self.ones_f[:, 0:1]
            with contextlib.ExitStack() as st1:
                S1 = lambda n, s, d=F32: st1.enter_context(nc.sbuf_tensor(self.nm(n), s, d))
                PS1 = lambda n, s, d=F32: st1.enter_context(nc.psum_tensor(self.nm(n), s, d))
                T = {}
                for nme in ("a", "q", "k", "e", "g", "F", "x1", "x2", "kh"):
                    T[nme] = [S1("L1_" + nme + str(i), [128, 512]) for i in range(2)]
                khb = [S1("L1_khb%d" % i, [128, 512], BF16) for i in range(2)]
                B1 = {nme: [Buf(nme + str(i)) for i in range(2)] for nme in list(T) + ["khb"]}
                pz = [PS1("L1_pz%d" % i, [128, 512]) for i in range(2)]
                bpz = [Buf("pz%d" % i) for i in range(2)]
                ptr = [PS1("L1_pt%d" % i, [128, 512], BF16) for i in range(2)]
                bptr = [Buf("ptr%d" % i) for i in range(2)]
                fq, fk, fa = FM_IDX["gq"], FM_IDX["gk"], FM_IDX["ga"]
                qscale = 32.0 ** -0.5
                for gi, (g0, gn) in enumerate(GROUPS):
                    i2 = gi % 2
                    a_, q_, k_ = T["a"][i2], T["q"][i2], T["k"][i2]
                    ba_, bq_, bk_ = B1["a"][i2], B1["q"][i2], B1["k"][i2]
                    self.dma("sp", a_[0:32, :gn], self.scr["projF"][fa, 0:32, g0:g0 + gn], [self.sb["projF"][fa]], [ba_])
                    self.dma("sp", q_[:, :gn], self.scr["projF"][fq, :, g0:g0 + gn], [self.sb["projF"][fq]], [bq_])
                    self.dma("sp", k_[:, :gn], self.scr["projF"][fk, :, g0:g0 + gn], [self.sb["projF"][fk]], [bk_])
                    cks = chunks_in(g0, gn)
                    for d in range(2):
                        e_, g_, F_ = T["e"][d], T["g"][d], T["F"][d]
                        be_, bg_, bF_ = B1["e"][d], B1["g"][d], B1["F"][d]
                        self.mm(pz[d][:, :gn], w2[:, d, :], a_[0:32, :gn], True, True, [bw2, ba_], [bpz[d]])
                        self.act(e_[:, :gn], pz[d][:, :gn], AF.Exp, [bpz[d], bnegb], [be_], bias=negb[:, d:d + 1], scale=-1.0)
                        self.act(e_[:, :gn], e_[:, :gn], AF.Ln, [be_, self.b_ones], [be_], bias=ones1, scale=1.0)
                        self.ts("dve", g_[:, :gn], e_[:, :gn], -1.0 / 16.0, ALU.mult, reads=[be_], writes=[bg_])
                        P.op("dve", (lambda e, F_=F_, g_=g_, gn=gn: e.tensor_tensor_scan(
                            out=F_[:, :gn], data0=rm[:, :gn], data1=g_[:, :gn], initial=0.0, op0=ALU.mult, op1=ALU.add)),
                            [brm, bg_], [bF_])
                    Ff, Fb, gb = T["F"][0], T["F"][1], T["g"][1]
                    x1, x2, kh = T["x1"][i2], T["x2"][i2], T["kh"][i2]
                    bx1, bx2, bkh = B1["x1"][i2], B1["x2"][i2], B1["kh"][i2]
                    bo = bfm[gi]
                    sl = slice(g0, g0 + gn)
                    self.act(x1[:, :gn], Ff[:, :gn], AF.Exp, [B1["F"][0]], [bx1])
                    self.stt("dve", Qf[:, sl], q_[:, :gn], qscale, x1[:, :gn], ALU.mult, ALU.mult, [bq_, bx1], [bo])
                    self.act(x2[:, :gn], Ff[:, :gn], AF.Exp, [B1["F"][0]], [bx2], scale=-1.0)
                    self.tt("pool", Kf[:, sl], k_[:, :gn], x2[:, :gn], ALU.mult, [bk_, bx2], [bo])
                    for (c0, cn) in cks:
                        ci = CHUNKS.index((c0, cn))
                        o = c0 - g0
                        last = Ff[:, o + cn - 1:o + cn]
                        self.act(kh[:, o:o + cn], Ff[:, o:o + cn], AF.Exp, [B1["F"][0]], [bkh], bias=last, scale=-1.0)
                        self.act(decf[:, ci:ci + 1], last, AF.Exp, [B1["F"][0]], [bdec[gi]])
                    self.tt("dve", khb[0][:, :gn], k_[:, :gn], kh[:, :gn], ALU.mult, [bk_, bkh], [B1["khb"][0]])
                    self.tt("dve", Fb[:, :gn], Fb[:, :gn], gb[:, :gn], ALU.subtract, [B1["F"][1], B1["g"][1]], [B1["F"][1]])
                    self.act(x1[:, :gn], Fb[:, :gn], AF.Exp, [B1["F"][1]], [bx1], scale=-1.0)
                    self.stt("dve", Qb[:, sl], q_[:, :gn], qscale, x1[:, :gn], ALU.mult, ALU.mult, [bq_, bx1], [bo])
                    self.act(x2[:, :gn], Fb[:, :gn], AF.Exp, [B1["F"][1]], [bx2])
                    self.tt("pool", khb[1][:, :gn], k_[:, :gn], x2[:, :gn], ALU.mult, [bk_, bx2], [B1["khb"][1]])
                    self.cp("pool", Kb[:, sl], khb[1][:, :gn], [B1["khb"][1]], [bo])
                    for (c0, cn) in cks:
                        ci = CHUNKS.index((c0, cn))
                        o = c0 - g0
                        self.tt("dve", decb[:, ci:ci + 1], Fb[:, o + cn - 1:o + cn], gb[:, o + cn - 1:o + cn], ALU.add,
                                [B1["F"][1], B1["g"][1]], [bdec[gi]])
                        self.act(kh[:, o:o + cn], Fb[:, o:o + cn], AF.Exp, [B1["F"][1], bdec[gi]], [bkh],
                                 bias=decb[:, ci:ci + 1], scale=-1.0)
                        self.act(decb[:, ci:ci + 1], decb[:, ci:ci + 1], AF.Exp, [bdec[gi], bkh], [bdec[gi]])
                    self.stt("dve", Qbar[:, sl], q_[:, :gn], qscale, kh[:, :gn], ALU.mult, ALU.mult, [bq_, bkh], [bo])
                    for (c0, cn) in cks:
                        ci = CHUNKS.index((c0, cn))
                        o = c0 - g0
                        for d, dstT in ((0, Ktf), (1, Ktb)):
                            pt_, bpt_ = ptr[d], bptr[d]
                            self.tr(pt_[:cn, 0:128], khb[d][:, o:o + cn], self.identb[:, :], [B1["khb"][d], self.b_identb], [bpt_])
                            self.cp("act" if d == 0 else "dve", dstT[:cn, ci, :], pt_[:cn, 0:128], [bpt_], [bKt[ci]])
            P.barrier()
            with contextlib.ExitStack() as st2:
                S2 = lambda n, s, d=F32: st2.enter_context(nc.sbuf_tensor(self.nm(n), s, d))
                PS2 = lambda n, s, d=F32: st2.enter_context(nc.psum_tensor(self.nm(n), s, d))
                allfm = bfm
                SbBD = S2("L2_SbBD", [128, NC, 256], BF16)
                bSbBD = [Buf("SbBD%d" % i) for i in range(NC)]
                Sst = S2("L2_S", [128, 256]); bS = Buf("S")
                oraw = S2("L2_oraw", [128, 2, L]); boraw = [Buf("oraw%d" % i) for i in range(len(GROUPS))]
                pdS = [PS2("L2_pdS%d" % i, [128, 256]) for i in range(2)]
                bpdS = [Buf("pdS%d" % i) for i in range(2)]
                self.memset("dve", Sst[:], 0.0, [bS])
                for n_, ci in enumerate(range(NC - 1, -1, -1)):
                    c0, cn = CHUNKS[ci]
                    gi = 0 if ci == 0 else 1 + (ci - 1) // 4
                    self.tt("pool", SbBD[:, ci, :], Sst[:], bdm[:], ALU.mult, [bS, bbdm], [bSbBD[ci]])
                    p_, bp_ = pdS[n_ % 2], bpdS[n_ % 2]
                    self.mm(p_[:, :], Ktb[:cn, ci, :], vt[:cn, ci, :], True, True, [bKt[ci], bvt], [bp_])
                    self.stt("dve", Sst[:], Sst[:], decb[:, ci:ci + 1], p_[:, :], ALU.mult, ALU.add, [bS, bdec[gi], bp_], [bS])
                self.memset("dve", Sst[:], 0.0, [bS])
                SfBD = [S2("L2_SfBD%d" % i, [128, 256], BF16) for i in range(2)]
                bSfBD = [Buf("SfBD%d" % i) for i in range(2)]
                Qm = [S2("L2_Qm%d" % i, [128, 4, 128], BF16) for i in range(4)]
                bQm = [Buf("Qm%d" % i) for i in range(4)]
                Am = [S2("L2_Am%d" % i, [128, 4, 128], BF16) for i in range(4)]
                bAm = [Buf("Am%d" % i) for i in range(4)]
                pA = [PS2("L2_pA%d" % i, [128, 512]) for i in range(2)]
                bpA = [Buf("pA%d" % i) for i in range(2)]
                po = [PS2("L2_po%d" % i, [128, 128]) for i in range(2)]
                bpo = [Buf("po%d" % i) for i in range(2)]
                hmb = hm[:, 0:4].unsqueeze(2)
                def gla_front(ci):
                    c0, cn = CHUNKS[ci]
                    gi = 0 if ci == 0 else 1 + (ci - 1) // 4
                    sl = slice(c0, c0 + cn)
                    for d, (Qd, Kd, tri) in enumerate(((Qf, Kf, "tri_incl"), (Qb, Kb, "tri_ge"))):
                        j = (ci % 2) * 2 + d
                        qm, bqm = Qm[j], bQm[j]
                        am, bam = Am[j], bAm[j]
                        self.tt("dve" if d == 0 else "pool", qm[:, :, :cn], Qd[:, sl].unsqueeze(1).to_broadcast([128, 4, cn]),
                                hmb.to_broadcast([128, 4, cn]), ALU.mult, [bfm[gi], bhm], [bqm])
                        pa2 = pA[d][:cn, 0:4 * cn]
                        pa = pa2.rearrange("p (h l) -> p h l", h=4)
                        self.mm(pa2, Kd[:, sl], qm[:, :, :cn], True, True, [bfm[gi], bqm], [bpA[d]])
                        self.tt("dve", am[:cn, :, :cn], pa, self.c[tri][:cn, :cn].unsqueeze(1).to_broadcast([cn, 4, cn]), ALU.mult,
                                [bpA[d], self.const_bufs[tri]], [bam])

                def gla_back(ci):
                    c0, cn = CHUNKS[ci]
                    gi = 0 if ci == 0 else 1 + (ci - 1) // 4
                    sl = slice(c0, c0 + cn)
                    sf, bsf = SfBD[ci % 2], bSfBD[ci % 2]
                    self.tt("pool", sf[:], Sst[:], bdm[:], ALU.mult, [bS, bbdm], [bsf])
                    amf, amb = Am[(ci % 2) * 2], Am[(ci % 2) * 2 + 1]
                    bamf, bamb = bAm[(ci % 2) * 2], bAm[(ci % 2) * 2 + 1]
                    for t in range(2):
                        for hh in range(2):
                            h = 2 * t + hh
                            o_ = po[t][hh * 64:(hh + 1) * 64, :cn]
                            self.mm(o_, sf[:, h * 64:(h + 1) * 64], Qf[:, sl], True, False, [bsf, bfm[gi]], [bpo[t]])
                            self.mm(o_, SbBD[:, ci, h * 64:(h + 1) * 64], Qbar[:, sl], False, False, [bSbBD[ci], bfm[gi]], [bpo[t]])
                            self.mm(o_, vt[:cn, ci, h * 64:(h + 1) * 64], amf[:cn, h, :cn], False, False, [bvt, bamf], [bpo[t]])
                            self.mm(o_, vt[:cn, ci, h * 64:(h + 1) * 64], amb[:cn, h, :cn], False, True, [bvt, bamb], [bpo[t]])
                        self.cp("act", oraw[:, t, sl], po[t][:, :cn], [bpo[t]], [boraw[gi]])
                    p_, bp_ = pdS[ci % 2], bpdS[ci % 2]
                    self.mm(p_[:, :], Ktf[:cn, ci, :], vt[:cn, ci, :], True, True, [bKt[ci], bvt], [bp_])
                    self.stt("dve", Sst[:], Sst[:], decf[:, ci:ci + 1], p_[:, :], ALU.mult, ALU.add, [bS, bdec[gi], bp_], [bS])

                gla_front(0)
                for ci in range(NC):
                    if ci + 1 < NC:
                        gla_front(ci + 1)
                    gla_back(ci)
                xs = [S2("L3_x%d" % i, [128, 512]) for i in range(2)]
                bxs = [Buf("x%d" % i) for i in range(2)]
                sq = S2("L3_sq", [128, 512]); bsq = Buf("sq")
                rs = S2("L3_rs", [128, 512]); brs = Buf("rs")
                on = S2("L3_on", [128, 512]); bon = Buf("on")
                stg = [S2("L3_stg%d" % i, [128, 512], BF16) for i in range(2)]
                bstg = [Buf("stg%d" % i) for i in range(2)]
                it = 0
                for gi, (g0, gn) in enumerate(GROUPS):
                    sl = slice(g0, g0 + gn)
                    for t in range(2):
                        i2 = it % 2
                        it += 1
                        fr = FM_IDX["gr%d" % t]
                        self.dma("sp", xs[i2][:, :gn], self.scr["projF"][fr, :, sl], [self.sb["projF"][fr]], [bxs[i2]])
                        self.act(xs[i2][:, :gn], xs[i2][:, :gn], AF.Silu, [bxs[i2]], [bxs[i2]])
                        self.act(sq[:, :gn], oraw[:, t, sl], AF.Square, [boraw[gi]], [bsq])
                        self.mm(pA[0][:, :gn], self.c["blk64"][:], sq[:, :gn], True, True, [bsq, self.const_bufs["blk64"]], [bpA[0]])
                        self.rstd_from_ss(rs[:, :gn], pA[0][:, :gn], 64, [bpA[0]], [brs])
                        self.stt("dve", on[:, :gn], oraw[:, t, sl], gnw[:, 0:1], rs[:, :gn], ALU.mult, ALU.mult, [boraw[gi], bgnw, brs], [bon])
                        self.tt("dve", stg[i2][:, :gn], on[:, :gn], xs[i2][:, :gn], ALU.mult, [bon, bxs[i2]], [bstg[i2]])
                        self.dma("pool", self.scr["mixT"][2 + t, :, sl], stg[i2][:, :gn], [bstg[i2]], [self.sb["mixT"][2 + t]])

    def phase_SSD(self, layer):
        nc, P = self.nc, self.P
        P.barrier()
        NC = len(CHUNKS)
        NG = len(GROUPS)
        gof = lambda ci: 0 if ci == 0 else 1 + (ci - 1) // 4
        with contextlib.ExitStack() as st:
            S = lambda n, s, d=F32: st.enter_context(nc.sbuf_tensor(self.nm(n), s, d))
            BT = [S("D_BT%d" % g, [128, L], BF16) for g in range(2)]
            CT = [S("D_CT%d" % g, [128, L], BF16) for g in range(2)]
            xT = S("D_xT", [128, 2, L])
            bfm = [Buf("fm%d" % i) for i in range(NG)]
            xtok = S("D_xtok", [128, NC, 256], BF16)
            Btok = S("D_Btok", [128, NC, 256], BF16)
            btok = [Buf("tok%d" % i) for i in range(NC)]
            with contextlib.ExitStack() as st1:
                S1 = lambda n, s, d=F32: st1.enter_context(nc.sbuf_tensor(self.nm(n), s, d))
                PS1 = lambda n, s, d=F32: st1.enter_context(nc.psum_tensor(self.nm(n), s, d))
                w6 = S1("D1_w6", [6, 768]); bw6 = Buf("w6")
                self.dma("sp", w6[0:5, :], self.din["ssd_conv_w"][layer], (), [bw6])
                self.dma("sp", w6[5:6, :], self.din["ssd_conv_b"][layer:layer + 1, :], (), [bw6])
                wcol = S1("D1_wcol", [128, 6, 6]); bwcol = Buf("wcol")
                pw = PS1("D1_pw", [128, 64]); bpw = Buf("pw")
                for ct in range(6):
                    self.tr(pw[:, ct * 6:ct * 6 + 6], w6[0:6, ct * 128:(ct + 1) * 128], self.c["ident_f"][0:6, 0:6],
                            [bw6, self.const_bufs["ident_f"]], [bpw])
                self.cp("dve", wcol[:].rearrange("p a b -> p (a b)"), pw[:, 0:36], [bpw], [bwcol])
                Dg = S1("D1_Dg", [128, 6, 5, 128], BF16); bDg = Buf("Dg")
                for ct in range(6):
                    for j in range(5):
                        self.ts("dve" if (ct + j) % 2 == 0 else "pool", Dg[:, ct, j, :], self.c["ident_f"][:, :], wcol[:, ct, j:j + 1], ALU.mult,
                                reads=[bwcol, self.const_bufs["ident_f"]], writes=[bDg])
                Ub = [S1("D1_Ub%d" % ct, [128, L + 4], BF16) for ct in range(6)]
                bUb = [Buf("Ub%d" % ct) for ct in range(6)]
                names = ["xs0", "xs1", "B0", "B1", "C0", "C1"]
                for ct in range(6):
                    f = FM_IDX[names[ct]]
                    self.memset("pool", Ub[ct][:, 0:2], 0.0, [bUb[ct]])
                    self.memset("pool", Ub[ct][:, L + 2:L + 4], 0.0, [bUb[ct]])
                    self.dma("pool", Ub[ct][:, 2:L + 2], self.scr["projF"][f, :, :], [self.sb["projF"][f]], [bUb[ct]])
                pc = [PS1("D1_pc%d" % i, [128, 512]) for i in range(3)]
                bpc = [Buf("pc%d" % i) for i in range(3)]
                xb = [S1("D1_xb%d" % i, [128, 2, 512], BF16) for i in range(2)]
                bxb = [Buf("xb%d" % i) for i in range(2)]
                ptr = [PS1("D1_ptr%d" % i, [128, 512], BF16) for i in range(2)]
                bptr = [Buf("ptr%d" % i) for i in range(2)]
                n = 0
                for gi, (g0, gn) in enumerate(GROUPS):
                    sl = slice(g0, g0 + gn)
                    x_b, bx_b = xb[gi % 2], bxb[gi % 2]
                    for ct in range(6):
                        p_, bp_ = pc[n % 3], bpc[n % 3]
                        n += 1
                        for j in range(5):
                            self.mm(p_[:, :gn], Dg[:, ct, j, :], Ub[ct][:, g0 + j:g0 + j + gn], j == 0, j == 4, [bDg, bUb[ct]], [bp_])
                        bias = wcol[:, ct, 5:6]
                        if ct < 2:
                            self.act(xT[:, ct, sl], p_[:, :gn], AF.Silu, [bp_, bwcol], [bfm[gi]], bias=bias)
                            self.cp("pool", x_b[:, ct, :gn], xT[:, ct, sl], [bfm[gi]], [bx_b])
                        elif ct < 4:
                            self.act(BT[ct - 2][:, sl], p_[:, :gn], AF.Silu, [bp_, bwcol], [bfm[gi]], bias=bias)
                        else:
                            self.act(CT[ct - 4][:, sl], p_[:, :gn], AF.Silu, [bp_, bwcol], [bfm[gi]], bias=bias)
                    for (c0, cn) in chunks_in(g0, gn):
                        ci = CHUNKS.index((c0, cn))
                        o = c0 - g0
                        pt_, bpt_ = ptr[ci % 2], bptr[ci % 2]
                        for t in range(2):
                            self.tr(pt_[:cn, t * 128:(t + 1) * 128], x_b[:, t, o:o + cn], self.identb[:, :], [bx_b, self.b_identb], [bpt_])
                            self.tr(pt_[:cn, 256 + t * 128:256 + (t + 1) * 128], BT[t][:, c0:c0 + cn], self.identb[:, :],
                                    [bfm[gi], self.b_identb], [bpt_])
                        self.cp("act", xtok[:cn, ci, :], pt_[:cn, 0:256], [bpt_], [btok[ci]])
                        self.cp("dve", Btok[:cn, ci, :], pt_[:cn, 256:512], [bpt_], [btok[ci]])
            P.barrier()
            with contextlib.ExitStack() as st2:
                S2 = lambda n, s, d=F32: st2.enter_context(nc.sbuf_tensor(self.nm(n), s, d))
                PS2 = lambda n, s, d=F32: st2.enter_context(nc.psum_tensor(self.nm(n), s, d))
                dt = S2("D2_dt", [128, NC, 8]); bdt = Buf("dt")
                dA = S2("D2_dA", [128, NC, 8]); bdA = Buf("dA")
                self.memset("pool", dt[:], 0.0, [bdt])
                vtk = self.scr["vtok"]
                self.dma("sp", dt[:, 1:NC, :], vtk[NM:, V_DT:V_DT + 8].rearrange("(c p) d -> p c d", p=128), [self.sb["vtok"][0]], [bdt])
                self.dma("sp", dt[0:NM, 0, :], vtk[0:NM, V_DT:V_DT + 8], [self.sb["vtok"][0]], [bdt])
                brow = S2("D2_brow", [128, 8]); bbrow = Buf("brow")
                arow = S2("D2_arow", [128, 8]); barow = Buf("arow")
                self.dma("sp", brow[:], self.din["ssd_dt_bias"][layer:layer + 1].rearrange("o d h -> o (d h)").partition_broadcast(128), (), [bbrow])
                self.dma("sp", arow[:], self.din["ssd_a_log"][layer:layer + 1].rearrange("o d h -> o (d h)").partition_broadcast(128), (), [barow])
                self.act(arow[:], arow[:], AF.Exp, [barow], [barow])
                self.ts("dve", arow[:], arow[:], -1.0, ALU.mult, reads=[barow], writes=[barow])
                bc8 = lambda t: t[:].unsqueeze(1).to_broadcast([128, NC, 8])
                self.tt("dve", dt[:], dt[:], bc8(brow), ALU.add, [bdt, bbrow], [bdt])
                self.act(dt[:], dt[:], AF.Exp, [bdt], [bdt])
                self.act(dt[:], dt[:], AF.Ln, [bdt, self.b_ones], [bdt], bias=self.ones_f[:, 0:1])
                self.tt("dve", dA[:], dt[:], bc8(arow), ALU.mult, [bdt, barow], [bdA])
                Tst = S2("D2_T", [128, NC, 8]); eT = S2("D2_eT", [128, NC, 8]); bT = [Buf("T%d" % i) for i in range(NC)]
                xwb = S2("D2_xwb", [128, NC, 256], BF16); bxwb = [Buf("xwb%d" % i) for i in range(NC)]
                SbT = S2("D2_SbT", [128, NC, 256], BF16); bSbT = [Buf("SbT%d" % i) for i in range(NC)]
                yraw = S2("D2_yraw", [128, 2, L]); byraw = [Buf("yraw%d" % i) for i in range(NG)]
                pSm = [PS2("D2_pSm0", [128, 16])] * 2
                bpSm = [Buf("pSm0")] * 2
                sm = [S2("D2_sm%d" % i, [128, 16]) for i in range(2)]
                bsm = [Buf("sm%d" % i) for i in range(2)]
                tri_i, tri_s, tri_g = self.c["tri_incl"], self.c["tri_strict"], self.c["tri_ge"]
                btri = [self.const_bufs["tri_incl"], self.const_bufs["tri_strict"], self.const_bufs["tri_ge"]]
                v4 = lambda ap, cn: ap.rearrange("p (h d) -> p h d", h=4)
                for ci, (c0, cn) in enumerate(CHUNKS):
                    i2 = ci % 2
                    ps_, bps_ = pSm[i2], bpSm[i2]
                    s_, bs_ = sm[i2], bsm[i2]
                    self.mm(ps_[:cn, 4:8], tri_s[:cn, :cn], dA[:cn, ci, 4:8], True, True, [bdA, btri[1]], [bps_])
                    self.mm(ps_[:, 8:16], self.ones_f[:cn, :], dA[:cn, ci, 0:8], True, True, [bdA, self.b_ones], [bps_])
                    self.cp("dve", Tst[:, ci, :], ps_[:, 8:16], [bps_], [bT[ci]])
                    self.act(eT[:, ci, :], ps_[:, 8:16], AF.Exp, [bps_], [bT[ci]])
                    self.act(s_[:cn, 4:8], ps_[:cn, 4:8], AF.Exp, [bps_], [bs_])
                    self.tt("dve", s_[:cn, 4:8], s_[:cn, 4:8], dt[:cn, ci, 4:8], ALU.mult, [bs_, bdt], [bs_])
                    self.tt("pool", v4(xwb[:cn, ci, :], cn), v4(xtok[:cn, ci, :], cn), s_[:cn, 4:8].unsqueeze(2).to_broadcast([cn, 4, 64]),
                            ALU.mult, [btok[ci], bs_], [bxwb[ci]])
                Sst = S2("D2_S", [128, 256]); bS = Buf("S")
                pdS = [PS2("D2_pdS%d" % i, [128, 256]) for i in range(2)]
                bpdS = [Buf("pdS%d" % i) for i in range(2)]
                self.memset("dve", Sst[:], 0.0, [bS])
                for n_, ci in enumerate(range(NC - 1, -1, -1)):
                    c0, cn = CHUNKS[ci]
                    self.cp("pool", SbT[:, ci, :], Sst[:], [bS], [bSbT[ci]])
                    p_, bp_ = pdS[n_ % 2], bpdS[n_ % 2]
                    for g in range(2):
                        self.mm(p_[:, g * 128:(g + 1) * 128], Btok[:cn, ci, g * 128:(g + 1) * 128], xwb[:cn, ci, g * 128:(g + 1) * 128],
                                True, True, [btok[ci], bxwb[ci]], [bp_])
                    self.tt("dve", v4(Sst[:], 0), v4(Sst[:], 0), eT[:, ci, 4:8].unsqueeze(2).to_broadcast([128, 4, 64]), ALU.mult,
                            [bS, bT[ci]], [bS])
                    self.tt("dve", Sst[:], Sst[:], p_[:, :], ALU.add, [bS, bp_], [bS])
                self.memset("dve", Sst[:], 0.0, [bS])
                SfT = [S2("D2_SfT%d" % i, [128, 256], BF16) for i in range(2)]
                bSfT = [Buf("SfT%d" % i) for i in range(2)]
                dArep = [S2("D2_dArep%d" % i, [128, 8, 128]) for i in range(2)]
                bdArep = [Buf("dArep%d" % i) for i in range(2)]
                pG = PS2("D2_pG", [128, 2, 128]); bpG = Buf("pG")
                pR = [PS2("D2_pR%d" % d, [128, 4, 128]) for d in range(2)]
                bpR = [Buf("pR%d" % d) for d in range(2)]
                po = [PS2("D2_po%d" % t, [128, 128]) for t in range(2)]; bpo = [Buf("po%d" % t) for t in range(2)]
                E = [S2("D2_E%d" % d, [128, 4, 128]) for d in range(2)]
                bE = [Buf("E%d" % d) for d in range(2)]
                Wm = [[S2("D2_W%d_%d" % (i, d), [128, 4, 128], BF16) for d in range(2)] for i in range(2)]
                bWm = [[Buf("W%d_%d" % (i, d)) for d in range(2)] for i in range(2)]
                er = [S2("D2_er%d" % d, [128, 4, 128]) for d in range(2)]
                ber = [Buf("er%d" % d) for d in range(2)]
                Cs = [[S2("D2_Cs%d_%d" % (i, d), [128, 4, 128], BF16) for d in range(2)] for i in range(2)]
                bCs = [[Buf("Cs%d_%d" % (i, d)) for d in range(2)] for i in range(2)]
                xd = [[S2("D2_xd%d_%d" % (i, d), [128, 256], BF16) for d in range(2)] for i in range(2)]
                bxd = [[Buf("xd%d_%d" % (i, d)) for d in range(2)] for i in range(2)]
                xwf = [S2("D2_xwf%d" % i, [128, 256], BF16) for i in range(2)]
                bxwf = [Buf("xwf%d" % i) for i in range(2)]
                def ssd_front(ci):
                    c0, cn = CHUNKS[ci]
                    i2 = ci % 2
                    gi = gof(ci)
                    sl = slice(c0, c0 + cn)
                    ps_, bps_ = pSm[i2], bpSm[i2]
                    s_, bs_ = sm[i2], bsm[i2]
                    self.mm(ps_[:cn, 0:4], tri_i[:cn, :cn], dA[:cn, ci, 0:4], True, True, [bdA, btri[0]], [bps_])
                    self.mm(ps_[:cn, 4:8], tri_s[:cn, :cn], dA[:cn, ci, 4:8], True, True, [bdA, btri[1]], [bps_])
                    self.cp("act", s_[:cn, 0:8], ps_[:cn, 0:8], [bps_], [bs_])
                    for d in range(2):
                        tri_r = tri_i if d == 0 else tri_s
                        for h in range(4):
                            self.mm(pR[d][:, h, :cn], dA[:cn, ci, 4 * d + h:4 * d + h + 1].to_broadcast([cn, 128]), tri_r[:cn, :cn], True, True, [bdA, btri[d]], [bpR[d]])
                    for g in range(2):
                        self.mm(pG[:cn, g, :cn], BT[g][:, sl], CT[g][:, sl], True, True, [bfm[gi]], [bpG])

                def ssd_front2(ci):
                    c0, cn = CHUNKS[ci]
                    i2 = ci % 2
                    gi = gof(ci)
                    sl = slice(c0, c0 + cn)
                    ps_, bps_ = pSm[i2], bpSm[i2]
                    s_, bs_ = sm[i2], bsm[i2]
                    for d in range(2):
                        colb = s_[:cn, 4 * d:4 * d + 4].unsqueeze(2).to_broadcast([cn, 4, cn])
                        self.stt("dve", E[d][:cn, :, :cn], colb, -1.0, pR[d][:cn, :, :cn], ALU.mult, ALU.add, [bs_, bpR[d]], [bE[d]])
                    for d in range(2):
                        self.stt("dve", E[d][:cn, :, :cn], E[d][:cn, :, :cn], -1.0, E[d][:cn, :, :cn], ALU.mult, ALU.min, [bE[d]], [bE[d]])
                    self.stt("dve", er[1][:, :, :cn], pR[1][:, :, :cn], -1.0, Tst[:, ci, 4:8].unsqueeze(2).to_broadcast([128, 4, cn]),
                             ALU.mult, ALU.add, [bpR[1], bT[ci]], [ber[1]])
                    for d in range(2):
                        self.tt("dve", v4(xd[i2][d][:cn, :], cn), v4(xtok[:cn, ci, :], cn),
                                dt[:cn, ci, 4 * d:4 * d + 4].unsqueeze(2).to_broadcast([cn, 4, 64]), ALU.mult, [btok[ci], bdt], [bxd[i2][d]])
                    self.tt("dve", s_[:cn, 8:12], Tst[:cn, ci, 0:4], s_[:cn, 0:4], ALU.subtract, [bT[ci], bs_], [bs_])
                    for d in range(2):
                        self.act(E[d][:cn, :, :cn], E[d][:cn, :, :cn], AF.Exp, [bE[d]], [bE[d]])
                    self.act(er[0][:, :, :cn], pR[0][:, :, :cn], AF.Exp, [bpR[0]], [ber[0]])
                    self.act(er[1][:, :, :cn], er[1][:, :, :cn], AF.Exp, [ber[1]], [ber[1]])
                    self.act(s_[:cn, 8:12], s_[:cn, 8:12], AF.Exp, [bs_], [bs_])
                    for d in range(2):
                        tri_m = tri_i if d == 0 else tri_g
                        self.tt("pool", E[d][:cn, :, :cn], E[d][:cn, :, :cn], tri_m[:cn, :cn].unsqueeze(1).to_broadcast([cn, 4, cn]), ALU.mult,
                                [bE[d], btri[0 if d == 0 else 2]], [bE[d]])
                    for d in range(2):
                        for g in range(2):
                            self.tt("pool", Cs[i2][d][:, 2 * g:2 * g + 2, :cn], er[d][:, 2 * g:2 * g + 2, :cn],
                                    CT[g][:, sl].unsqueeze(1).to_broadcast([128, 2, cn]), ALU.mult, [ber[d], bfm[gi]], [bCs[i2][d]])
                    self.tt("pool", v4(xwf[i2][:cn, :], cn), v4(xd[i2][0][:cn, :], cn), s_[:cn, 8:12].unsqueeze(2).to_broadcast([cn, 4, 64]),
                            ALU.mult, [bxd[i2][0], bs_], [bxwf[i2]])
                    for d in range(2):
                        w_, bw_ = Wm[i2][d], bWm[i2][d]
                        self.tt("dve", w_[:cn, :, :cn].rearrange("p (g r) l -> p g r l", g=2),
                                E[d][:cn, :, :cn].rearrange("p (g r) l -> p g r l", g=2),
                                pG[:cn, :, :cn].unsqueeze(2).to_broadcast([cn, 2, 2, cn]), ALU.mult, [bE[d], bpG], [bw_])

                def ssd_back(ci):
                    c0, cn = CHUNKS[ci]
                    i2 = ci % 2
                    gi = gof(ci)
                    sl = slice(c0, c0 + cn)
                    sf, bsf = SfT[i2], bSfT[i2]
                    self.cp("act", sf[:], Sst[:], [bS], [bsf])
                    for t in range(2):
                        for hh in range(2):
                            h = 2 * t + hh
                            o_ = po[t][hh * 64:(hh + 1) * 64, :cn]
                            self.mm(o_, sf[:, h * 64:(h + 1) * 64], Cs[i2][0][:, h, :cn], True, False, [bsf, bCs[i2][0]], [bpo[t]])
                            self.mm(o_, SbT[:, ci, h * 64:(h + 1) * 64], Cs[i2][1][:, h, :cn], False, False, [bSbT[ci], bCs[i2][1]], [bpo[t]])
                            self.mm(o_, xd[i2][0][:cn, h * 64:(h + 1) * 64], Wm[i2][0][:cn, h, :cn], False, False, [bxd[i2][0], bWm[i2][0]], [bpo[t]])
                            self.mm(o_, xd[i2][1][:cn, h * 64:(h + 1) * 64], Wm[i2][1][:cn, h, :cn], False, True, [bxd[i2][1], bWm[i2][1]], [bpo[t]])
                        self.cp("act", yraw[:, t, sl], po[t][:, :cn], [bpo[t]], [byraw[gi]])
                    p_, bp_ = pdS[i2], bpdS[i2]
                    for g in range(2):
                        self.mm(p_[:, g * 128:(g + 1) * 128], Btok[:cn, ci, g * 128:(g + 1) * 128], xwf[i2][:cn, g * 128:(g + 1) * 128],
                                True, True, [btok[ci], bxwf[i2]], [bp_])
                    self.tt("dve", v4(Sst[:], 0), v4(Sst[:], 0), eT[:, ci, 0:4].unsqueeze(2).to_broadcast([128, 4, 64]), ALU.mult,
                            [bS, bT[ci]], [bS])
                    self.tt("dve", Sst[:], Sst[:], p_[:, :], ALU.add, [bS, bp_], [bS])

                ssd_front(0)
                ssd_front2(0)
                for ci in range(NC):
                    if ci + 1 < NC:
                        ssd_front(ci + 1)
                    ssd_back(ci)
                    if ci + 1 < NC:
                        ssd_front2(ci + 1)
                Dcol = S2("D3_D", [128, 2]); bD = Buf("D")
                nw = S2("D3_nw", [128, 2]); bnw = Buf("nw")
                for t in range(2):
                    for hh in range(2):
                        h = 2 * t + hh
                        self.dma("sp", Dcol[hh * 64:(hh + 1) * 64, t:t + 1], self.din["ssd_d"][layer:layer + 1, h:h + 1].partition_broadcast(64), (), [bD])
                    self.dma("sp", nw[:, t:t + 1], self.din["ssd_norm_w"][layer:layer + 1, t * 128:(t + 1) * 128].rearrange("o d -> d o"), (), [bnw])
                P.barrier()
                f2 = lambda t_: t_[:].rearrange("p h l -> p (h l)")
                zs = [f2(E[0]), f2(E[1])]
                bzs = [Buf("z%d" % i) for i in range(2)]
                yg = [f2(er[0]), f2(er[1])]
                byg = [Buf("y%d" % i) for i in range(2)]
                sq = [f2(dArep[0])[:, 0:512], f2(dArep[1])[:, 0:512]]
                bsq = [Buf("sq%d" % i) for i in range(2)]
                rs = f2(dArep[0])[:, 512:1024]; brs = Buf("rs")
                stg = [f2(Wm[0][0]), f2(Wm[0][1])]
                bstg = [Buf("stg%d" % i) for i in range(2)]
                pss = pR[0][:].rearrange("p h l -> p (h l)"); bpss = Buf("pss")
                for gi, (g0, gn) in enumerate(GROUPS):
                    sl = slice(g0, g0 + gn)
                    for t in range(2):
                        fz = FM_IDX["z%d" % t]
                        self.dma("sp", zs[t][:, :gn], self.scr["projF"][fz, :, sl], [self.sb["projF"][fz]], [bzs[t]])
                        self.act(zs[t][:, :gn], zs[t][:, :gn], AF.Silu, [bzs[t]], [bzs[t]])
                        self.stt("dve", yg[t][:, :gn], xT[:, t, sl], Dcol[:, t:t + 1], yraw[:, t, sl], ALU.mult, ALU.add,
                                 [bfm[gi], bD, byraw[gi]], [byg[t]])
                        self.tt("dve", yg[t][:, :gn], yg[t][:, :gn], zs[t][:, :gn], ALU.mult, [byg[t], bzs[t]], [byg[t]])
                        self.act(sq[t][:, :gn], yg[t][:, :gn], AF.Square, [byg[t]], [bsq[t]])
                        self.mm(pss[:, :gn], self.ones_f[:, :], sq[t][:, :gn], t == 0, t == 1, [bsq[t], self.b_ones], [bpss])
                    self.rstd_from_ss(rs[:, :gn], pss[:, :gn], 256, [bpss], [brs])
                    for t in range(2):
                        self.stt("dve", stg[t][:, :gn], yg[t][:, :gn], nw[:, t:t + 1], rs[:, :gn], ALU.mult, ALU.mult,
                                 [byg[t], bnw, brs], [bstg[t]])
                        self.dma("pool", self.scr["mixT"][t, :, sl], stg[t][:, :gn], [bstg[t]], [self.sb["mixT"][t]])


def build_program():
    kb = KB()
    with contextlib.ExitStack() as st:
        kb.load_consts(st)
        for layer in range(DEPTH):
            kb.phase_A(layer)
            kb.phase_SSD(layer)
            kb.phase_GLA(layer)
            kb.phase_SWA(layer)
            kb.phase_G2(layer)
            kb.phase_C(layer)
        kb.P.emit()
    return kb


def kernel(**inputs):
    n_cores = 8
    kb = build_program()
    consts = make_consts()
    shared = {}
    for k in PARAM_SHAPES:
        shared[k] = np.ascontiguousarray(np.asarray(inputs[k], dtype=np.float32))
    for k, v in consts.items():
        shared["c_" + k] = v
    x = np.asarray(inputs["x"], dtype=np.float32)
    in_maps = []
    for b in range(n_cores):
        m = dict(shared)
        m["x"] = np.ascontiguousarray(x[b])
        in_maps.append(m)
    res = run_bass_kernel_spmd(kb.nc, in_maps, core_ids=list(range(n_cores)))
    out = np.stack([np.asarray(r["out"], dtype=np.float32) for r in res.results], axis=0)
    return out
```

```python
import contextlib
import math
import numpy as np
import concourse.bass as bass
import concourse.mybir as mybir
from concourse.bass_utils import run_bass_kernel_spmd

F32 = mybir.dt.float32
BF16 = mybir.dt.bfloat16
AF = mybir.ActivationFunctionType
ALU = mybir.AluOpType
AX = mybir.AxisListType

D = 1024
SEQ = 4096
NM = 16
L = SEQ + NM
DEPTH = 2
EPS = 1e-6
INW = 2856
FFN_F = 2816
MOE_F = 3584
NEXP = 8
NEG = -30000.0

O_Z, O_XS, O_B, O_C, O_DT = 0, 256, 512, 768, 1024
O_GQ, O_GK, O_GV, O_GR, O_GA = 1032, 1160, 1288, 1544, 1800
O_SQ, O_SK, O_SV = 1832, 2088, 2216
O_AQ, O_AK, O_AV = 2344, 2600, 2728

FM_TILES = [
    ("z0", [(O_Z, 128)]), ("z1", [(O_Z + 128, 128)]),
    ("xs0", [(O_XS, 128)]), ("xs1", [(O_XS + 128, 128)]),
    ("B0", [(O_B, 128)]), ("B1", [(O_B + 128, 128)]),
    ("C0", [(O_C, 128)]), ("C1", [(O_C + 128, 128)]),
    ("gq", [(O_GQ, 128)]), ("gk", [(O_GK, 128)]),
    ("gr0", [(O_GR, 128)]), ("gr1", [(O_GR + 128, 128)]),
    ("ga", [(O_GA, 32)]),
    ("sq0", [(O_SQ, 128)]), ("sq1", [(O_SQ + 128, 128)]),
    ("sk0", [(O_SK, 64), (O_SK, 64)]), ("sk1", [(O_SK + 64, 64), (O_SK + 64, 64)]),
    ("aq0", [(O_AQ, 128)]), ("aq1", [(O_AQ + 128, 128)]),
    ("ak0", [(O_AK, 64), (O_AK, 64)]), ("ak1", [(O_AK + 64, 64), (O_AK + 64, 64)]),
]
FM_IDX = {n: i for i, (n, _) in enumerate(FM_TILES)}
NFM = len(FM_TILES)
TM_SEGS = [(O_GV, 256), (O_SV, 128), (O_AV, 128), (O_DT, 8)]
TMW = 520
V_GV, V_SV, V_AV, V_DT = 0, 256, 384, 512

CHUNKS = [(0, NM)] + [(NM + 128 * i, 128) for i in range(32)]
GROUPS = [(0, NM)] + [(NM + 512 * i, 512) for i in range(8)]
HALVES = [(0, NM + 2048), (NM + 2048, 2048)]


def chunks_in(t0, n):
    return [(c0, cn) for (c0, cn) in CHUNKS if c0 >= t0 and c0 + cn <= t0 + n]


ENGS = ("pe", "act", "dve", "pool", "sp")
NDSEM = 8


class Buf:
    __slots__ = ("name", "w", "rc", "rd", "const")

    def __init__(self, name="b"):
        self.name = name
        self.w = None
        self.rc = {}
        self.rd = []
        self.const = False


class Prog:
    def __init__(self, nc):
        self.nc = nc
        self.q = {e: [] for e in ENGS}
        self.ndma = {e: 0 for e in ENGS}
        self.seen_c = {e: {} for e in ENGS}
        self.seen_d = {e: set() for e in ENGS}
        self.marked = {e: set() for e in ENGS}
        self.pending = {e: [] for e in ENGS}
        self.last_tok = None

    def _need(self, eng, tok):
        k, e2, i2 = tok
        if k == "c":
            if e2 == "pe" and eng == "pe":
                return False
            if self.seen_c[eng].get(e2, -1) >= i2:
                return False
            self.seen_c[eng][e2] = i2
            self.marked[e2].add(i2)
            return True
        if tok in self.seen_d[eng]:
            return False
        self.seen_d[eng].add(tok)
        return True

    def barrier(self):
        toks = []
        for e in ENGS:
            n = len(self.q[e])
            for i in range(n - 1, -1, -1):
                if self.q[e][i][2] < 0:
                    toks.append(("c", e, i))
                    break
            nd = self.ndma[e]
            for d in range(max(0, nd - NDSEM), nd):
                toks.append(("d", e, d))
        for e in ENGS:
            self.pending[e] = list(toks)

    def op(self, eng, fn, reads=(), writes=(), dma=False):
        idx = len(self.q[eng])
        deps = list(self.pending[eng])
        self.pending[eng] = []
        for b in reads:
            if b.w is not None:
                deps.append(b.w)
        for b in writes:
            if b.w is not None:
                deps.append(b.w)
            for e2, v in b.rc.items():
                deps.append(("c", e2, v))
            deps.extend(b.rd)
        best = {}
        dl = []
        for d in deps:
            if d[0] == "c":
                if d[1] == eng and d[2] >= idx:
                    continue
                if best.get(d[1], -1) < d[2]:
                    best[d[1]] = d[2]
            else:
                dl.append(d)
        waits = []
        for e2, i2 in best.items():
            if self._need(eng, ("c", e2, i2)):
                waits.append(("c", e2, i2))
        for d in dl:
            if self._need(eng, d):
                waits.append(d)
        if dma:
            didx = self.ndma[eng]
            self.ndma[eng] += 1
            if didx >= NDSEM:
                d = ("d", eng, didx - NDSEM)
                if self._need(eng, d):
                    waits.append(d)
            tok = ("d", eng, didx)
        else:
            didx = -1
            tok = ("c", eng, idx)
        self.q[eng].append((fn, waits, didx))
        for b in reads:
            if b.const:
                continue
            if dma:
                b.rd.append(tok)
            else:
                b.rc[eng] = idx
        for b in writes:
            b.w = tok
            b.rc = {}
            b.rd = []
        self.last_tok = tok
        return tok

    def emit(self):
        nc = self.nc
        with contextlib.ExitStack() as st:
            csem = {e: st.enter_context(nc.semaphore("cs_" + e)) for e in ENGS}
            dsem = {e: [st.enter_context(nc.semaphore("ds_%s%d" % (e, i))) for i in range(NDSEM)]
                    for e in ENGS if self.ndma[e]}
            block = st.enter_context(nc.Block())
            cnt = {}
            for e in ENGS:
                c = 0
                arr = []
                m = self.marked[e]
                for i in range(len(self.q[e])):
                    if i in m:
                        c += 1
                    arr.append(c)
                cnt[e] = arr

            def run(en, eobj):
                mk = self.marked[en]
                for i, (fn, waits, didx) in enumerate(self.q[en]):
                    for (k, e2, i2) in waits:
                        if k == "c":
                            eobj.wait_ge(csem[e2], cnt[e2][i2])
                        else:
                            eobj.wait_ge(dsem[e2][i2 % NDSEM], 16 * (i2 // NDSEM + 1))
                    ins = fn(eobj)
                    if didx >= 0:
                        ins.then_inc(dsem[en][didx % NDSEM], 16)
                    elif i in mk:
                        ins.then_inc(csem[en], 1)
                n = self.ndma[en]
                for s in range(NDSEM):
                    k = len(range(s, n, NDSEM))
                    if k:
                        eobj.wait_ge(dsem[en][s], 16 * k)

            block.tensor(lambda e: run("pe", e))
            block.scalar(lambda e: run("act", e))
            block.vector(lambda e: run("dve", e))
            block.gpsimd(lambda e: run("pool", e))
            block.sync(lambda e: run("sp", e))


def t5_bucket_np(rel):
    nb = 16
    max_exact = 8
    rel = rel.astype(np.int32)
    ret = (rel > 0).astype(np.int32) * nb
    n = np.abs(rel)
    nf = np.maximum(n, 1).astype(np.float32)
    large = max_exact + (np.log(nf / np.float32(max_exact)) / np.float32(math.log(128 / max_exact))
                         * np.float32(nb - max_exact)).astype(np.int32)
    large = np.minimum(large, nb - 1)
    return ret + np.where(n < max_exact, n, large)


def make_consts():
    c = {}
    c["ident_f"] = np.eye(128, dtype=np.float32)
    c["antiid_f"] = np.ascontiguousarray(np.eye(128, dtype=np.float32)[::-1])
    r = np.arange(128)
    c["tri_incl"] = (r[:, None] <= r[None, :]).astype(np.float32)
    c["tri_strict"] = (r[:, None] < r[None, :]).astype(np.float32)
    blk = np.zeros((128, 128), np.float32)
    blk[:64, :64] = 1
    blk[64:, 64:] = 1
    c["blk64"] = blk
    rot = np.zeros((128, 128), np.float32)
    for i in range(64):
        rot[2 * i, 2 * i + 1] = -1.0
        rot[2 * i + 1, 2 * i] = 1.0
    c["rotT"] = np.ascontiguousarray(rot.T)
    n_tok = SEQ
    t = np.arange(n_tok)
    meta_pos = np.arange(NM) - NM
    row = np.concatenate([meta_pos, t // 64]).astype(np.float32)
    col = np.concatenate([meta_pos, t % 64]).astype(np.float32)
    half = 32
    inv = (np.float32(10000.0) ** (-np.arange(0, half, 2, dtype=np.float32) / np.float32(half))).astype(np.float32)
    ang = np.concatenate([row[:, None] * inv, col[:, None] * inv], axis=-1).astype(np.float32)
    cs = np.cos(ang).astype(np.float32)
    sn = np.sin(ang).astype(np.float32)
    c["ropeC"] = np.ascontiguousarray(np.tile(np.repeat(cs.T, 2, axis=0), (2, 1)))
    c["ropeS"] = np.ascontiguousarray(np.tile(np.repeat(sn.T, 2, axis=0), (2, 1)))
    rel = np.arange(512) - 256
    bk = t5_bucket_np(rel)
    oh = np.zeros((33, 512), np.float32)
    for i in range(512):
        if abs(rel[i]) > 128:
            oh[32, i] = 1.0
        else:
            oh[bk[i], i] = 1.0
    c["t5oh"] = oh
    rm = np.ones((128, 512), np.float32)
    rm[:, ::128] = 0.0
    c["resetm"] = rm
    hm = np.zeros((128, 4), np.float32)
    for h in range(4):
        hm[h * 32:(h + 1) * 32, h] = 1.0
    c["headmask"] = hm
    c["tri_ge"] = (r[:, None] >= r[None, :]).astype(np.float32)
    return c


CONST_SHAPES = {k: v.shape for k, v in make_consts().items()}

PARAM_SHAPES = {
    "meta_tokens": (16, 1024), "rel_bias": (32, 4), "norm_mix_w": (2, 1024), "norm_ffn_w": (2, 1024),
    "w_in": (2, 1024, 2856), "ssd_conv_w": (2, 5, 768), "ssd_conv_b": (2, 768), "ssd_dt_bias": (2, 2, 4),
    "ssd_a_log": (2, 2, 4), "ssd_d": (2, 4), "ssd_norm_w": (2, 256), "gla_gate_w2": (2, 2, 16, 128),
    "gla_gate_b": (2, 2, 128), "gla_norm_w": (2, 64), "swa_sink": (2, 4), "gqa_q_norm_w": (2, 64),
    "gqa_k_norm_w": (2, 64), "w_out": (2, 1024, 1024), "ffn_w_gate": (1, 1024, 2816),
    "ffn_w_up": (1, 1024, 2816), "ffn_w_down": (1, 2816, 1024), "moe_router": (1, 1024, 8),
    "moe_w_gate": (1, 8, 1024, 3584), "moe_w_up": (1, 8, 1024, 3584), "moe_w_down": (1, 8, 3584, 1024),
    "final_norm_w": (1024,),
}


class KB:
    def __init__(self, dbg_out=(), dbg_in=()):
        self.nc = bass.Bass("TRN2", target_bir_lowering=False)
        self.P = Prog(self.nc)
        self.dbg_out = set(dbg_out)
        self.dbg_in = set(dbg_in)
        nc = self.nc
        self.din = {}
        self.din["x"] = nc.dram_tensor("x", [SEQ, D], F32, kind="ExternalInput").ap()
        for k, s in PARAM_SHAPES.items():
            self.din[k] = nc.dram_tensor(k, list(s), F32, kind="ExternalInput").ap()
        for k, s in CONST_SHAPES.items():
            self.din[k] = nc.dram_tensor("c_" + k, list(s), F32, kind="ExternalInput").ap()
        self.out = nc.dram_tensor("out", [SEQ, D], F32, kind="ExternalOutput").ap()
        self.bout = Buf("out")
        self.scr = {}
        self.sb = {}
        self._scratch("projF", [NFM, 128, L], F32, [NFM])
        self._scratch("vtok", [L, TMW], F32, [1])
        self._scratch("mixT", [8, 128, L], BF16, [8])
        self._scratch("hbuf", [L, D], F32, [1])
        self._scratch("t5tab", [4, 512], F32, [1])
        self.const_bufs = {}

    def _scratch(self, name, shape, dt, nb):
        kind = None
        if name in self.dbg_out:
            kind = "ExternalOutput"
        elif name in self.dbg_in:
            kind = "ExternalInput"
        if kind:
            t = self.nc.dram_tensor(name, shape, dt, kind=kind)
        else:
            t = self.nc.dram_tensor(name, shape, dt)
        self.scr[name] = t.ap()
        self.sb[name] = [Buf(name + str(i)) for i in range(nb[0])]

    def nm(self, n):
        self._uid = getattr(self, "_uid", 0) + 1
        return "%s_%d" % (n, self._uid)

    def dma(self, q, out, in_, reads=(), writes=()):
        self.P.op(q, lambda e: e.dma_start(out=out, in_=in_), reads, writes, dma=True)

    def mm(self, out, lhsT, rhs, start, stop, reads=(), writes=()):
        self.P.op("pe", lambda e: e.matmul(out, lhsT=lhsT, rhs=rhs, start=start, stop=stop), reads, writes)

    def tr(self, out, in_, ident, reads=(), writes=()):
        self.P.op("pe", lambda e: e.transpose(out, in_, ident), reads, writes)

    def act(self, out, in_, func, reads=(), writes=(), bias=0.0, scale=1.0, accum_out=None):
        if accum_out is None:
            self.P.op("act", lambda e: e.activation(out=out, in_=in_, func=func, bias=bias, scale=scale), reads, writes)
        else:
            self.P.op("act", lambda e: e.activation(out=out, in_=in_, func=func, bias=bias, scale=scale,
                                                    accum_out=accum_out), reads, writes)

    def tt(self, eng, out, in0, in1, op, reads=(), writes=()):
        self.P.op(eng, lambda e: e.tensor_tensor(out=out, in0=in0, in1=in1, op=op), reads, writes)

    def ts(self, eng, out, in0, s1, op0, s2=None, op1=None, reads=(), writes=()):
        if op1 is None:
            self.P.op(eng, lambda e: e.tensor_scalar(out=out, in0=in0, scalar1=s1, scalar2=None, op0=op0), reads, writes)
        else:
            self.P.op(eng, lambda e: e.tensor_scalar(out=out, in0=in0, scalar1=s1, scalar2=s2, op0=op0, op1=op1),
                      reads, writes)

    def stt(self, eng, out, in0, scalar, in1, op0, op1, reads=(), writes=()):
        self.P.op(eng, lambda e: e.scalar_tensor_tensor(out=out, in0=in0, scalar=scalar, in1=in1, op0=op0, op1=op1),
                  reads, writes)

    def cp(self, eng, out, in_, reads=(), writes=()):
        if eng == "act":
            self.P.op("act", lambda e: e.copy(out=out, in_=in_), reads, writes)
        else:
            self.P.op(eng, lambda e: e.tensor_copy(out=out, in_=in_), reads, writes)

    def memset(self, eng, ap, val, writes=()):
        self.P.op(eng, lambda e: e.memset(ap, val), (), writes)

    def recip(self, out, in_, reads=(), writes=()):
        self.P.op("dve", lambda e: e.reciprocal(out=out, in_=in_), reads, writes)

    def rstd_from_ss(self, dst, src, n_feat, reads, writes):
        self.act(dst, src, AF.Ln, list(reads) + [self.b_eps], writes, bias=self.eps_ap(dst), scale=1.0 / n_feat)
        self.act(dst, dst, AF.Exp, writes, writes, scale=-0.5)

    def eps_ap(self, like):
        np_ = like.shape[0]
        return self.epsT[0:np_, 0:1] if like.base_partition() == 0 else self.epsT[like.base_partition():like.base_partition() + np_, 0:1]

    def load_consts(self, st):
        nc = self.nc
        S = lambda n, s, d=F32: st.enter_context(nc.sbuf_tensor(self.nm(n), s, d))
        self.c = {}
        for k in ("ident_f", "antiid_f", "tri_incl", "tri_strict", "tri_ge", "blk64", "rotT"):
            t = S("sc_" + k, [128, 128])
            b = Buf(k)
            self.dma("sp", t[:], self.din[k][:, :], (), [b])
            b.const = True
            self.c[k] = t
            self.const_bufs[k] = b
        self.identb = S("identb", [128, 128], BF16)
        self.b_identb = Buf("identb")
        self.cp("dve", self.identb[:], self.c["ident_f"][:], [self.const_bufs["ident_f"]], [self.b_identb])
        self.b_identb.const = True
        self.epsT = S("epsT", [128, 1])
        self.b_eps = Buf("eps")
        self.memset("dve", self.epsT[:], EPS, [self.b_eps])
        self.b_eps.const = True
        self.ones_f = S("ones_f", [128, 128])
        self.b_ones = Buf("ones")
        self.memset("dve", self.ones_f[:], 1.0, [self.b_ones])
        self.b_ones.const = True

    def hsrc(self, layer, t0, nt):
        if layer == 0:
            if t0 < NM:
                return self.din["meta_tokens"][t0:t0 + nt, :], None
            return self.din["x"][t0 - NM:t0 - NM + nt, :], None
        return self.scr["hbuf"][t0:t0 + nt, :], self.sb["hbuf"][0]

    def norm_T(self, xt, nt, wrow, ub, uT, col0, pts, bx, bub, bpts, buT, bw, junk, ss, bss, keep_f32=None):
        self.norm_p1(xt, nt, wrow, ub, bx, bub, bw, junk, ss, bss, keep_f32)
        self.norm_p2(nt, ub, uT, col0, pts, bub, bpts, buT)

    def norm_p1(self, xt, nt, wrow, ub, bx, bub, bw, junk, ss, bss, keep_f32=None):
        self.act(junk[:nt, :], xt[:nt, :], AF.Square, [bx], [bss], accum_out=ss[:nt, 0:1])
        self.rstd_from_ss(ss[:nt, 0:1], ss[:nt, 0:1], D, [bss], [bss])
        self.stt("dve", ub[:nt, :], xt[:nt, :], ss[:nt, 0:1], wrow[:nt, :], ALU.mult, ALU.mult, [bx, bss, bw], [bub])
        if keep_f32 is not None:
            kf, bkf = keep_f32
            self.stt("dve", kf[:nt, :], xt[:nt, :], ss[:nt, 0:1], wrow[:nt, :], ALU.mult, ALU.mult, [bx, bss, bw], [bkf])

    def norm_p2(self, nt, ub, uT, col0, pts, bub, bpts, buT):
        for k4 in range(2):
            pt = pts[k4]
            for k in range(4):
                kk = k4 * 4 + k
                self.tr(pt[:, k * 128:k * 128 + nt], ub[:nt, kk * 128:(kk + 1) * 128], self.identb[:nt, :nt],
                        [bub, self.b_identb], [bpts[k4]])
            src = pt[:].rearrange("p (k t) -> p k t", k=4)[:, :, :nt]
            dst = uT[:, k4 * 4:(k4 + 1) * 4, col0:col0 + nt]
            self.cp("act" if k4 == 0 else "dve", dst, src, [bpts[k4]], [buT])

    def phase_A(self, layer):
        nc, P = self.nc, self.P
        P.barrier()
        with contextlib.ExitStack() as st:
            S = lambda n, s, d=F32: st.enter_context(nc.sbuf_tensor(self.nm(n), s, d))
            PS = lambda n, s, d=F32: st.enter_context(nc.psum_tensor(self.nm(n), s, d))
            Wf = S("A_Wf", [128, 8, NFM * 128], BF16)
            Wv = S("A_Wv", [128, 8, TMW], BF16)
            bWf = [Buf("Wf%d" % i) for i in range(NFM)]
            bWv = Buf("Wv")
            self.memset("pool", Wf[:, :, FM_IDX["ga"] * 128:(FM_IDX["ga"] + 1) * 128], 0.0, [bWf[FM_IDX["ga"]]])
            win = self.din["w_in"][layer].rearrange("(k p) c -> p k c", p=128)
            for i, (name, segs) in enumerate(FM_TILES):
                off = i * 128
                for (c0, n) in segs:
                    self.dma("pool", Wf[:, :, off:off + n], win[:, :, c0:c0 + n], (), [bWf[i]])
                    off += n
            off = 0
            for (c0, n) in TM_SEGS:
                self.dma("pool", Wv[:, :, off:off + n], win[:, :, c0:c0 + n], (), [bWv])
                off += n
            wrow = S("A_wrow", [128, D])
            bw = Buf("wrow")
            self.dma("sp", wrow[:], self.din["norm_mix_w"][layer:layer + 1, :].partition_broadcast(128), (), [bw])
            NX = 8
            xts = [S("A_xt%d" % i, [128, D]) for i in range(NX)]
            bxs = [Buf("xt%d" % i) for i in range(NX)]
            junk = S("A_junk", [128, D], BF16)
            ub = [S("A_ub%d" % i, [128, D], BF16) for i in range(NX)]
            bub = [Buf("ub%d" % i) for i in range(NX)]
            ss = [S("A_ss%d" % i, [128, 1]) for i in range(NX)]
            bss = [Buf("ss%d" % i) for i in range(NX)]
            uT = [S("A_uT%d" % i, [128, 8, 512], BF16) for i in range(2)]
            buT = [Buf("uT%d" % i) for i in range(2)]
            pts = [PS("A_pt%d" % i, [128, 512], BF16) for i in range(2)]
            bpts = [Buf("pt%d" % i) for i in range(2)]
            pmm = [PS("A_pm%d" % i, [128, 512]) for i in range(4)]
            bpmm = [Buf("pm%d" % i) for i in range(4)]
            stg = [S("A_stg%d" % i, [128, 512]) for i in range(4)]
            bstg = [Buf("stg%d" % i) for i in range(4)]
            pv8 = PS("A_pv8", [128, 8])
            bpv8 = Buf("pv8")
            stv = [S("A_stv%d" % i, [128, TMW]) for i in range(2)]
            bstv = [Buf("stv%d" % i) for i in range(2)]
            im = 0
            slot = {}
            cnt = [0]

            def pre(gi):
                g0, gn = GROUPS[gi]
                for (t0, nt) in chunks_in(g0, gn):
                    k = cnt[0] % NX
                    cnt[0] += 1
                    slot[(gi, t0)] = k
                    src, sbuf_ = self.hsrc(layer, t0, nt)
                    self.dma("sp", xts[k][:nt, :], src, [sbuf_] if sbuf_ else (), [bxs[k]])
                    self.norm_p1(xts[k], nt, wrow, ub[k], bxs[k], bub[k], bw, junk, ss[k], bss[k])

            def trs(gi):
                g0, gn = GROUPS[gi]
                for (t0, nt) in chunks_in(g0, gn):
                    k = slot[(gi, t0)]
                    self.norm_p2(nt, ub[k], uT[gi % 2], t0 - g0, pts, bub[k], bpts, buT[gi % 2])

            def mms(gi):
                nonlocal im
                g0, gn = GROUPS[gi]
                u = uT[gi % 2]
                bu = buT[gi % 2]
                subs = chunks_in(g0, gn)
                for f in range(NFM):
                    pm, bpm = pmm[im % 4], bpmm[im % 4]
                    sg, bsg = stg[im % 4], bstg[im % 4]
                    for k in range(8):
                        self.mm(pm[:, :gn], Wf[:, k, f * 128:(f + 1) * 128], u[:, k, :gn], k == 0, k == 7, [bWf[f], bu], [bpm])
                    self.cp("act" if im % 2 == 0 else "dve", sg[:, :gn], pm[:, :gn], [bpm], [bsg])
                    self.dma("sp", self.scr["projF"][f, :, g0:g0 + gn], sg[:, :gn], [bsg], [self.sb["projF"][f]])
                    im += 1
                for (t0, nt) in subs:
                    pm, bpm = pmm[im % 4], bpmm[im % 4]
                    sv_, bsv = stv[im % 2], bstv[im % 2]
                    c0 = t0 - g0
                    for k in range(8):
                        self.mm(pm[:nt, :], u[:, k, c0:c0 + nt], Wv[:, k, 0:512], k == 0, k == 7, [bWv, bu], [bpm])
                    for k in range(8):
                        self.mm(pv8[:nt, :], u[:, k, c0:c0 + nt], Wv[:, k, 512:520], k == 0, k == 7, [bWv, bu], [bpv8])
                    self.cp("act", sv_[:nt, 0:512], pm[:nt, :], [bpm], [bsv])
                    self.cp("dve", sv_[:nt, 512:520], pv8[:nt, :], [bpv8], [bsv])
                    self.dma("sp", self.scr["vtok"][t0:t0 + nt, :], sv_[:nt, :], [bsv], [self.sb["vtok"][0]])
                    im += 1

            NGR = len(GROUPS)
            pre(0)
            trs(0)
            for gi in range(NGR):
                if gi + 1 < NGR:
                    pre(gi + 1)
                mms(gi)
                if gi + 1 < NGR:
                    trs(gi + 1)

    def load_vaug(self, S, name, vcol, sink_row=None):
        va = S(name, [128, 33, 2, 65], BF16)
        b = Buf(name)
        self.memset("pool", va[:], 0.0, [b])
        self.memset("pool", va[:, :, :, 64:65], 1.0, [b])
        if sink_row is not None:
            self.memset("pool", va[:, 0, :, 64:65], 0.0, [b])
            self.memset("pool", va[0:NM, 0, :, 64:65], 1.0, [b])
            self.memset("pool", va[sink_row:sink_row + 1, 0, :, 64:65], 1.0, [b])
        vt = self.scr["vtok"]
        for g in range(2):
            src = vt[NM:, vcol + g * 64:vcol + (g + 1) * 64].rearrange("(c p) d -> p c d", p=128)
            self.dma("pool", va[:, 1:33, g, 0:64], src, [self.sb["vtok"][0]], [b])
            self.dma("pool", va[0:NM, 0, g, 0:64], vt[0:NM, vcol + g * 64:vcol + (g + 1) * 64], [self.sb["vtok"][0]], [b])
        return va, b

    def attn_finish(self, ps_o, bps_o, n, rden, brden, ps_b, bps_b, osb, bosb, stg, bstg, dst, bdst):
        self.attn_fin_a(ps_o, bps_o, n, rden, brden, osb, bosb)
        self.attn_fin_b(n, rden, brden, ps_b, bps_b, osb, bosb, stg, bstg, dst, bdst)

    def attn_fin_a(self, ps_o, bps_o, n, rden, brden, osb, bosb):
        self.recip(rden[64:65, :n], ps_o[64:65, :n], [bps_o], [brden])
        self.cp("dve", osb[0:64, :n], ps_o[0:64, :n], [bps_o], [bosb])

    def attn_fin_b(self, n, rden, brden, ps_b, bps_b, osb, bosb, stg, bstg, dst, bdst):
        self.mm(ps_b[0:64, :n], self.ones_f[64:65, 0:64], rden[64:65, :n], True, True, [brden, self.b_ones], [bps_b])
        self.tt("dve", stg[0:64, :n], osb[0:64, :n], ps_b[0:64, :n], ALU.mult, [bosb, bps_b], [bstg])
        self.dma("sp", dst, stg[0:64, :n], [bstg], [bdst])

    def phase_G2(self, layer):
        nc, P = self.nc, self.P
        P.barrier()
        with contextlib.ExitStack() as st:
            S = lambda n, s, d=F32: st.enter_context(nc.sbuf_tensor(self.nm(n), s, d))
            PS = lambda n, s, d=F32: st.enter_context(nc.psum_tensor(self.nm(n), s, d))
            qr = [S("G_qr%d" % g, [128, L], BF16) for g in range(2)]
            kr = [S("G_kr%d" % g, [128, L], BF16) for g in range(2)]
            bqk = {}
            va, bva = self.load_vaug(S, "G_va", V_AV)
            wn = S("G_wn", [128, 2])
            bwn = Buf("wn")
            for c, nm in enumerate(("gqa_q_norm_w", "gqa_k_norm_w")):
                src = self.din[nm][layer:layer + 1, :].rearrange("o d -> d o")
                self.dma("sp", wn[0:64, c:c + 1], src, (), [bwn])
                self.dma("sp", wn[64:128, c:c + 1], src, (), [bwn])
            Ct = [S("G_C%d" % i, [128, 512]) for i in range(2)]
            St = [S("G_S%d" % i, [128, 512]) for i in range(2)]
            bCS = [Buf("CS%d" % i) for i in range(2)]
            xs = [S("G_x%d" % i, [128, 512]) for i in range(2)]
            bxs = [Buf("x%d" % i) for i in range(2)]
            mk2 = lambda nme: ([S("G_%s%d" % (nme, i), [128, 512]) for i in range(3)], [Buf("%s%d" % (nme, i)) for i in range(3)])
            sq2, bsq2 = mk2("sq"); rs2, brs2 = mk2("rs"); xn2, bxn2 = mk2("xn"); t12, bt12 = mk2("t1"); t22, bt22 = mk2("t2")
            NB = 6
            SK = 2
            pss2 = [PS("G_ps%d" % i, [128, 1024]) for i in range(3)]
            bpss2 = [Buf("ps%d" % i) for i in range(3)]
            pss = [pss2[i // 2][:, (i % 2) * 512:(i % 2 + 1) * 512] for i in range(NB)]
            bpss = [bpss2[i // 2] for i in range(NB)]
            pso = [PS("G_po%d" % i, [128, 512]) for i in range(2)]
            bpso = [Buf("po%d" % i) for i in range(2)]
            pn = [pss[3], pss[4]]
            bpn = [bpss[3], bpss[4]]
            xs3, bxs3 = mk2("xx")
            tiles = [("aq0", qr[0], 0), ("aq1", qr[1], 0), ("ak0", kr[0], 1), ("ak1", kr[1], 1)]
            work = []
            for gi, (g0, gn) in enumerate(GROUPS):
                for (nm, dst, wc) in tiles:
                    work.append((gi, g0, gn, nm, dst, wc))

            def st_a(it):
                gi, g0, gn, nm, dst, wc = work[it]
                i3 = it % 3
                if nm == "aq0":
                    C_, S_, bcs = Ct[gi % 2], St[gi % 2], bCS[gi % 2]
                    self.dma("sp", C_[:, :gn], self.din["ropeC"][:, g0:g0 + gn], (), [bcs])
                    self.dma("sp", S_[:, :gn], self.din["ropeS"][:, g0:g0 + gn], (), [bcs])
                f = FM_IDX[nm]
                x, bx = xs3[i3], bxs3[i3]
                self.dma("sp", x[:, :gn], self.scr["projF"][f, :, g0:g0 + gn], [self.sb["projF"][f]], [bx])
                self.act(sq2[i3][:, :gn], x[:, :gn], AF.Square, [bx], [bsq2[i3]])
                p1, bp1 = pn[it % 2], bpn[it % 2]
                self.mm(p1[:, :gn], self.c["blk64"][:], sq2[i3][:, :gn], True, True, [bsq2[i3], self.const_bufs["blk64"]], [bp1])
                self.rstd_from_ss(rs2[i3][:, :gn], p1[:, :gn], 64, [bp1], [brs2[i3]])
                self.stt("dve", xn2[i3][:, :gn], x[:, :gn], wn[:, wc:wc + 1], rs2[i3][:, :gn], ALU.mult, ALU.mult,
                         [bx, bwn, brs2[i3]], [bxn2[i3]])

            def st_b(it):
                gi, g0, gn, nm, dst, wc = work[it]
                i3 = it % 3
                self.mm(pss[i3][:, :gn], self.c["rotT"][:], xn2[i3][:, :gn], True, True, [bxn2[i3], self.const_bufs["rotT"]], [bpss[i3]])

            def st_c(it):
                gi, g0, gn, nm, dst, wc = work[it]
                i3 = it % 3
                C_, S_, bcs = Ct[gi % 2], St[gi % 2], bCS[gi % 2]
                bd = bqk.setdefault((nm, gi), Buf(nm + str(gi)))
                self.tt("dve", t12[i3][:, :gn], xn2[i3][:, :gn], C_[:, :gn], ALU.mult, [bxn2[i3], bcs], [bt12[i3]])
                self.tt("dve", t22[i3][:, :gn], pss[i3][:, :gn], S_[:, :gn], ALU.mult, [bpss[i3], bcs], [bt22[i3]])
                self.tt("pool", dst[:, g0:g0 + gn], t12[i3][:, :gn], t22[i3][:, :gn], ALU.add, [bt12[i3], bt22[i3]], [bd])

            NW = len(work)
            for it in range(NW + 2):
                if it < NW:
                    st_a(it)
                if 1 <= it <= NW:
                    st_b(it - 1)
                if it >= 2:
                    st_c(it - 2)
            pT2 = [S("G_pT%d" % i, [128, 1024], BF16) for i in range(3)]
            bpT2 = [Buf("pT%d" % i) for i in range(3)]
            rden = [S("G_rden%d" % i, [128, 512]) for i in range(2)]; brden = [Buf("rden%d" % i) for i in range(2)]
            osb = [S("G_osb%d" % i, [128, 512]) for i in range(2)]; bosb = [Buf("osb%d" % i) for i in range(2)]
            stg = [S("G_stg%d" % i, [128, 512], BF16) for i in range(2)]
            bstg = [Buf("stg%d" % i) for i in range(2)]
            kbufs = {g: [bqk[("ak%d" % g, gi)] for gi in range(len(GROUPS))] for g in range(2)}
            items = [(gi, g, kt) for gi in range(len(GROUPS)) for g in range(2) for kt in range(len(CHUNKS))]
            pend_b = []
            NI = 3

            def stage_qk(i3, it):
                gi, g, kt = it
                g0, gn = GROUPS[gi]
                k0, kn = CHUNKS[kt]
                kb = kbufs[g][0] if kt == 0 else kbufs[g][1 + (kt - 1) // 4]
                j = i3 % NI
                for r in range(2):
                    hp = r * 64
                    self.mm(pss2[j][:kn, r * 512:r * 512 + gn], kr[g][hp:hp + 64, k0:k0 + kn], qr[g][hp:hp + 64, g0:g0 + gn],
                            True, True, [kb, bqk[("aq%d" % g, gi)]], [bpss2[j]])
                v3 = lambda t: t[:kn, :].rearrange("p (r q) -> p r q", r=2)[:, :, :gn]
                self.act(v3(pT2[j]), v3(pss2[j]), AF.Exp, [bpss2[j]], [bpT2[j]], scale=0.125)

            def stage_pv(i3, it):
                gi, g, kt = it
                g0, gn = GROUPS[gi]
                k0, kn = CHUNKS[kt]
                j = i3 % NI
                for r in range(2):
                    self.mm(pso[r][0:65, :gn], va[:kn, kt, g, :], pT2[j][:kn, r * 512:r * 512 + gn], kt == 0, kt == 32,
                            [bva, bpT2[j]], [bpso[r]])
                if kt == 32:
                    for r in range(2):
                        hp = r * 64
                        self.attn_fin_a(pso[r], bpso[r], gn, rden[r], brden[r], osb[r], bosb[r])
                        dst = self.scr["mixT"][6 + g, hp:hp + 64, g0:g0 + gn]
                        pend_b.append((i3 + 1, (gn, rden[r], brden[r], pso[r], bpso[r], osb[r], bosb[r],
                                                stg[r], bstg[r], dst, self.sb["mixT"][6 + g])))

            N = len(items)
            for i3 in range(N + SK):
                if i3 < N:
                    stage_qk(i3, items[i3])
                if i3 >= SK:
                    stage_pv(i3 - SK, items[i3 - SK])
                while pend_b and pend_b[0][0] <= i3:
                    self.attn_fin_b(*pend_b.pop(0)[1])
            while pend_b:
                self.attn_fin_b(*pend_b.pop(0)[1])

    def setup_swa_bias(self, st):
        nc, P = self.nc, self.P
        S = lambda n, s, d=F32: st.enter_context(nc.sbuf_tensor(self.nm(n), s, d))
        self.BT1 = [S("BT1_%d" % h, [128, 512]) for h in range(4)]
        self.BT0 = [S("BT0_%d" % h, [128, 512]) for h in range(4)]
        self.BTq = [S("BTq_%d" % h, [128, 32]) for h in range(4)]
        self.bBT = [Buf("BT%d" % h) for h in range(4)]
        self.zrow = S("zrow", [128, 128])
        self.b_zrow = Buf("zrow")
        self.memset("pool", self.zrow[:], 0.0, [self.b_zrow])
        self.b_zrow.const = True
        with contextlib.ExitStack() as st2:
            S2 = lambda n, s, d=F32: st2.enter_context(nc.sbuf_tensor(self.nm(n), s, d))
            PS2 = lambda n, s, d=F32: st2.enter_context(nc.psum_tensor(self.nm(n), s, d))
            relb = S2("sw_relb", [33, 4]); brelb = Buf("relb")
            oh = S2("sw_oh", [33, 512]); boh = Buf("oh")
            tab = S2("sw_tab", [4, 512]); btab = Buf("tab")
            t15 = S2("sw_t15", [16, 4]); bt15 = Buf("t15")
            self.memset("dve", relb[32:33, :], NEG, [brelb])
            self.dma("sp", relb[0:32, :], self.din["rel_bias"][:, :], (), [brelb])
            self.dma("sp", oh[:], self.din["t5oh"][:, :], (), [boh])
            self.dma("sp", t15[:], self.din["rel_bias"][15:16, :].partition_broadcast(16), (), [bt15])
            pt = PS2("sw_pt", [128, 512]); bpt = Buf("pt")
            self.mm(pt[0:4, :], relb[:, :], oh[:, :], True, True, [brelb, boh], [bpt])
            self.cp("act", tab[:], pt[0:4, :], [bpt], [btab])
            btd = self.sb["t5tab"][0]
            self.dma("sp", self.scr["t5tab"][:, :], tab[:], [btab], [btd])
            hk = [S2("sw_hk%d" % i, [128, 128]) for i in range(2)]
            bhk = [Buf("hk%d" % i) for i in range(2)]
            pm = [PS2("sw_pm%d" % i, [128, 128]) for i in range(2)]
            bpm = [Buf("pm%d" % i) for i in range(2)]
            n = 0
            t5t = self.scr["t5tab"].tensor
            for h in range(4):
                b = self.bBT[h]
                self.memset("pool", self.BT1[h][:, 384:512], NEG, [b])
                self.memset("pool", self.BT0[h][:, 384:512], NEG, [b])
                self.memset("pool", self.BTq[h][:, 0:16], NEG, [b])
                self.ts("dve", self.BT1[h][0:16, 384:512], self.zrow[0:16, :], t15[0:16, h:h + 1], ALU.add,
                        reads=[bt15, self.b_zrow], writes=[b])
                for c in (-128, 0, 128, -16, 16):
                    i = n % 2
                    src = bass.AP(t5t, h * 512 + c + 129, [[1, 128], [1, 128]])
                    self.dma("sp", hk[i][:], src, [btd], [bhk[i]])
                    self.mm(pm[i][:], hk[i][:], self.c["antiid_f"][:], True, True, [bhk[i], self.const_bufs["antiid_f"]], [bpm[i]])
                    if c in (-128, 0, 128):
                        e = c // 128 + 1
                        self.cp("act", self.BT1[h][:, e * 128:(e + 1) * 128], pm[i][:], [bpm[i]], [b])
                        self.cp("dve", self.BT0[h][:, e * 128:(e + 1) * 128], pm[i][:], [bpm[i]], [b])
                        if c == 0:
                            self.cp("dve", self.BTq[h][0:16, 0:16], pm[i][0:16, 0:16], [bpm[i]], [b])
                    elif c == -16:
                        self.cp("dve", self.BT0[h][0:16, 384:512], pm[i][0:16, :], [bpm[i]], [b])
                    else:
                        self.cp("dve", self.BTq[h][:, 16:32], pm[i][:, 0:16], [bpm[i]], [b])
                    n += 1
            P.barrier()

    def phase_SWA(self, layer):
        nc, P = self.nc, self.P
        P.barrier()
        SINK = 32
        with contextlib.ExitStack() as st:
            S = lambda n, s, d=F32: st.enter_context(nc.sbuf_tensor(self.nm(n), s, d))
            PS = lambda n, s, d=F32: st.enter_context(nc.psum_tensor(self.nm(n), s, d))
            self.setup_swa_bias(st)
            qT = [S("W_q%d" % g, [128, L], BF16) for g in range(2)]
            kT = [S("W_k%d" % g, [128, L], BF16) for g in range(2)]
            bq = [Buf("q%d" % g) for g in range(2)]
            bk = [Buf("k%d" % g) for g in range(2)]
            km = [S("W_km%d" % g, [128, 128], BF16) for g in range(2)]
            bkm = [Buf("km%d" % g) for g in range(2)]
            zk = S("W_zk", [128, 128], BF16); bzk = Buf("zk")
            self.memset("pool", zk[:], 0.0, [bzk])
            for g in range(2):
                fq, fk = FM_IDX["sq%d" % g], FM_IDX["sk%d" % g]
                self.dma("pool", qT[g][:], self.scr["projF"][fq, :, :], [self.sb["projF"][fq]], [bq[g]])
                self.dma("pool", kT[g][:], self.scr["projF"][fk, :, :], [self.sb["projF"][fk]], [bk[g]])
                self.memset("pool", km[g][:], 0.0, [bkm[g]])
                self.cp("pool", km[g][:, 0:NM], kT[g][:, 0:NM], [bk[g]], [bkm[g]])
            va, bva = self.load_vaug(S, "W_va", V_SV, sink_row=SINK)
            sinkt = S("W_sink", [128, 4]); bsink = Buf("sink")
            self.dma("sp", sinkt[SINK:SINK + 1, :], self.din["swa_sink"][layer:layer + 1, :], (), [bsink])
            for h in range(4):
                b = self.bBT[h]
                for (t, c0, n) in ((self.BT1[h], 384, 128), (self.BT0[h], 384, 128), (self.BTq[h], 0, 16)):
                    self.ts("dve", t[SINK:SINK + 1, c0:c0 + n], self.zrow[SINK:SINK + 1, 0:n],
                            sinkt[SINK:SINK + 1, h:h + 1], ALU.add, reads=[bsink, self.b_zrow], writes=[b])
            ND = 3
            pss = [PS("W_ps%d" % i, [128, 512]) for i in range(ND)]
            bpss = [Buf("ps%d" % i) for i in range(ND)]
            pso = [PS("W_po%d" % i, [128, 512]) for i in range(ND)]
            bpso = [Buf("po%d" % i) for i in range(ND)]
            psb = PS("W_pb", [128, 512]); bpsb = Buf("pb")
            sbs = [S("W_sb%d" % i, [128, 512]) for i in range(ND)]
            bsbs = [Buf("sb%d" % i) for i in range(ND)]
            pT = [S("W_pT%d" % i, [128, 512], BF16) for i in range(ND)]
            bpT = [Buf("pT%d" % i) for i in range(ND)]
            rden = [S("W_rden%d" % i, [128, 128]) for i in range(ND)]; brden = [Buf("rden%d" % i) for i in range(ND)]
            osb = [S("W_osb%d" % i, [128, 128]) for i in range(ND)]; bosb = [Buf("osb%d" % i) for i in range(ND)]
            stg = [S("W_stg%d" % i, [128, 128], BF16) for i in range(ND)]
            bstg = [Buf("stg%d" % i) for i in range(ND)]
            items = [(qb, g, r) for qb in range(-1, 32) for g in range(2) for r in range(2)]

            def stage_qk(it, item):
                qb, g, r = item
                h = 2 * g + r
                hp = r * 64
                i2 = it % ND
                ps_, bps_ = pss[i2], bpss[i2]
                sb_, bsb_ = sbs[i2], bsbs[i2]
                p_, bp_ = pT[i2], bpT[i2]
                if qb < 0:
                    qa = qT[g][hp:hp + 64, 0:NM]
                    self.mm(ps_[:, 0:16], km[g][hp:hp + 64, :], qa, True, True, [bkm[g], bq[g]], [bps_])
                    self.mm(ps_[:, 16:32], kT[g][hp:hp + 64, NM:NM + 128], qa, True, True, [bk[g], bq[g]], [bps_])
                    self.stt("dve", sb_[:, 0:32], ps_[:, 0:32], 0.125, self.BTq[h][:, :], ALU.mult, ALU.add,
                             [bps_, self.bBT[h]], [bsb_])
                    self.act(p_[:, 0:32], sb_[:, 0:32], AF.Exp, [bsb_], [bp_])
                else:
                    q0 = NM + 128 * qb
                    qa = qT[g][hp:hp + 64, q0:q0 + 128]
                    for e in (-1, 0, 1):
                        kb = qb + e
                        if 0 <= kb < 32:
                            self.mm(ps_[:, (e + 1) * 128:(e + 2) * 128], kT[g][hp:hp + 64, NM + 128 * kb:NM + 128 * kb + 128],
                                    qa, True, True, [bk[g], bq[g]], [bps_])
                        else:
                            self.mm(ps_[:, (e + 1) * 128:(e + 2) * 128], zk[hp:hp + 64, :], qa, True, True,
                                    [bzk, bq[g]], [bps_])
                    self.mm(ps_[:, 384:512], km[g][hp:hp + 64, :], qa, True, True, [bkm[g], bq[g]], [bps_])
                    BT = self.BT0[h] if qb == 0 else self.BT1[h]
                    self.stt("dve", sb_[:, :], ps_[:, :], 0.125, BT[:, :], ALU.mult, ALU.add,
                             [bps_, self.bBT[h]], [bsb_])
                    self.act(p_[:, :], sb_[:, :], AF.Exp, [bsb_], [bp_])

            def stage_pv(it, item):
                qb, g, r = item
                hp = r * 64
                i2 = it % ND
                p_, bp_ = pT[i2], bpT[i2]
                po, bpo = pso[i2], bpso[i2]
                if qb < 0:
                    self.mm(po[0:65, 0:NM], va[:, 0, g, :], p_[:, 0:16], True, False, [bva, bp_], [bpo])
                    self.mm(po[0:65, 0:NM], va[:, 1, g, :], p_[:, 16:32], False, True, [bva, bp_], [bpo])
                    nq, q0 = NM, 0
                else:
                    q0 = NM + 128 * qb
                    nq = 128
                    self.mm(po[0:65, 0:128], va[:, 0, g, :], p_[:, 384:512], True, False, [bva, bp_], [bpo])
                    valid = [e for e in (-1, 0, 1) if 0 <= qb + e < 32]
                    for j, e in enumerate(valid):
                        self.mm(po[0:65, 0:128], va[:, qb + e + 1, g, :], p_[:, (e + 1) * 128:(e + 2) * 128],
                                False, j == len(valid) - 1, [bva, bp_], [bpo])
                self.attn_fin_a(po, bpo, nq, rden[i2], brden[i2], osb[i2], bosb[i2])
                dst = self.scr["mixT"][4 + g, hp:hp + 64, q0:q0 + nq]
                return (nq, rden[i2], brden[i2], psb, bpsb, osb[i2], bosb[i2], stg[i2], bstg[i2], dst, self.sb["mixT"][4 + g])

            N = len(items)
            pend = {}
            for it in range(N + 2):
                if it < N:
                    stage_qk(it, items[it])
                if 1 <= it <= N:
                    pend[it - 1] = stage_pv(it - 1, items[it - 1])
                if it >= 2:
                    self.attn_fin_b(*pend.pop(it - 2))

    def phase_C(self, layer):
        nc, P = self.nc, self.P
        is_moe = (layer % 2 == 1)
        last = (layer == DEPTH - 1)
        with contextlib.ExitStack() as st0:
            lg = st0.enter_context(nc.sbuf_tensor(self.nm("C_lg"), [128, 17, 8], F32))
            comb = st0.enter_context(nc.sbuf_tensor(self.nm("C_comb"), [128, 17, 8], F32))
            self._phase_C_halves(layer, is_moe, last, lg, comb)

    def _phase_C_halves(self, layer, is_moe, last, lg, comb):
        nc, P = self.nc, self.P
        for (h0, hn) in HALVES:
            P.barrier()
            subs = chunks_in(h0, hn)
            nsub = len(subs)
            with contextlib.ExitStack() as st:
                S = lambda n, s, d=F32: st.enter_context(nc.sbuf_tensor(self.nm(n), s, d))
                acc = S("C_acc", [128, 17, D])
                bacc = [Buf("acc%d" % i) for i in range(17)]
                u2T = S("C_u2T", [128, 8, NM + 2048], BF16)
                bu2 = [Buf("u2T%d" % i) for i in range(17)]
                wrow = S("C_wrow", [128, D]); bw = Buf("wrow")
                self.dma("sp", wrow[:], self.din["norm_ffn_w"][layer:layer + 1, :].partition_broadcast(128), (), [bw])
                blg = Buf("lg")
                bcomb = Buf("comb")
                with contextlib.ExitStack() as st1:
                    S1 = lambda n, s, d=F32: st1.enter_context(nc.sbuf_tensor(self.nm(n), s, d))
                    PS1 = lambda n, s, d=F32: st1.enter_context(nc.psum_tensor(self.nm(n), s, d))
                    wout = S1("C_wout", [128, 8, D], BF16); bwout = Buf("wout")
                    self.dma("pool", wout[:], self.din["w_out"][layer].rearrange("(c p) d -> p c d", p=128), (), [bwout])
                    mx = [S1("C_mx%d" % i, [128, 8, 128], BF16) for i in range(2)]
                    bmx = [Buf("mx%d" % i) for i in range(2)]
                    junk = S1("C_junk", [128, D], BF16)
                    ub = [S1("C_ub%d" % i, [128, D], BF16) for i in range(2)]
                    bub = [Buf("ub%d" % i) for i in range(2)]
                    ss = [S1("C_ss%d" % i, [128, 1]) for i in range(2)]
                    bss = [Buf("ss%d" % i) for i in range(2)]
                    if is_moe:
                        pd = [PS1("C_pd0", [128, D])] * 2
                        bpd = [Buf("pd0")] * 2
                    else:
                        pd = [PS1("C_pd%d" % i, [128, D]) for i in range(2)]
                        bpd = [Buf("pd%d" % i) for i in range(2)]
                    pts = [PS1("C_pt%d" % i, [128, 512], BF16) for i in range(2)]
                    bpts = [Buf("pt%d" % i) for i in range(2)]
                    if is_moe:
                        ufs = [S1("C_uf%d" % i, [128, D]) for i in range(2)]; bufs_ = [Buf("uf%d" % i) for i in range(2)]
                        ufT = S1("C_ufT", [128, 8, 128]); bufT = Buf("ufT")
                        rt = S1("C_rt", [128, 8, NEXP]); brt = Buf("rt")
                        self.dma("sp", rt[:], self.din["moe_router"][0].rearrange("(k p) e -> p k e", p=128), (), [brt])
                        ptf = PS1("C_ptf", [128, D]); bptf = Buf("ptf")
                        pl = PS1("C_pl", [128, NEXP]); bpl = Buf("pl")
                        self.memset("dve", lg[:], 0.0, [blg])
                    def p1_stage(si):
                        t0, nt = subs[si]
                        a = acc[:, si, :]
                        ba = bacc[si]
                        src, sbuf_ = self.hsrc(layer, t0, nt)
                        self.dma("sp", a[:nt, :], src, [sbuf_] if sbuf_ else (), [ba])
                        m_, bm_ = mx[si % 2], bmx[si % 2]
                        self.dma("sp", m_[:, :, :nt], self.scr["mixT"][:, :, t0:t0 + nt].rearrange("c p t -> p c t"),
                                 self.sb["mixT"], [bm_])
                        p_, bp_ = pd[si % 2], bpd[si % 2]
                        for dh in range(2):
                            for c in range(8):
                                self.mm(p_[:nt, dh * 512:(dh + 1) * 512], m_[:, c, :nt], wout[:, c, dh * 512:(dh + 1) * 512],
                                        c == 0, c == 7, [bm_, bwout], [bp_])
                        self.tt("dve", a[:nt, :], a[:nt, :], p_[:nt, :], ALU.add, [ba, bp_], [ba])
                        kf = (ufs[si % 2], bufs_[si % 2]) if is_moe else None
                        self.norm_p1(a, nt, wrow, ub[si % 2], ba, bub[si % 2], bw, junk, ss[si % 2], bss[si % 2], keep_f32=kf)

                    def p2_stage(si):
                        t0, nt = subs[si]
                        self.norm_p2(nt, ub[si % 2], u2T, t0 - h0, pts, bub[si % 2], bpts, bu2[si])
                        if is_moe:
                            uf, buf_ = ufs[si % 2], bufs_[si % 2]
                            for k in range(8):
                                self.tr(ptf[:, k * 128:k * 128 + nt], uf[:nt, k * 128:(k + 1) * 128], self.c["ident_f"][:nt, :nt],
                                        [buf_, self.const_bufs["ident_f"]], [bptf])
                            self.cp("act", ufT[:, :, :nt], ptf[:].rearrange("p (k t) -> p k t", k=8)[:, :, :nt], [bptf], [bufT])
                            for k in range(8):
                                self.mm(pl[:nt, :], ufT[:, k, :nt], rt[:, k, :], k == 0, k == 7, [bufT, brt], [bpl])
                            self.cp("dve", lg[:nt, si, :], pl[:nt, :], [bpl], [blg])

                    p1_stage(0)
                    for si in range(nsub):
                        if si + 1 < nsub:
                            p1_stage(si + 1)
                        p2_stage(si)
                    if is_moe:
                        m1 = S1("C_m1", [128, 17]); bm1 = Buf("m1")
                        m2 = S1("C_m2", [128, 17]); bm2 = Buf("m2")
                        t3 = S1("C_t3", [128, 17, 8]); bt3 = Buf("t3")
                        t4 = S1("C_t4", [128, 17, 8]); bt4 = Buf("t4")
                        bc = lambda t: t[:].unsqueeze(2).to_broadcast([128, 17, 8])
                        P.op("dve", lambda e: e.tensor_reduce(out=m1[:], in_=lg[:], axis=AX.X, op=ALU.max), [blg], [bm1])
                        self.tt("dve", t3[:], lg[:], bc(m1), ALU.is_equal, [blg, bm1], [bt3])
                        self.stt("dve", t3[:], t3[:], -1e30, lg[:], ALU.mult, ALU.add, [bt3, blg], [bt3])
                        P.op("dve", lambda e: e.tensor_reduce(out=m2[:], in_=t3[:], axis=AX.X, op=ALU.max), [bt3], [bm2])
                        self.tt("dve", t3[:], lg[:], bc(m2), ALU.is_ge, [blg, bm2], [bt3])
                        self.tt("dve", t4[:], lg[:], bc(m1), ALU.subtract, [blg, bm1], [bt4])
                        self.act(t4[:], t4[:], AF.Exp, [bt4], [bt4])
                        self.tt("dve", t4[:], t4[:], t3[:], ALU.mult, [bt4, bt3], [bt4])
                        P.op("dve", lambda e: e.tensor_reduce(out=m1[:], in_=t4[:], axis=AX.X, op=ALU.add), [bt4], [bm1])
                        self.recip(m1[:], m1[:], [bm1], [bm1])
                        self.tt("dve", comb[:], t4[:], bc(m1), ALU.mult, [bt4, bm1], [bcomb])
                P.barrier()
                with contextlib.ExitStack() as st2:
                    S2 = lambda n, s, d=F32: st2.enter_context(nc.sbuf_tensor(self.nm(n), s, d))
                    PS2 = lambda n, s, d=F32: st2.enter_context(nc.psum_tensor(self.nm(n), s, d))
                    G = 4
                    Wg = [S2("F_wg%d" % i, [128, 8, G * 128], BF16) for i in range(2)]
                    Wu = [S2("F_wu%d" % i, [128, 8, G * 128], BF16) for i in range(2)]
                    Wd = [S2("F_wd%d" % i, [128, G, D], BF16) for i in range(2)]
                    bW = [Buf("W%d" % i) for i in range(2)]
                    aT = [S2("F_aT%d" % i, [128, G, 512], BF16) for i in range(2)]
                    baT = [Buf("aT%d" % i) for i in range(2)]
                    sg = [S2("F_sg%d" % i, [128, 512]) for i in range(2)]
                    bsg = [Buf("sg%d" % i) for i in range(2)]
                    pgu = [PS2("F_pgu%d" % i, [128, 512]) for i in range(4)]
                    bpgu = [Buf("pgu%d" % i) for i in range(4)]
                    py = [PS2("F_py%d" % i, [128, D]) for i in range(2)]
                    bpy = [Buf("py%d" % i) for i in range(2)]
                    if is_moe:
                        wsets = [(self.din["moe_w_gate"][0, e], self.din["moe_w_up"][0, e], self.din["moe_w_down"][0, e], MOE_F, e)
                                 for e in range(NEXP)]
                    else:
                        j = layer // 2
                        wsets = [(self.din["ffn_w_gate"][j], self.din["ffn_w_up"][j], self.din["ffn_w_down"][j], FFN_F, None)]
                    tgroups = [(g0, gn) for (g0, gn) in GROUPS if g0 >= h0 and g0 + gn <= h0 + hn]
                    nw = 0
                    ia = 0
                    ig = 0
                    iy = 0
                    for (wg_ap, wu_ap, wd_ap, F, e) in wsets:
                        wgr = wg_ap.rearrange("(k p) f -> p k f", p=128)
                        wur = wu_ap.rearrange("(k p) f -> p k f", p=128)
                        for f0 in range(0, F, G * 128):
                            fw = min(G * 128, F - f0)
                            nj = fw // 128
                            s_ = nw % 2
                            self.dma("pool", Wg[s_][:, :, :fw], wgr[:, :, f0:f0 + fw], (), [bW[s_]])
                            self.dma("pool", Wu[s_][:, :, :fw], wur[:, :, f0:f0 + fw], (), [bW[s_]])
                            self.dma("pool", Wd[s_][:, :nj, :], wd_ap[f0:f0 + fw, :].rearrange("(j p) d -> p j d", p=128), (), [bW[s_]])
                            nw += 1
                            def gate_up(g0, gn, a_, ba_):
                                nonlocal ig
                                c0 = g0 - h0
                                rb = [bu2[subs.index(s)] for s in chunks_in(g0, gn)]
                                for j in range(nj):
                                    pg, bpg = pgu[ig % 4], bpgu[ig % 4]
                                    pu, bpu = pgu[(ig + 1) % 4], bpgu[(ig + 1) % 4]
                                    s2, bs2 = sg[(ig // 2) % 2], bsg[(ig // 2) % 2]
                                    ig += 2
                                    for k in range(8):
                                        self.mm(pg[:, :gn], Wg[s_][:, k, j * 128:(j + 1) * 128], u2T[:, k, c0:c0 + gn], k == 0, k == 7,
                                                [bW[s_]] + rb, [bpg])
                                    for k in range(8):
                                        self.mm(pu[:, :gn], Wu[s_][:, k, j * 128:(j + 1) * 128], u2T[:, k, c0:c0 + gn], k == 0, k == 7,
                                                [bW[s_]] + rb, [bpu])
                                    self.act(s2[:, :gn], pg[:, :gn], AF.Silu, [bpg], [bs2])
                                    self.tt("dve", a_[:, j, :gn], s2[:, :gn], pu[:, :gn], ALU.mult, [bs2, bpu], [ba_])

                            def down(g0, gn, a_, ba_):
                                nonlocal iy
                                for (t0, nt) in chunks_in(g0, gn):
                                    si = subs.index((t0, nt))
                                    cs = t0 - g0
                                    p_, bp_ = py[iy % 2], bpy[iy % 2]
                                    iy += 1
                                    for j in range(nj):
                                        for dh in range(2):
                                            self.mm(p_[:nt, dh * 512:(dh + 1) * 512], a_[:, j, cs:cs + nt],
                                                    Wd[s_][:, j, dh * 512:(dh + 1) * 512], j == 0, j == nj - 1, [ba_, bW[s_]], [bp_])
                                    sc = 1.0 if e is None else comb[:nt, si, e:e + 1]
                                    rd = [bp_, bacc[si]] + ([] if e is None else [bcomb])
                                    self.stt("dve", acc[:nt, si, :], p_[:nt, :], sc, acc[:nt, si, :], ALU.mult, ALU.add, rd, [bacc[si]])

                            prev = None
                            for (g0, gn) in tgroups:
                                a_, ba_ = aT[ia % 2], baT[ia % 2]
                                ia += 1
                                gate_up(g0, gn, a_, ba_)
                                if prev is not None:
                                    down(*prev)
                                prev = (g0, gn, a_, ba_)
                            down(*prev)
                    if not last:
                        for si, (t0, nt) in enumerate(subs):
                            self.dma("sp", self.scr["hbuf"][t0:t0 + nt, :], acc[:nt, si, :], [bacc[si]], [self.sb["hbuf"][0]])
                    else:
                        wf = S2("F_wf", [128, D]); bwf = Buf("wf")
                        self.dma("sp", wf[:], self.din["final_norm_w"].rearrange("(o d) -> o d", o=1).partition_broadcast(128), (), [bwf])
                        junk2 = S2("F_junk", [128, D], BF16)
                        ss2 = [S2("F_ss%d" % i, [128, 1]) for i in range(2)]
                        bss2 = [Buf("ss%d" % i) for i in range(2)]
                        ob = [S2("F_ob%d" % i, [128, D]) for i in range(2)]
                        bob = [Buf("ob%d" % i) for i in range(2)]
                        for si, (t0, nt) in enumerate(subs):
                            if t0 < NM:
                                continue
                            i2 = si % 2
                            self.act(junk2[:nt, :], acc[:nt, si, :], AF.Square, [bacc[si]], [bss2[i2]], accum_out=ss2[i2][:nt, 0:1])
                            self.rstd_from_ss(ss2[i2][:nt, 0:1], ss2[i2][:nt, 0:1], D, [bss2[i2]], [bss2[i2]])
                            self.stt("dve", ob[i2][:nt, :], acc[:nt, si, :], ss2[i2][:nt, 0:1], wf[:nt, :], ALU.mult, ALU.mult,
                                     [bacc[si], bss2[i2], bwf], [bob[i2]])
                            self.dma("sp", self.out[t0 - NM:t0 - NM + nt, :], ob[i2][:nt, :], [bob[i2]], [self.bout])

    def phase_GLA(self, layer):
        nc, P = self.nc, self.P
        P.barrier()
        NC = len(CHUNKS)
        with contextlib.ExitStack() as st:
            S = lambda n, s, d=F32: st.enter_context(nc.sbuf_tensor(self.nm(n), s, d))
            PS = lambda n, s, d=F32: st.enter_context(nc.psum_tensor(self.nm(n), s, d))
            Qf = S("L_Qf", [128, L], BF16); Kf = S("L_Kf", [128, L], BF16)
            Qb = S("L_Qb", [128, L], BF16); Kb = S("L_Kb", [128, L], BF16)
            Qbar = S("L_Qbar", [128, L], BF16)
            bfm = [Buf("fm%d" % i) for i in range(len(GROUPS))]
            Ktf = S("L_Ktf", [128, NC, 128], BF16); Ktb = S("L_Ktb", [128, NC, 128], BF16)
            bKt = [Buf("Kt%d" % i) for i in range(NC)]
            decf = S("L_decf", [128, NC]); decb = S("L_decb", [128, NC])
            bdec = [Buf("dec%d" % i) for i in range(len(GROUPS))]
            vt = S("L_vt", [128, NC, 256], BF16); bvt = Buf("vt")
            src = self.scr["vtok"][NM:, V_GV:V_GV + 256].rearrange("(c p) d -> p c d", p=128)
            self.dma("pool", vt[:, 1:NC, :], src, [self.sb["vtok"][0]], [bvt])
            self.dma("pool", vt[0:NM, 0, :], self.scr["vtok"][0:NM, V_GV:V_GV + 256], [self.sb["vtok"][0]], [bvt])
            w2 = S("L_w2", [32, 2, 128]); bw2 = Buf("w2")
            self.memset("pool", w2[:], 0.0, [bw2])
            for d in range(2):
                self.dma("sp", w2[16 * d:16 * d + 16, d, :], self.din["gla_gate_w2"][layer, d], (), [bw2])
            negb = S("L_negb", [128, 2]); bnegb = Buf("negb")
            for d in range(2):
                self.dma("sp", negb[:, d:d + 1], self.din["gla_gate_b"][layer, d:d + 1, :].rearrange("o c -> c o"), (), [bnegb])
            self.ts("dve", negb[:], negb[:], -1.0, ALU.mult, reads=[bnegb], writes=[bnegb])
            gnw = S("L_gnw", [128, 1]); bgnw = Buf("gnw")
            for r_ in range(2):
                self.dma("sp", gnw[64 * r_:64 * r_ + 64, :], self.din["gla_norm_w"][layer:layer + 1, :].rearrange("o d -> d o"), (), [bgnw])
            hm = S("L_hm", [128, 4]); bhm = Buf("hm")
            self.dma("sp", hm[:], self.din["headmask"][:, :], (), [bhm])
            bdm = S("L_bdm", [128, 256]); bbdm = Buf("bdm")
            self.cp("dve", bdm[:].rearrange("p (h v) -> p h v", h=4), hm[:].unsqueeze(2).to_broadcast([128, 4, 64]), [bhm], [bbdm])
            rm = S("L_rm", [128, 512]); brm = Buf("rm")
            self.dma("sp", rm[:], self.din["resetm"][:, :], (), [brm])
            ones1 = self.ones_f[:, 0:1]
            with contextlib.ExitStack() as st1:
                S1 = lambda n, s, d=F32: st1.enter_context(nc.sbuf_tensor(self.nm(n), s, d))
                PS1 = lambda n, s, d=F32: st1.enter_context(nc.psum_tensor(self.nm(n), s, d))
                T = {}
                for nme in ("a", "q", "k", "e", "g", "F", "x1", "x2", "kh"):
                    T[nme] = [S1("L1_" + nme + str(i), [128, 512]) for i in range(2)]
                khb = [S1("L1_khb%d" % i, [128, 512], BF16) for i in range(2)]
                B1 = {nme: [Buf(nme + str(i)) for i in range(2)] for nme in list(T) + ["khb"]}
                pz = [PS1("L1_pz%d" % i, [128, 512]) for i in range(2)]
                bpz = [Buf("pz%d" % i) for i in range(2)]
                ptr = [PS1("L1_pt%d" % i, [128, 512], BF16) for i in range(2)]
                bptr = [Buf("ptr%d" % i) for i in range(2)]
                fq, fk, fa = FM_IDX["gq"], FM_IDX["gk"], FM_IDX["ga"]
                qscale = 32.0 ** -0.5
                for gi, (g0, gn) in enumerate(GROUPS):
                    i2 = gi % 2
                    a_, q_, k_ = T["a"][i2], T["q"][i2], T["k"][i2]
                    ba_, bq_, bk_ = B1["a"][i2], B1["q"][i2], B1["k"][i2]
                    self.dma("sp", a_[0:32, :gn], self.scr["projF"][fa, 0:32, g0:g0 + gn], [self.sb["projF"][fa]], [ba_])
                    self.dma("sp", q_[:, :gn], self.scr["projF"][fq, :, g0:g0 + gn], [self.sb["projF"][fq]], [bq_])
                    self.dma("sp", k_[:, :gn], self.scr["projF"][fk, :, g0:g0 + gn], [self.sb["projF"][fk]], [bk_])
                    cks = chunks_in(g0, gn)
                    for d in range(2):
                        e_, g_, F_ = T["e"][d], T["g"][d], T["F"][d]
                        be_, bg_, bF_ = B1["e"][d], B1["g"][d], B1["F"][d]
                        self.mm(pz[d][:, :gn], w2[:, d, :], a_[0:32, :gn], True, True, [bw2, ba_], [bpz[d]])
                        self.act(e_[:, :gn], pz[d][:, :gn], AF.Exp, [bpz[d], bnegb], [be_], bias=negb[:, d:d + 1], scale=-1.0)
                        self.act(e_[:, :gn], e_[:, :gn], AF.Ln, [be_, self.b_ones], [be_], bias=ones1, scale=1.0)
                        self.ts("dve", g_[:, :gn], e_[:, :gn], -1.0 / 16.0, ALU.mult, reads=[be_], writes=[bg_])
                        P.op("dve", (lambda e, F_=F_, g_=g_, gn=gn: e.tensor_tensor_scan(
                            out=F_[:, :gn], data0=rm[:, :gn], data1=g_[:, :gn], initial=0.0, op0=ALU.mult, op1=ALU.add)),
                            [brm, bg_], [bF_])
                    Ff, Fb, gb = T["F"][0], T["F"][1], T["g"][1]
                    x1, x2, kh = T["x1"][i2], T["x2"][i2], T["kh"][i2]
                    bx1, bx2, bkh = B1["x1"][i2], B1["x2"][i2], B1["kh"][i2]
                    bo = bfm[gi]
                    sl = slice(g0, g0 + gn)
                    self.act(x1[:, :gn], Ff[:, :gn], AF.Exp, [B1["F"][0]], [bx1])
                    self.stt("dve", Qf[:, sl], q_[:, :gn], qscale, x1[:, :gn], ALU.mult, ALU.mult, [bq_, bx1], [bo])
                    self.act(x2[:, :gn], Ff[:, :gn], AF.Exp, [B1["F"][0]], [bx2], scale=-1.0)
                    self.tt("pool", Kf[:, sl], k_[:, :gn], x2[:, :gn], ALU.mult, [bk_, bx2], [bo])
                    for (c0, cn) in cks:
                        ci = CHUNKS.index((c0, cn))
                        o = c0 - g0
                        last = Ff[:, o + cn - 1:o + cn]
                        self.act(kh[:, o:o + cn], Ff[:, o:o + cn], AF.Exp, [B1["F"][0]], [bkh], bias=last, scale=-1.0)
                        self.act(decf[:, ci:ci + 1], last, AF.Exp, [B1["F"][0]], [bdec[gi]])
                    self.tt("dve", khb[0][:, :gn], k_[:, :gn], kh[:, :gn], ALU.mult, [bk_, bkh], [B1["khb"][0]])
                    self.tt("dve", Fb[:, :gn], Fb[:, :gn], gb[:, :gn], ALU.subtract, [B1["F"][1], B1["g"][1]], [B1["F"][1]])
                    self.act(x1[:, :gn], Fb[:, :gn], AF.Exp, [B1["F"][1]], [bx1], scale=-1.0)
                    self.stt("dve", Qb[:, sl], q_[:, :gn], qscale, x1[:, :gn], ALU.mult, ALU.mult, [bq_, bx1], [bo])
                    self.act(x2[:, :gn], Fb[:, :gn], AF.Exp, [B1["F"][1]], [bx2])
                    self.tt("pool", khb[1][:, :gn], k_[:, :gn], x2[:, :gn], ALU.mult, [bk_, bx2], [B1["khb"][1]])
                    self.cp("pool", Kb[:, sl], khb[1][:, :gn], [B1["khb"][1]], [bo])
                    for (c0, cn) in cks:
                        ci = CHUNKS.index((c0, cn))
                        o = c0 - g0
                        self.tt("dve", decb[:, ci:ci + 1], Fb[:, o + cn - 1:o + cn], gb[:, o + cn - 1:o + cn], ALU.add,
                                [B1["F"][1], B1["g"][1]], [bdec[gi]])
                        self.act(kh[:, o:o + cn], Fb[:, o:o + cn], AF.Exp, [B1["F"][1], bdec[gi]], [bkh],
                                 bias=decb[:, ci:ci + 1], scale=-1.0)
                        self.act(decb[:, ci:ci + 1], decb[:, ci:ci + 1], AF.Exp, [bdec[gi], bkh], [bdec[gi]])
                    self.stt("dve", Qbar[:, sl], q_[:, :gn], qscale, kh[:, :gn], ALU.mult, ALU.mult, [bq_, bkh], [bo])
                    for (c0, cn) in cks:
                        ci = CHUNKS.index((c0, cn))
                        o = c0 - g0
                        for d, dstT in ((0, Ktf), (1, Ktb)):
                            pt_, bpt_ = ptr[d], bptr[d]
                            self.tr(pt_[:cn, 0:128], khb[d][:, o:o + cn], self.identb[:, :], [B1["khb"][d], self.b_identb], [bpt_])
                            self.cp("act" if d == 0 else "dve", dstT[:cn, ci, :], pt_[:cn, 0:128], [bpt_], [bKt[ci]])
            P.barrier()
            with contextlib.ExitStack() as st2:
                S2 = lambda n, s, d=F32: st2.enter_context(nc.sbuf_tensor(self.nm(n), s, d))
                PS2 = lambda n, s, d=F32: st2.enter_context(nc.psum_tensor(self.nm(n), s, d))
                allfm = bfm
                SbBD = S2("L2_SbBD", [128, NC, 256], BF16)
                bSbBD = [Buf("SbBD%d" % i) for i in range(NC)]
                Sst = S2("L2_S", [128, 256]); bS = Buf("S")
                oraw = S2("L2_oraw", [128, 2, L]); boraw = [Buf("oraw%d" % i) for i in range(len(GROUPS))]
                pdS = [PS2("L2_pdS%d" % i, [128, 256]) for i in range(2)]
                bpdS = [Buf("pdS%d" % i) for i in range(2)]
                self.memset("dve", Sst[:], 0.0, [bS])
                for n_, ci in enumerate(range(NC - 1, -1, -1)):
                    c0, cn = CHUNKS[ci]
                    gi = 0 if ci == 0 else 1 + (ci - 1) // 4
                    self.tt("pool", SbBD[:, ci, :], Sst[:], bdm[:], ALU.mult, [bS, bbdm], [bSbBD[ci]])
                    p_, bp_ = pdS[n_ % 2], bpdS[n_ % 2]
                    self.mm(p_[:, :], Ktb[:cn, ci, :], vt[:cn, ci, :], True, True, [bKt[ci], bvt], [bp_])
                    self.stt("dve", Sst[:], Sst[:], decb[:, ci:ci + 1], p_[:, :], ALU.mult, ALU.add, [bS, bdec[gi], bp_], [bS])
                self.memset("dve", Sst[:], 0.0, [bS])
                SfBD = [S2("L2_SfBD%d" % i, [128, 256], BF16) for i in range(2)]
                bSfBD = [Buf("SfBD%d" % i) for i in range(2)]
                Qm = [S2("L2_Qm%d" % i, [128, 4, 128], BF16) for i in range(4)]
                bQm = [Buf("Qm%d" % i) for i in range(4)]
                Am = [S2("L2_Am%d" % i, [128, 4, 128], BF16) for i in range(4)]
                bAm = [Buf("Am%d" % i) for i in range(4)]
                pA = [PS2("L2_pA%d" % i, [128, 512]) for i in range(2)]
                bpA = [Buf("pA%d" % i) for i in range(2)]
                po = [PS2("L2_po%d" % i, [128, 128]) for i in range(2)]
                bpo = [Buf("po%d" % i) for i in range(2)]
                hmb = hm[:, 0:4].unsqueeze(2)
                def gla_front(ci):
                    c0, cn = CHUNKS[ci]
                    gi = 0 if ci == 0 else 1 + (ci - 1) // 4
                    sl = slice(c0, c0 + cn)
                    for d, (Qd, Kd, tri) in enumerate(((Qf, Kf, "tri_incl"), (Qb, Kb, "tri_ge"))):
                        j = (ci % 2) * 2 + d
                        qm, bqm = Qm[j], bQm[j]
                        am, bam = Am[j], bAm[j]
                        self.tt("dve" if d == 0 else "pool", qm[:, :, :cn], Qd[:, sl].unsqueeze(1).to_broadcast([128, 4, cn]),
                                hmb.to_broadcast([128, 4, cn]), ALU.mult, [bfm[gi], bhm], [bqm])
                        pa2 = pA[d][:cn, 0:4 * cn]
                        pa = pa2.rearrange("p (h l) -> p h l", h=4)
                        self.mm(pa2, Kd[:, sl], qm[:, :, :cn], True, True, [bfm[gi], bqm], [bpA[d]])
                        self.tt("dve", am[:cn, :, :cn], pa, self.c[tri][:cn, :cn].unsqueeze(1).to_broadcast([cn, 4, cn]), ALU.mult,
                                [bpA[d], self.const_bufs[tri]], [bam])

                def gla_back(ci):
                    c0, cn = CHUNKS[ci]
                    gi = 0 if ci == 0 else 1 + (ci - 1) // 4
                    sl = slice(c0, c0 + cn)
                    sf, bsf = SfBD[ci % 2], bSfBD[ci % 2]
                    self.tt("pool", sf[:], Sst[:], bdm[:], ALU.mult, [bS, bbdm], [bsf])
                    amf, amb = Am[(ci % 2) * 2], Am[(ci % 2) * 2 + 1]
                    bamf, bamb = bAm[(ci % 2) * 2], bAm[(ci % 2) * 2 + 1]
                    for t in range(2):
                        for hh in range(2):
                            h = 2 * t + hh
                            o_ = po[t][hh * 64:(hh + 1) * 64, :cn]
                            self.mm(o_, sf[:, h * 64:(h + 1) * 64], Qf[:, sl], True, False, [bsf, bfm[gi]], [bpo[t]])
                            self.mm(o_, SbBD[:, ci, h * 64:(h + 1) * 64], Qbar[:, sl], False, False, [bSbBD[ci], bfm[gi]], [bpo[t]])
                            self.mm(o_, vt[:cn, ci, h * 64:(h + 1) * 64], amf[:cn, h, :cn], False, False, [bvt, bamf], [bpo[t]])
                            self.mm(o_, vt[:cn, ci, h * 64:(h + 1) * 64], amb[:cn, h, :cn], False, True, [bvt, bamb], [bpo[t]])
                        self.cp("act", oraw[:, t, sl], po[t][:, :cn], [bpo[t]], [boraw[gi]])
                    p_, bp_ = pdS[ci % 2], bpdS[ci % 2]
                    self.mm(p_[:, :], Ktf[:cn, ci, :], vt[:cn, ci, :], True, True, [bKt[ci], bvt], [bp_])
                    self.stt("dve", Sst[:], Sst[:], decf[:, ci:ci + 1], p_[:, :], ALU.mult, ALU.add, [bS, bdec[gi], bp_], [bS])

                gla_front(0)
                for ci in range(NC):
                    if ci + 1 < NC:
                        gla_front(ci + 1)
                    gla_back(ci)
                xs = [S2("L3_x%d" % i, [128, 512]) for i in range(2)]
                bxs = [Buf("x%d" % i) for i in range(2)]
                sq = S2("L3_sq", [128, 512]); bsq = Buf("sq")
                rs = S2("L3_rs", [128, 512]); brs = Buf("rs")
                on = S2("L3_on", [128, 512]); bon = Buf("on")
                stg = [S2("L3_stg%d" % i, [128, 512], BF16) for i in range(2)]
                bstg = [Buf("stg%d" % i) for i in range(2)]
                it = 0
                for gi, (g0, gn) in enumerate(GROUPS):
                    sl = slice(g0, g0 + gn)
                    for t in range(2):
                        i2 = it % 2
                        it += 1
                        fr = FM_IDX["gr%d" % t]
                        self.dma("sp", xs[i2][:, :gn], self.scr["projF"][fr, :, sl], [self.sb["projF"][fr]], [bxs[i2]])
                        self.act(xs[i2][:, :gn], xs[i2][:, :gn], AF.Silu, [bxs[i2]], [bxs[i2]])
                        self.act(sq[:, :gn], oraw[:, t, sl], AF.Square, [boraw[gi]], [bsq])
                        self.mm(pA[0][:, :gn], self.c["blk64"][:], sq[:, :gn], True, True, [bsq, self.const_bufs["blk64"]], [bpA[0]])
                        self.rstd_from_ss(rs[:, :gn], pA[0][:, :gn], 64, [bpA[0]], [brs])
                        self.stt("dve", on[:, :gn], oraw[:, t, sl], gnw[:, 0:1], rs[:, :gn], ALU.mult, ALU.mult, [boraw[gi], bgnw, brs], [bon])
                        self.tt("dve", stg[i2][:, :gn], on[:, :gn], xs[i2][:, :gn], ALU.mult, [bon, bxs[i2]], [bstg[i2]])
                        self.dma("pool", self.scr["mixT"][2 + t, :, sl], stg[i2][:, :gn], [bstg[i2]], [self.sb["mixT"][2 + t]])

    def phase_SSD(self, layer):
        nc, P = self.nc, self.P
        P.barrier()
        NC = len(CHUNKS)
        NG = len(GROUPS)
        gof = lambda ci: 0 if ci == 0 else 1 + (ci - 1) // 4
        with contextlib.ExitStack() as st:
            S = lambda n, s, d=F32: st.enter_context(nc.sbuf_tensor(self.nm(n), s, d))
            BT = [S("D_BT%d" % g, [128, L], BF16) for g in range(2)]
            CT = [S("D_CT%d" % g, [128, L], BF16) for g in range(2)]
            xT = S("D_xT", [128, 2, L])
            bfm = [Buf("fm%d" % i) for i in range(NG)]
            xtok = S("D_xtok", [128, NC, 256], BF16)
            Btok = S("D_Btok", [128, NC, 256], BF16)
            btok = [Buf("tok%d" % i) for i in range(NC)]
            with contextlib.ExitStack() as st1:
                S1 = lambda n, s, d=F32: st1.enter_context(nc.sbuf_tensor(self.nm(n), s, d))
                PS1 = lambda n, s, d=F32: st1.enter_context(nc.psum_tensor(self.nm(n), s, d))
                w6 = S1("D1_w6", [6, 768]); bw6 = Buf("w6")
                self.dma("sp", w6[0:5, :], self.din["ssd_conv_w"][layer], (), [bw6])
                self.dma("sp", w6[5:6, :], self.din["ssd_conv_b"][layer:layer + 1, :], (), [bw6])
                wcol = S1("D1_wcol", [128, 6, 6]); bwcol = Buf("wcol")
                pw = PS1("D1_pw", [128, 64]); bpw = Buf("pw")
                for ct in range(6):
                    self.tr(pw[:, ct * 6:ct * 6 + 6], w6[0:6, ct * 128:(ct + 1) * 128], self.c["ident_f"][0:6, 0:6],
                            [bw6, self.const_bufs["ident_f"]], [bpw])
                self.cp("dve", wcol[:].rearrange("p a b -> p (a b)"), pw[:, 0:36], [bpw], [bwcol])
                Dg = S1("D1_Dg", [128, 6, 5, 128], BF16); bDg = Buf("Dg")
                for ct in range(6):
                    for j in range(5):
                        self.ts("dve" if (ct + j) % 2 == 0 else "pool", Dg[:, ct, j, :], self.c["ident_f"][:, :], wcol[:, ct, j:j + 1], ALU.mult,
                                reads=[bwcol, self.const_bufs["ident_f"]], writes=[bDg])
                Ub = [S1("D1_Ub%d" % ct, [128, L + 4], BF16) for ct in range(6)]
                bUb = [Buf("Ub%d" % ct) for ct in range(6)]
                names = ["xs0", "xs1", "B0", "B1", "C0", "C1"]
                for ct in range(6):
                    f = FM_IDX[names[ct]]
                    self.memset("pool", Ub[ct][:, 0:2], 0.0, [bUb[ct]])
                    self.memset("pool", Ub[ct][:, L + 2:L + 4], 0.0, [bUb[ct]])
                    self.dma("pool", Ub[ct][:, 2:L + 2], self.scr["projF"][f, :, :], [self.sb["projF"][f]], [bUb[ct]])
                pc = [PS1("D1_pc%d" % i, [128, 512]) for i in range(3)]
                bpc = [Buf("pc%d" % i) for i in range(3)]
                xb = [S1("D1_xb%d" % i, [128, 2, 512], BF16) for i in range(2)]
                bxb = [Buf("xb%d" % i) for i in range(2)]
                ptr = [PS1("D1_ptr%d" % i, [128, 512], BF16) for i in range(2)]
                bptr = [Buf("ptr%d" % i) for i in range(2)]
                n = 0
                for gi, (g0, gn) in enumerate(GROUPS):
                    sl = slice(g0, g0 + gn)
                    x_b, bx_b = xb[gi % 2], bxb[gi % 2]
                    for ct in range(6):
                        p_, bp_ = pc[n % 3], bpc[n % 3]
                        n += 1
                        for j in range(5):
                            self.mm(p_[:, :gn], Dg[:, ct, j, :], Ub[ct][:, g0 + j:g0 + j + gn], j == 0, j == 4, [bDg, bUb[ct]], [bp_])
                        bias = wcol[:, ct, 5:6]
                        if ct < 2:
                            self.act(xT[:, ct, sl], p_[:, :gn], AF.Silu, [bp_, bwcol], [bfm[gi]], bias=bias)
                            self.cp("pool", x_b[:, ct, :gn], xT[:, ct, sl], [bfm[gi]], [bx_b])
                        elif ct < 4:
                            self.act(BT[ct - 2][:, sl], p_[:, :gn], AF.Silu, [bp_, bwcol], [bfm[gi]], bias=bias)
                        else:
                            self.act(CT[ct - 4][:, sl], p_[:, :gn], AF.Silu, [bp_, bwcol], [bfm[gi]], bias=bias)
                    for (c0, cn) in chunks_in(g0, gn):
                        ci = CHUNKS.index((c0, cn))
                        o = c0 - g0
                        pt_, bpt_ = ptr[ci % 2], bptr[ci % 2]
                        for t in range(2):
                            self.tr(pt_[:cn, t * 128:(t + 1) * 128], x_b[:, t, o:o + cn], self.identb[:, :], [bx_b, self.b_identb], [bpt_])
                            self.tr(pt_[:cn, 256 + t * 128:256 + (t + 1) * 128], BT[t][:, c0:c0 + cn], self.identb[:, :],
                                    [bfm[gi], self.b_identb], [bpt_])
                        self.cp("act", xtok[:cn, ci, :], pt_[:cn, 0:256], [bpt_], [btok[ci]])
                        self.cp("dve", Btok[:cn, ci, :], pt_[:cn, 256:512], [bpt_], [btok[ci]])
            P.barrier()
            with contextlib.ExitStack() as st2:
                S2 = lambda n, s, d=F32: st2.enter_context(nc.sbuf_tensor(self.nm(n), s, d))
                PS2 = lambda n, s, d=F32: st2.enter_context(nc.psum_tensor(self.nm(n), s, d))
                dt = S2("D2_dt", [128, NC, 8]); bdt = Buf("dt")
                dA = S2("D2_dA", [128, NC, 8]); bdA = Buf("dA")
                self.memset("pool", dt[:], 0.0, [bdt])
                vtk = self.scr["vtok"]
                self.dma("sp", dt[:, 1:NC, :], vtk[NM:, V_DT:V_DT + 8].rearrange("(c p) d -> p c d", p=128), [self.sb["vtok"][0]], [bdt])
                self.dma("sp", dt[0:NM, 0, :], vtk[0:NM, V_DT:V_DT + 8], [self.sb["vtok"][0]], [bdt])
                brow = S2("D2_brow", [128, 8]); bbrow = Buf("brow")
                arow = S2("D2_arow", [128, 8]); barow = Buf("arow")
                self.dma("sp", brow[:], self.din["ssd_dt_bias"][layer:layer + 1].rearrange("o d h -> o (d h)").partition_broadcast(128), (), [bbrow])
                self.dma("sp", arow[:], self.din["ssd_a_log"][layer:layer + 1].rearrange("o d h -> o (d h)").partition_broadcast(128), (), [barow])
                self.act(arow[:], arow[:], AF.Exp, [barow], [barow])
                self.ts("dve", arow[:], arow[:], -1.0, ALU.mult, reads=[barow], writes=[barow])
                bc8 = lambda t: t[:].unsqueeze(1).to_broadcast([128, NC, 8])
                self.tt("dve", dt[:], dt[:], bc8(brow), ALU.add, [bdt, bbrow], [bdt])
                self.act(dt[:], dt[:], AF.Exp, [bdt], [bdt])
                self.act(dt[:], dt[:], AF.Ln, [bdt, self.b_ones], [bdt], bias=self.ones_f[:, 0:1])
                self.tt("dve", dA[:], dt[:], bc8(arow), ALU.mult, [bdt, barow], [bdA])
                Tst = S2("D2_T", [128, NC, 8]); eT = S2("D2_eT", [128, NC, 8]); bT = [Buf("T%d" % i) for i in range(NC)]
                xwb = S2("D2_xwb", [128, NC, 256], BF16); bxwb = [Buf("xwb%d" % i) for i in range(NC)]
                SbT = S2("D2_SbT", [128, NC, 256], BF16); bSbT = [Buf("SbT%d" % i) for i in range(NC)]
                yraw = S2("D2_yraw", [128, 2, L]); byraw = [Buf("yraw%d" % i) for i in range(NG)]
                pSm = [PS2("D2_pSm0", [128, 16])] * 2
                bpSm = [Buf("pSm0")] * 2
                sm = [S2("D2_sm%d" % i, [128, 16]) for i in range(2)]
                bsm = [Buf("sm%d" % i) for i in range(2)]
                tri_i, tri_s, tri_g = self.c["tri_incl"], self.c["tri_strict"], self.c["tri_ge"]
                btri = [self.const_bufs["tri_incl"], self.const_bufs["tri_strict"], self.const_bufs["tri_ge"]]
                v4 = lambda ap, cn: ap.rearrange("p (h d) -> p h d", h=4)
                for ci, (c0, cn) in enumerate(CHUNKS):
                    i2 = ci % 2
                    ps_, bps_ = pSm[i2], bpSm[i2]
                    s_, bs_ = sm[i2], bsm[i2]
                    self.mm(ps_[:cn, 4:8], tri_s[:cn, :cn], dA[:cn, ci, 4:8], True, True, [bdA, btri[1]], [bps_])
                    self.mm(ps_[:, 8:16], self.ones_f[:cn, :], dA[:cn, ci, 0:8], True, True, [bdA, self.b_ones], [bps_])
                    self.cp("dve", Tst[:, ci, :], ps_[:, 8:16], [bps_], [bT[ci]])
                    self.act(eT[:, ci, :], ps_[:, 8:16], AF.Exp, [bps_], [bT[ci]])
                    self.act(s_[:cn, 4:8], ps_[:cn, 4:8], AF.Exp, [bps_], [bs_])
                    self.tt("dve", s_[:cn, 4:8], s_[:cn, 4:8], dt[:cn, ci, 4:8], ALU.mult, [bs_, bdt], [bs_])
                    self.tt("pool", v4(xwb[:cn, ci, :], cn), v4(xtok[:cn, ci, :], cn), s_[:cn, 4:8].unsqueeze(2).to_broadcast([cn, 4, 64]),
                            ALU.mult, [btok[ci], bs_], [bxwb[ci]])
                Sst = S2("D2_S", [128, 256]); bS = Buf("S")
                pdS = [PS2("D2_pdS%d" % i, [128, 256]) for i in range(2)]
                bpdS = [Buf("pdS%d" % i) for i in range(2)]
                self.memset("dve", Sst[:], 0.0, [bS])
                for n_, ci in enumerate(range(NC - 1, -1, -1)):
                    c0, cn = CHUNKS[ci]
                    self.cp("pool", SbT[:, ci, :], Sst[:], [bS], [bSbT[ci]])
                    p_, bp_ = pdS[n_ % 2], bpdS[n_ % 2]
                    for g in range(2):
                        self.mm(p_[:, g * 128:(g + 1) * 128], Btok[:cn, ci, g * 128:(g + 1) * 128], xwb[:cn, ci, g * 128:(g + 1) * 128],
                                True, True, [btok[ci], bxwb[ci]], [bp_])
                    self.tt("dve", v4(Sst[:], 0), v4(Sst[:], 0), eT[:, ci, 4:8].unsqueeze(2).to_broadcast([128, 4, 64]), ALU.mult,
                            [bS, bT[ci]], [bS])
                    self.tt("dve", Sst[:], Sst[:], p_[:, :], ALU.add, [bS, bp_], [bS])
                self.memset("dve", Sst[:], 0.0, [bS])
                SfT = [S2("D2_SfT%d" % i, [128, 256], BF16) for i in range(2)]
                bSfT = [Buf("SfT%d" % i) for i in range(2)]
                dArep = [S2("D2_dArep%d" % i, [128, 8, 128]) for i in range(2)]
                bdArep = [Buf("dArep%d" % i) for i in range(2)]
                pG = PS2("D2_pG", [128, 2, 128]); bpG = Buf("pG")
                pR = [PS2("D2_pR%d" % d, [128, 4, 128]) for d in range(2)]
                bpR = [Buf("pR%d" % d) for d in range(2)]
                po = [PS2("D2_po%d" % t, [128, 128]) for t in range(2)]; bpo = [Buf("po%d" % t) for t in range(2)]
                E = [S2("D2_E%d" % d, [128, 4, 128]) for d in range(2)]
                bE = [Buf("E%d" % d) for d in range(2)]
                Wm = [[S2("D2_W%d_%d" % (i, d), [128, 4, 128], BF16) for d in range(2)] for i in range(2)]
                bWm = [[Buf("W%d_%d" % (i, d)) for d in range(2)] for i in range(2)]
                er = [S2("D2_er%d" % d, [128, 4, 128]) for d in range(2)]
                ber = [Buf("er%d" % d) for d in range(2)]
                Cs = [[S2("D2_Cs%d_%d" % (i, d), [128, 4, 128], BF16) for d in range(2)] for i in range(2)]
                bCs = [[Buf("Cs%d_%d" % (i, d)) for d in range(2)] for i in range(2)]
                xd = [[S2("D2_xd%d_%d" % (i, d), [128, 256], BF16) for d in range(2)] for i in range(2)]
                bxd = [[Buf("xd%d_%d" % (i, d)) for d in range(2)] for i in range(2)]
                xwf = [S2("D2_xwf%d" % i, [128, 256], BF16) for i in range(2)]
                bxwf = [Buf("xwf%d" % i) for i in range(2)]
                def ssd_front(ci):
                    c0, cn = CHUNKS[ci]
                    i2 = ci % 2
                    gi = gof(ci)
                    sl = slice(c0, c0 + cn)
                    ps_, bps_ = pSm[i2], bpSm[i2]
                    s_, bs_ = sm[i2], bsm[i2]
                    self.mm(ps_[:cn, 0:4], tri_i[:cn, :cn], dA[:cn, ci, 0:4], True, True, [bdA, btri[0]], [bps_])
                    self.mm(ps_[:cn, 4:8], tri_s[:cn, :cn], dA[:cn, ci, 4:8], True, True, [bdA, btri[1]], [bps_])
                    self.cp("act", s_[:cn, 0:8], ps_[:cn, 0:8], [bps_], [bs_])
                    for d in range(2):
                        tri_r = tri_i if d == 0 else tri_s
                        for h in range(4):
                            self.mm(pR[d][:, h, :cn], dA[:cn, ci, 4 * d + h:4 * d + h + 1].to_broadcast([cn, 128]), tri_r[:cn, :cn], True, True, [bdA, btri[d]], [bpR[d]])
                    for g in range(2):
                        self.mm(pG[:cn, g, :cn], BT[g][:, sl], CT[g][:, sl], True, True, [bfm[gi]], [bpG])

                def ssd_front2(ci):
                    c0, cn = CHUNKS[ci]
                    i2 = ci % 2
                    gi = gof(ci)
                    sl = slice(c0, c0 + cn)
                    ps_, bps_ = pSm[i2], bpSm[i2]
                    s_, bs_ = sm[i2], bsm[i2]
                    for d in range(2):
                        colb = s_[:cn, 4 * d:4 * d + 4].unsqueeze(2).to_broadcast([cn, 4, cn])
                        self.stt("dve", E[d][:cn, :, :cn], colb, -1.0, pR[d][:cn, :, :cn], ALU.mult, ALU.add, [bs_, bpR[d]], [bE[d]])
                    for d in range(2):
                        self.stt("dve", E[d][:cn, :, :cn], E[d][:cn, :, :cn], -1.0, E[d][:cn, :, :cn], ALU.mult, ALU.min, [bE[d]], [bE[d]])
                    self.stt("dve", er[1][:, :, :cn], pR[1][:, :, :cn], -1.0, Tst[:, ci, 4:8].unsqueeze(2).to_broadcast([128, 4, cn]),
                             ALU.mult, ALU.add, [bpR[1], bT[ci]], [ber[1]])
                    for d in range(2):
                        self.tt("dve", v4(xd[i2][d][:cn, :], cn), v4(xtok[:cn, ci, :], cn),
                                dt[:cn, ci, 4 * d:4 * d + 4].unsqueeze(2).to_broadcast([cn, 4, 64]), ALU.mult, [btok[ci], bdt], [bxd[i2][d]])
                    self.tt("dve", s_[:cn, 8:12], Tst[:cn, ci, 0:4], s_[:cn, 0:4], ALU.subtract, [bT[ci], bs_], [bs_])
                    for d in range(2):
                        self.act(E[d][:cn, :, :cn], E[d][:cn, :, :cn], AF.Exp, [bE[d]], [bE[d]])
                    self.act(er[0][:, :, :cn], pR[0][:, :, :cn], AF.Exp, [bpR[0]], [ber[0]])
                    self.act(er[1][:, :, :cn], er[1][:, :, :cn], AF.Exp, [ber[1]], [ber[1]])
                    self.act(s_[:cn, 8:12], s_[:cn, 8:12], AF.Exp, [bs_], [bs_])
                    for d in range(2):
                        tri_m = tri_i if d == 0 else tri_g
                        self.tt("pool", E[d][:cn, :, :cn], E[d][:cn, :, :cn], tri_m[:cn, :cn].unsqueeze(1).to_broadcast([cn, 4, cn]), ALU.mult,
                                [bE[d], btri[0 if d == 0 else 2]], [bE[d]])
                    for d in range(2):
                        for g in range(2):
                            self.tt("pool", Cs[i2][d][:, 2 * g:2 * g + 2, :cn], er[d][:, 2 * g:2 * g + 2, :cn],
                                    CT[g][:, sl].unsqueeze(1).to_broadcast([128, 2, cn]), ALU.mult, [ber[d], bfm[gi]], [bCs[i2][d]])
                    self.tt("pool", v4(xwf[i2][:cn, :], cn), v4(xd[i2][0][:cn, :], cn), s_[:cn, 8:12].unsqueeze(2).to_broadcast([cn, 4, 64]),
                            ALU.mult, [bxd[i2][0], bs_], [bxwf[i2]])
                    for d in range(2):
                        w_, bw_ = Wm[i2][d], bWm[i2][d]
                        self.tt("dve", w_[:cn, :, :cn].rearrange("p (g r) l -> p g r l", g=2),
                                E[d][:cn, :, :cn].rearrange("p (g r) l -> p g r l", g=2),
                                pG[:cn, :, :cn].unsqueeze(2).to_broadcast([cn, 2, 2, cn]), ALU.mult, [bE[d], bpG], [bw_])

                def ssd_back(ci):
                    c0, cn = CHUNKS[ci]
                    i2 = ci % 2
                    gi = gof(ci)
                    sl = slice(c0, c0 + cn)
                    sf, bsf = SfT[i2], bSfT[i2]
                    self.cp("act", sf[:], Sst[:], [bS], [bsf])
                    for t in range(2):
                        for hh in range(2):
                            h = 2 * t + hh
                            o_ = po[t][hh * 64:(hh + 1) * 64, :cn]
                            self.mm(o_, sf[:, h * 64:(h + 1) * 64], Cs[i2][0][:, h, :cn], True, False, [bsf, bCs[i2][0]], [bpo[t]])
                            self.mm(o_, SbT[:, ci, h * 64:(h + 1) * 64], Cs[i2][1][:, h, :cn], False, False, [bSbT[ci], bCs[i2][1]], [bpo[t]])
                            self.mm(o_, xd[i2][0][:cn, h * 64:(h + 1) * 64], Wm[i2][0][:cn, h, :cn], False, False, [bxd[i2][0], bWm[i2][0]], [bpo[t]])
                            self.mm(o_, xd[i2][1][:cn, h * 64:(h + 1) * 64], Wm[i2][1][:cn, h, :cn], False, True, [bxd[i2][1], bWm[i2][1]], [bpo[t]])
                        self.cp("act", yraw[:, t, sl], po[t][:, :cn], [bpo[t]], [byraw[gi]])
                    p_, bp_ = pdS[i2], bpdS[i2]
                    for g in range(2):
                        self.mm(p_[:, g * 128:(g + 1) * 128], Btok[:cn, ci, g * 128:(g + 1) * 128], xwf[i2][:cn, g * 128:(g + 1) * 128],
                                True, True, [btok[ci], bxwf[i2]], [bp_])
                    self.tt("dve", v4(Sst[:], 0), v4(Sst[:], 0), eT[:, ci, 0:4].unsqueeze(2).to_broadcast([128, 4, 64]), ALU.mult,
                            [bS, bT[ci]], [bS])
                    self.tt("dve", Sst[:], Sst[:], p_[:, :], ALU.add, [bS, bp_], [bS])

                ssd_front(0)
                ssd_front2(0)
                for ci in range(NC):
                    if ci + 1 < NC:
                        ssd_front(ci + 1)
                    ssd_back(ci)
                    if ci + 1 < NC:
                        ssd_front2(ci + 1)
                Dcol = S2("D3_D", [128, 2]); bD = Buf("D")
                nw = S2("D3_nw", [128, 2]); bnw = Buf("nw")
                for t in range(2):
                    for hh in range(2):
                        h = 2 * t + hh
                        self.dma("sp", Dcol[hh * 64:(hh + 1) * 64, t:t + 1], self.din["ssd_d"][layer:layer + 1, h:h + 1].partition_broadcast(64), (), [bD])
                    self.dma("sp", nw[:, t:t + 1], self.din["ssd_norm_w"][layer:layer + 1, t * 128:(t + 1) * 128].rearrange("o d -> d o"), (), [bnw])
                P.barrier()
                f2 = lambda t_: t_[:].rearrange("p h l -> p (h l)")
                zs = [f2(E[0]), f2(E[1])]
                bzs = [Buf("z%d" % i) for i in range(2)]
                yg = [f2(er[0]), f2(er[1])]
                byg = [Buf("y%d" % i) for i in range(2)]
                sq = [f2(dArep[0])[:, 0:512], f2(dArep[1])[:, 0:512]]
                bsq = [Buf("sq%d" % i) for i in range(2)]
                rs = f2(dArep[0])[:, 512:1024]; brs = Buf("rs")
                stg = [f2(Wm[0][0]), f2(Wm[0][1])]
                bstg = [Buf("stg%d" % i) for i in range(2)]
                pss = pR[0][:].rearrange("p h l -> p (h l)"); bpss = Buf("pss")
                for gi, (g0, gn) in enumerate(GROUPS):
                    sl = slice(g0, g0 + gn)
                    for t in range(2):
                        fz = FM_IDX["z%d" % t]
                        self.dma("sp", zs[t][:, :gn], self.scr["projF"][fz, :, sl], [self.sb["projF"][fz]], [bzs[t]])
                        self.act(zs[t][:, :gn], zs[t][:, :gn], AF.Silu, [bzs[t]], [bzs[t]])
                        self.stt("dve", yg[t][:, :gn], xT[:, t, sl], Dcol[:, t:t + 1], yraw[:, t, sl], ALU.mult, ALU.add,
                                 [bfm[gi], bD, byraw[gi]], [byg[t]])
                        self.tt("dve", yg[t][:, :gn], yg[t][:, :gn], zs[t][:, :gn], ALU.mult, [byg[t], bzs[t]], [byg[t]])
                        self.act(sq[t][:, :gn], yg[t][:, :gn], AF.Square, [byg[t]], [bsq[t]])
                        self.mm(pss[:, :gn], self.ones_f[:, :], sq[t][:, :gn], t == 0, t == 1, [bsq[t], self.b_ones], [bpss])
                    self.rstd_from_ss(rs[:, :gn], pss[:, :gn], 256, [bpss], [brs])
                    for t in range(2):
                        self.stt("dve", stg[t][:, :gn], yg[t][:, :gn], nw[:, t:t + 1], rs[:, :gn], ALU.mult, ALU.mult,
                                 [byg[t], bnw, brs], [bstg[t]])
                        self.dma("pool", self.scr["mixT"][t, :, sl], stg[t][:, :gn], [bstg[t]], [self.sb["mixT"][t]])


def build_program():
    kb = KB()
    with contextlib.ExitStack() as st:
        kb.load_consts(st)
        for layer in range(DEPTH):
            kb.phase_A(layer)
            kb.phase_SSD(layer)
            kb.phase_GLA(layer)
            kb.phase_SWA(layer)
            kb.phase_G2(layer)
            kb.phase_C(layer)
        kb.P.emit()
    return kb


def kernel(**inputs):
    n_cores = 8
    kb = build_program()
    consts = make_consts()
    shared = {}
    for k in PARAM_SHAPES:
        shared[k] = np.ascontiguousarray(np.asarray(inputs[k], dtype=np.float32))
    for k, v in consts.items():
        shared["c_" + k] = v
    x = np.asarray(inputs["x"], dtype=np.float32)
    in_maps = []
    for b in range(n_cores):
        m = dict(shared)
        m["x"] = np.ascontiguousarray(x[b])
        in_maps.append(m)
    res = run_bass_kernel_spmd(kb.nc, in_maps, core_ids=list(range(n_cores)))
    out = np.stack([np.asarray(r["out"], dtype=np.float32) for r in res.results], axis=0)
    return out
```
